# Optimizing a Trainium2 kernel written in Bass

```python
import math
import jax, jax.numpy as jnp
from jax import lax
import numpy as np

D_MODEL = 1024
BATCH = 4
SEQ = 8192
DEPTH = 2

N_MIXERS = 2
N_HGRN_LAYERS = (DEPTH + 1) // 2
N_SSD_LAYERS = DEPTH // 2
EPS = 1e-6

HG_EXPAND = 128
HG_HEADS = D_MODEL // HG_EXPAND
HG_DK = HG_EXPAND
HG_DV = D_MODEL // HG_HEADS
HG_QK = HG_HEADS * HG_DK
HG_V = HG_HEADS * HG_DV
HG_IN = 2 * HG_QK + 2 * HG_V
HG_CHUNK = 64

SSD_EXPAND = 2
D_INNER = SSD_EXPAND * D_MODEL
SSD_HEADDIM = 64
SSD_HEADS = D_INNER // SSD_HEADDIM
D_STATE = 128
N_GROUPS = 4
HEADS_PER_GROUP = SSD_HEADS // N_GROUPS
CONV_W = 4
CONV_DIM = D_INNER + 2 * N_GROUPS * D_STATE
SSD_IN = D_INNER + CONV_DIM + SSD_HEADS
SSD_CHUNK = 64
DT_MIN = 1e-3
DT_MAX = 1e-1

D_FF = 2816
N_EXPERTS = 8
TOP_K = 2

kernel_name = "hybrid_hgrn2_mamba2_moe_trunk"


def rmsnorm(x, w):
    xf = x.astype(jnp.float32)
    y = xf * lax.rsqrt(jnp.mean(xf * xf, axis=-1, keepdims=True) + EPS)
    return (y * w.astype(jnp.float32)).astype(x.dtype)


def to_chunks(t, c):
    b, l = t.shape[:2]
    t = t.reshape((b, l // c, c) + t.shape[2:])
    return jnp.moveaxis(t, 1, 0)


def from_chunks(t):
    t = jnp.moveaxis(t, 0, 1)
    return t.reshape((t.shape[0], t.shape[1] * t.shape[2]) + t.shape[3:])


def hgrn_lower_bounds(lb_logits):
    p = jax.nn.softmax(lb_logits.astype(jnp.float32), axis=0)
    return jnp.cumsum(p, axis=0)[:-1]


def hgrn2_mixer(h, w_in, lb, norm_w, w_out):
    b, l, _ = h.shape
    f32 = jnp.float32
    proj = (h @ w_in).astype(f32)
    q, f_pre, v, g = jnp.split(proj, [HG_QK, 2 * HG_QK, 2 * HG_QK + HG_V], axis=-1)
    f = lb + (1.0 - lb) * jax.nn.sigmoid(f_pre)
    log_f = jnp.log(f)
    k = 1.0 - f
    qh = to_chunks(q.reshape(b, l, HG_HEADS, HG_DK), HG_CHUNK)
    kh = to_chunks(k.reshape(b, l, HG_HEADS, HG_DK), HG_CHUNK)
    lfh = to_chunks(log_f.reshape(b, l, HG_HEADS, HG_DK), HG_CHUNK)
    vh = to_chunks(v.reshape(b, l, HG_HEADS, HG_DV), HG_CHUNK)
    causal = jnp.tril(jnp.ones((HG_CHUNK, HG_CHUNK), bool))[None, :, :, None, None]

    def step(state, inp):
        qc, kc, lfc, vc = inp
        bcum = jnp.cumsum(lfc, axis=1)
        diff = bcum[:, :, None] - bcum[:, None, :]
        decay = jnp.exp(jnp.where(causal, diff, -jnp.inf))
        attn = jnp.einsum('bthk,bshk,btshk->bhts', qc, kc, decay)
        o_intra = jnp.einsum('bhts,bshv->bthv', attn, vc)
        o_inter = jnp.einsum('bthk,bhkv->bthv', qc * jnp.exp(bcum), state)
        b_last = bcum[:, -1]
        k_dec = kc * jnp.exp(b_last[:, None] - bcum)
        new_state = state * jnp.exp(b_last)[..., None] + jnp.einsum('bshk,bshv->bhkv', k_dec, vc)
        return new_state, o_intra + o_inter

    s0 = jnp.zeros((b, HG_HEADS, HG_DK, HG_DV), f32)
    _, o = lax.scan(step, s0, (qh, kh, lfh, vh))
    o = from_chunks(o)
    o = rmsnorm(o, norm_w) * jax.nn.sigmoid(g).reshape(b, l, HG_HEADS, HG_DV)
    return o.reshape(b, l, HG_V).astype(h.dtype) @ w_out


def ssd_mixer(h, w_in, conv_w, conv_b, dt_bias, a_log, d_skip, norm_w, w_out):
    b, l, _ = h.shape
    f32 = jnp.float32
    proj = h @ w_in
    z, xbc, dt = jnp.split(proj, [D_INNER, D_INNER + CONV_DIM], axis=-1)
    xbc = lax.conv_general_dilated(
        xbc, conv_w[:, None, :].astype(xbc.dtype), window_strides=(1,),
        padding=((CONV_W - 1, 0),), dimension_numbers=('NWC', 'WIO', 'NWC'),
        feature_group_count=CONV_DIM)
    xbc = jax.nn.silu((xbc + conv_b.astype(xbc.dtype)).astype(f32))
    xs, bm, cm = jnp.split(xbc, [D_INNER, D_INNER + N_GROUPS * D_STATE], axis=-1)
    xs = xs.reshape(b, l, N_GROUPS, HEADS_PER_GROUP, SSD_HEADDIM)
    bm = bm.reshape(b, l, N_GROUPS, D_STATE)
    cm = cm.reshape(b, l, N_GROUPS, D_STATE)
    dt = jax.nn.softplus(dt.astype(f32) + dt_bias.astype(f32))
    dt = dt.reshape(b, l, N_GROUPS, HEADS_PER_GROUP)
    a = -jnp.exp(a_log.astype(f32)).reshape(N_GROUPS, HEADS_PER_GROUP)
    da = dt * a
    causal = jnp.tril(jnp.ones((SSD_CHUNK, SSD_CHUNK), bool))[None, :, :, None, None]

    def step(state, inp):
        xc, bc, cc, dtc, dac = inp
        cs = jnp.cumsum(dac, axis=1)
        seg = cs[:, :, None] - cs[:, None, :]
        lmat = jnp.exp(jnp.where(causal, seg, -jnp.inf))
        cb = jnp.einsum('btgn,bsgn->bgts', cc, bc)
        y_intra = jnp.einsum('bgts,btsgr,bsgr,bsgrp->btgrp', cb, lmat, dtc, xc)
        y_inter = jnp.einsum('btgn,bgrpn->btgrp', cc, state) * jnp.exp(cs)[..., None]
        last = cs[:, -1]
        w = dtc * jnp.exp(last[:, None] - cs)
        new_state = state * jnp.exp(last)[..., None, None] + jnp.einsum('bsgn,bsgr,bsgrp->bgrpn', bc, w, xc)
        return new_state, y_intra + y_inter

    s0 = jnp.zeros((b, N_GROUPS, HEADS_PER_GROUP, SSD_HEADDIM, D_STATE), f32)
    inputs = tuple(to_chunks(t, SSD_CHUNK) for t in (xs, bm, cm, dt, da))
    _, y = lax.scan(step, s0, inputs)
    y = from_chunks(y) + d_skip.astype(f32).reshape(N_GROUPS, HEADS_PER_GROUP)[..., None] * xs
    y = y.reshape(b, l, D_INNER) * jax.nn.silu(z.astype(f32))
    y = rmsnorm(y.reshape(b, l, N_GROUPS, D_INNER // N_GROUPS), jnp.ones((), f32))
    y = y.reshape(b, l, D_INNER) * norm_w.astype(f32)
    return y.astype(h.dtype) @ w_out


def swiglu(h, w_gate, w_up, w_down):
    return (jax.nn.silu(h @ w_gate) * (h @ w_up)) @ w_down


def moe_ffn(h, router_w, w_gate, w_up, w_down):
    b, l, d = h.shape
    t = h.reshape(b * l, d)
    probs = jax.nn.softmax((t @ router_w).astype(jnp.float32), axis=-1)
    top_p, top_i = lax.top_k(probs, TOP_K)
    top_p = top_p / jnp.sum(top_p, axis=-1, keepdims=True)
    gates = jnp.sum(jax.nn.one_hot(top_i, N_EXPERTS, dtype=jnp.float32) * top_p[..., None], axis=1)
    gates = gates.astype(t.dtype)
    out = jnp.zeros_like(t)
    for e in range(N_EXPERTS):
        out = out + gates[:, e:e + 1] * swiglu(t, w_gate[e], w_up[e], w_down[e])
    return out.reshape(b, l, d)


def setup_inputs(seed: int = 0) -> dict:
    key = jax.random.key(seed)
    ks = jax.random.split(key, 26)
    f32 = jnp.float32
    na, nb = N_HGRN_LAYERS, N_SSD_LAYERS

    def nrm(k, shape, fan_in):
        return jax.random.normal(k, shape, f32) * fan_in ** -0.5

    def gain(k, shape):
        return 1.0 + 0.02 * jax.random.normal(k, shape, f32)

    dt = jnp.exp(jax.random.uniform(ks[12], (nb, SSD_HEADS), f32, math.log(DT_MIN), math.log(DT_MAX)))
    return {
        "x": jax.random.normal(ks[0], (BATCH, SEQ, D_MODEL), f32),
        "mix_norm_w": gain(ks[1], (DEPTH, D_MODEL)),
        "ffn_norm_w": gain(ks[2], (DEPTH, D_MODEL)),
        "final_norm_w": gain(ks[3], (D_MODEL,)),
        "hg_w_in": nrm(ks[4], (na, D_MODEL, HG_IN), D_MODEL),
        "hg_lb_logits": 0.5 * jax.random.normal(ks[5], (na + 1, HG_QK), f32),
        "hg_norm_w": gain(ks[6], (na, HG_DV)),
        "hg_w_out": nrm(ks[7], (na, HG_V, D_MODEL), HG_V),
        "ssd_w_in": nrm(ks[8], (nb, D_MODEL, SSD_IN), D_MODEL),
        "ssd_conv_w": nrm(ks[9], (nb, CONV_W, CONV_DIM), CONV_W),
        "ssd_conv_b": 0.02 * jax.random.normal(ks[10], (nb, CONV_DIM), f32),
        "ssd_dt_bias": dt + jnp.log(-jnp.expm1(-dt)),
        "ssd_a_log": jnp.log(jax.random.uniform(ks[13], (nb, SSD_HEADS), f32, 1.0, 16.0)),
        "ssd_d": 1.0 + 0.1 * jax.random.normal(ks[14], (nb, SSD_HEADS), f32),
        "ssd_norm_w": gain(ks[15], (nb, D_INNER)),
        "ssd_w_out": nrm(ks[16], (nb, D_INNER, D_MODEL), D_INNER),
        "ffn_w_gate": nrm(ks[17], (na, D_MODEL, D_FF), D_MODEL),
        "ffn_w_up": nrm(ks[18], (na, D_MODEL, D_FF), D_MODEL),
        "ffn_w_down": nrm(ks[19], (na, D_FF, D_MODEL), D_FF),
        "moe_router": nrm(ks[20], (nb, D_MODEL, N_EXPERTS), D_MODEL),
        "moe_w_gate": nrm(ks[21], (nb, N_EXPERTS, D_MODEL, D_FF), D_MODEL),
        "moe_w_up": nrm(ks[22], (nb, N_EXPERTS, D_MODEL, D_FF), D_MODEL),
        "moe_w_down": nrm(ks[23], (nb, N_EXPERTS, D_FF, D_MODEL), D_FF),
    }


def reference(x, mix_norm_w, ffn_norm_w, final_norm_w,
              hg_w_in, hg_lb_logits, hg_norm_w, hg_w_out,
              ssd_w_in, ssd_conv_w, ssd_conv_b, ssd_dt_bias, ssd_a_log, ssd_d, ssd_norm_w, ssd_w_out,
              ffn_w_gate, ffn_w_up, ffn_w_down,
              moe_router, moe_w_gate, moe_w_up, moe_w_down):
    lbs = hgrn_lower_bounds(hg_lb_logits)
    for layer in range(DEPTH):
        j = layer // N_MIXERS
        h = rmsnorm(x, mix_norm_w[layer])
        if layer % N_MIXERS == 0:
            x = x + hgrn2_mixer(h, hg_w_in[j], lbs[j], hg_norm_w[j], hg_w_out[j])
        else:
            x = x + ssd_mixer(h, ssd_w_in[j], ssd_conv_w[j], ssd_conv_b[j], ssd_dt_bias[j],
                              ssd_a_log[j], ssd_d[j], ssd_norm_w[j], ssd_w_out[j])
        h = rmsnorm(x, ffn_norm_w[layer])
        if layer % 2 == 0:
            x = x + swiglu(h, ffn_w_gate[j], ffn_w_up[j], ffn_w_down[j])
        else:
            x = x + moe_ffn(h, moe_router[j], moe_w_gate[j], moe_w_up[j], moe_w_down[j])
    return rmsnorm(x, final_norm_w)
```

```python
import numpy as np
from contextlib import ExitStack
import concourse.bass as bass
import concourse.mybir as mybir
from concourse.bass_utils import run_bass_kernel_spmd

F32, BF16 = mybir.dt.float32, mybir.dt.bfloat16
AF = mybir.ActivationFunctionType
ALU = mybir.AluOpType
AX = mybir.AxisListType

D = 1024
EPS = 1e-6
HG_H = 8
DFF = 2816
NFC = DFF // 128
NE = 8
DIN = 2048
SSD_H = 32
NG = 4
SSD_IN = 5152
CONV = 3072
SELF_SYNC = True


class T:
    def __init__(self, t, name=""):
        self.t = t
        self.name = name
        self.lw = None
        self.rd = {}

    def __getitem__(self, idx):
        return self.t[idx]


class Stream:
    def __init__(self, name):
        self.name = name
        self.ops = []
        self.seen = {}
        self.cnt = 0
        self.dcnt = None
        self.di = 0


class Prog:
    NDS = 12

    def __init__(self, nc, es):
        self.nc = nc
        self.es = es
        self.sems = {}
        self.streams = {}
        for n in ["pe", "act", "dve", "pool", "sp"]:
            st = Stream(n)
            self.streams[n] = st
            self.sems[n] = es.enter_context(nc.semaphore("s_" + n))
        for n in ["pool", "sp", "act"]:
            st = self.streams[n]
            st.dcnt = [0] * self.NDS
            for i in range(self.NDS):
                self.sems[(n, i)] = es.enter_context(nc.semaphore("d_%s%d" % (n, i)))

    def sb(self, name, shape, dt):
        return T(self.es.enter_context(self.nc.sbuf_tensor(name, list(shape), dt)), name)

    def ps(self, name, shape, dt):
        return T(self.es.enter_context(self.nc.psum_tensor(name, list(shape), dt)), name)

    def op(self, s, fn, r=(), w=(), dma=False):
        st = self.streams[s]
        waits = {}

        def addw(ev, raw):
            if ev is None:
                return
            key, val, owner, is_dma = ev
            if owner == s and not is_dma:
                if s == "pe" or not SELF_SYNC:
                    return
            if st.seen.get(key, 0) >= val:
                return
            if waits.get(key, 0) < val:
                waits[key] = val

        for t in r:
            addw(t.lw, True)
        for t in w:
            addw(t.lw, False)
            for key, (val, owner, is_dma) in t.rd.items():
                addw((key, val, owner, is_dma), False)
        if dma:
            i = st.di % self.NDS
            st.di += 1
            if st.dcnt[i] > 0 and st.seen.get((s, i), 0) < st.dcnt[i]:
                waits[(s, i)] = st.dcnt[i]
        for key, val in waits.items():
            st.seen[key] = val
        if dma:
            st.dcnt[i] += 16
            key = (s, i)
            ev = (key, st.dcnt[i], s, True)
            inc = 16
        else:
            st.cnt += 1
            key = s
            ev = (key, st.cnt, s, False)
            inc = 1
        st.ops.append((list(waits.items()), fn, key, inc))
        for t in w:
            t.lw = ev
            t.rd = {}
        for t in r:
            if t.lw is ev:
                continue
            old = t.rd.get(key)
            if old is None or old[0] < ev[1]:
                t.rd[key] = (ev[1], s, ev[3])
        return ev

    def dma(self, q, out, in_, r=(), w=(), **kw):
        return self.op(q, ("dma_start", dict(out=out, in_=in_, **kw)), r=r, w=w, dma=True)

    def barrier(self):
        targets = {}
        for n, st in self.streams.items():
            if st.cnt > 0:
                targets[n] = st.cnt
            if st.dcnt is not None:
                for i, v in enumerate(st.dcnt):
                    if v > 0:
                        targets[(n, i)] = v
        for n, st in self.streams.items():
            waits = {}
            for key, val in targets.items():
                if key == n and n == "pe":
                    continue
                if st.seen.get(key, 0) < val:
                    waits[key] = val
                    st.seen[key] = val
            st.ops.append((list(waits.items()), None, None, 0))

    def final_wait(self, s, tiles):
        st = self.streams[s]
        waits = {}
        for t in tiles:
            ev = t.lw
            if ev is not None and waits.get(ev[0], 0) < ev[1]:
                waits[ev[0]] = ev[1]
        st.ops.append((list(waits.items()), None, None, 0))

    def emit(self):
        nc = self.nc
        names = {"pe": "tensor", "act": "scalar", "dve": "vector", "pool": "gpsimd", "sp": "sync"}
        with nc.Block() as block:
            for s, attr in names.items():
                st = self.streams[s]

                def body(e, st=st):
                    for waits, fn, key, inc in st.ops:
                        for k, v in waits:
                            e.wait_ge(self.sems[k], v)
                        if fn is None:
                            continue
                        if isinstance(fn, tuple):
                            ins = getattr(e, fn[0])(**fn[1])
                        else:
                            ins = fn(e)
                        ins.then_inc(self.sems[key], inc)

                getattr(block, attr)(body)


def mm_group(P, out_t, out_ap, pairs, r):
    def fn(e):
        n = len(pairs)
        ins = None
        for i, (l, rr) in enumerate(pairs):
            ins = e.matmul(out_ap, lhsT=l, rhs=rr, start=(i == 0), stop=(i == n - 1))
        return ins

    return P.op("pe", fn, r=r, w=[out_t])


class StopBuild(Exception):
    pass


class PhaseStack(ExitStack):
    stopped = False

    def __exit__(self, et, ev, tb):
        if et is StopBuild:
            PhaseStack.stopped = True
            super().__exit__(None, None, None)
            return True
        return super().__exit__(et, ev, tb)


def build_program(L, debug=None):
    nc = bass.Bass("TRN2", target_bir_lowering=False)
    es = ExitStack()
    PhaseStack.stopped = False
    NT = L // 128
    NBLK = L // 512
    LH = L // 2

    _ins = {}
    SHAPES = {
        "x": [L, D], "sel": [2], "mix_norm_w": [2, D], "ffn_norm_w": [2, D], "final_norm_w": [D],
        "hg_w_in": [D, 4096], "hg_lb_logits": [2, D], "hg_norm_w": [128], "hg_w_out": [D, D],
        "ssd_w_in": [D, SSD_IN], "ssd_conv_w": [4, CONV], "ssd_conv_b": [CONV], "ssd_dt_bias": [SSD_H],
        "ssd_a_log": [SSD_H], "ssd_d": [SSD_H], "ssd_norm_w": [DIN], "ssd_w_out": [DIN, D],
        "ffn_w_gate": [D, DFF], "ffn_w_up": [D, DFF], "ffn_w_down": [DFF, D], "moe_router": [D, NE],
        "moe_w_gate": [NE, D, DFF], "moe_w_up": [NE, D, DFF], "moe_w_down": [NE, DFF, D],
    }

    def IN(name):
        if name not in _ins:
            _ins[name] = nc.dram_tensor(name, list(SHAPES[name]), F32, kind="ExternalInput").ap()
        return _ins[name]

    out = nc.dram_tensor("out", [LH, D], F32, kind="ExternalOutput").ap()
    dbg = None
    if debug is not None:
        dbg = nc.dram_tensor("dbg", [L, D], F32, kind="ExternalOutput").ap()

    X1 = nc.dram_tensor("X1", [L, D], F32, kind="Internal").ap()
    X2 = nc.dram_tensor("X2", [L, D], F32, kind="Internal").ap()
    X3 = nc.dram_tensor("X3", [L, D], F32, kind="Internal").ap()
    X1t = [T(None, "X1_%d" % i) for i in range(NT)]
    X2t = [T(None, "X2_%d" % i) for i in range(NT)]
    X3t = [T(None, "X3_%d" % i) for i in range(NT)]

    P = Prog(nc, es)
    with es:
        if debug is not None:
            dbgt = P.sb("dbgt", [128, 256], F32)
        ident_f = P.sb("ident_f", [128, 128], F32)
        ident = P.sb("ident", [128, 128], BF16)
        ones_f = P.sb("ones_f", [128, 128], F32)
        mask01 = P.sb("mask01", [128, 128], F32)
        P.op("pool", ("memset", dict(ap=ones_f[:], constant=1.0)), w=[ones_f])
        P.op("pool", ("affine_select", dict(out=ident_f[:], in_=ones_f[:], pattern=[[1, 128]],
                                               compare_op=ALU.is_equal, fill=0.0, base=0,
                                               channel_multiplier=-1)), r=[ones_f], w=[ident_f])
        P.op("pool", ("affine_select", dict(out=mask01[:], in_=ones_f[:], pattern=[[1, 128]],
                                               compare_op=ALU.is_ge, fill=0.0, base=0,
                                               channel_multiplier=-1)), r=[ones_f], w=[mask01])
        P.op("dve", ("tensor_copy", dict(out=ident[:], in_=ident_f[:])), r=[ident_f], w=[ident])

        psb = [P.ps("psb%d" % i, [128, 512], F32) for i in range(8)]
        ps_rr = [0]

        def ps1():
            t = psb[ps_rr[0] % 4]
            ps_rr[0] += 1
            return t

        def norm_to_hT(xt, wb, hT_t, hT_ap, tmp, eng_copy="act"):
            junk, ssq, rstd, hb = tmp
            P.op("act", ("activation", dict(out=junk[:], in_=xt[:], func=AF.Square, accum_out=ssq[:])),
                 r=[xt], w=[junk, ssq])
            P.op("dve", ("tensor_scalar", dict(out=rstd[:], in0=ssq[:], scalar1=1.0 / D, scalar2=EPS,
                                                  op0=ALU.mult, op1=ALU.add)), r=[ssq], w=[rstd])
            P.op("act", ("activation", dict(out=rstd[:], in_=rstd[:], func=AF.Sqrt)), r=[rstd], w=[rstd])
            P.op("dve", ("reciprocal", dict(out=rstd[:], in_=rstd[:])), r=[rstd], w=[rstd])
            P.op("dve", ("scalar_tensor_tensor", dict(out=hb[:], in0=xt[:], scalar=rstd[:], in1=wb[:],
                                                         op0=ALU.mult, op1=ALU.mult)), r=[xt, rstd, wb], w=[hb])
            pt = ps1()
            ptv = pt[:].bitcast(BF16).rearrange("p (k t) -> p k t", k=8)

            def tr(e):
                ins = None
                for k in range(8):
                    ins = e.transpose(out=ptv[:, k, :], in_=hb[:, k * 128:(k + 1) * 128], identity=ident[:])
                return ins
            P.op("pe", tr, r=[hb, ident], w=[pt])
            if eng_copy == "act":
                P.op("act", ("copy", dict(out=hT_ap, in_=ptv)), r=[pt], w=[hT_t])
            else:
                P.op("dve", ("tensor_copy", dict(out=hT_ap, in_=ptv)), r=[pt], w=[hT_t])

        def phase1():
            with PhaseStack() as es1:
                def sb(name, shape, dt):
                    return T(es1.enter_context(nc.sbuf_tensor(name, list(shape), dt)), name)
                w_in = sb("hg_win", [128, 8, 4096], BF16)
                w_out = sb("hg_wout", [128, 8, 1024], BF16)
                for k in range(8):
                    P.dma("pool", w_in[:, k, :], IN("hg_w_in")[k * 128:(k + 1) * 128, :], w=[w_in])
                P.dma("pool", w_out[:], IN("hg_w_out").rearrange("(k p) n -> p k n", p=128), w=[w_out])
                wb = sb("p1_wb", [128, D], F32)
                P.dma("sp", wb[:], IN("mix_norm_w")[0].partition_broadcast(128), w=[wb])
                gw = sb("p1_gw", [128, 128], F32)
                P.dma("sp", gw[:], IN("hg_norm_w").partition_broadcast(128), w=[gw])
                lbl = sb("p1_lbl", [128, 2, 8], F32)
                P.dma("sp", lbl[:], IN("hg_lb_logits").rearrange("r (h p) -> p r h", p=128), w=[lbl],
                      allow_slow_non_contiguous=True)
                lb = sb("p1_lb", [128, 8], F32)
                oml = sb("p1_oml", [128, 8], F32)
                noml = sb("p1_noml", [128, 8], F32)
                P.op("dve", ("tensor_tensor", dict(out=lb[:], in0=lbl[:, 0, :], in1=lbl[:, 1, :], op=ALU.subtract)),
                     r=[lbl], w=[lb])
                P.op("act", ("activation", dict(out=lb[:], in_=lb[:], func=AF.Sigmoid)), r=[lb], w=[lb])
                P.op("dve", ("tensor_scalar", dict(out=oml[:], in0=lb[:], scalar1=-1.0, scalar2=1.0,
                                                      op0=ALU.mult, op1=ALU.add)), r=[lb], w=[oml])
                P.op("dve", ("tensor_scalar", dict(out=noml[:], in0=lb[:], scalar1=-1.0, scalar2=None,
                                                      op0=ALU.add)), r=[lb], w=[noml])

                xts = [sb("p1_xt%d" % i, [128, D], F32) for i in range(3)]
                junk = sb("p1_junk", [128, D], BF16)
                ssq = sb("p1_ssq", [128, 1], F32)
                rstd = sb("p1_rstd", [128, 1], F32)
                hb = sb("p1_hb", [128, D], BF16)
                hT = sb("p1_hT", [128, 8, 512], BF16)
                QT = sb("p1_QT", [128, 8, 512], BF16)
                KT = sb("p1_KT", [128, 8, 512], BF16)
                vblk = sb("p1_v", [128, 4, D], BF16)
                sgblk = sb("p1_sg", [128, 4, D], BF16)
                sig_ = [sb("p1_sig%d" % i, [128, 512], F32) for i in range(2)]
                kk_ = [sb("p1_kk%d" % i, [128, 512], F32) for i in range(2)]
                ff_ = [sb("p1_ff%d" % i, [128, 512], F32) for i in range(2)]
                bcum_ = [sb("p1_bcum%d" % i, [128, 512], F32) for i in range(2)]
                Ep_ = [sb("p1_Ep%d" % i, [128, 512], F32) for i in range(2)]
                Em_ = [sb("p1_Em%d" % i, [128, 512], F32) for i in range(2)]
                negm_ = [sb("p1_negm%d" % i, [128, 4], F32) for i in range(2)]
                qsb_ = [sb("p1_qsb%d" % i, [128, 512], F32) for i in range(2)]
                e2b = sb("p1_e2", [128, 4, 8], F32)
                e3b = sb("p1_e3", [128, 4, 8], F32)
                emb = sb("p1_em", [128, 4, 8], F32)
                S = sb("p1_S", [128, 8, 128], F32)
                S3 = sb("p1_S3", [128, 8, 128], F32)
                Sp_ = [sb("p1_Sp%d" % i, [128, 8, 128], BF16) for i in range(2)]
                Ktok_ = [sb("p1_Ktok%d" % i, [128, 8, 128], BF16) for i in range(2)]
                attn_ = [sb("p1_attn%d" % i, [128, 8, 128], BF16) for i in range(2)]
                oss = sb("p1_oss", [128, 8], F32)
                orstd = sb("p1_orstd", [128, 8], F32)
                on = sb("p1_on", [128, D], F32)
                osq = on
                ob_ = [sb("p1_ob%d" % i, [128, D], BF16) for i in range(2)]
                obT_ = [sb("p1_obT0", [128, 8, 128], BF16)] * 2
                x1 = [sb("p1_x1_0", [128, D], F32)] * 2
                P.op("pool", ("memset", dict(ap=S[:], constant=0.0)), w=[S])
                P.op("dve", ("memset", dict(ap=psb[4][:], constant=0.0)), w=[psb[4]])
                P.op("dve", ("memset", dict(ap=psb[5][:], constant=0.0)), w=[psb[5]])

                if debug == "s1":
                    raise StopBuild()

                def load_x(i):
                    P.dma("sp", xts[i % 3][:], IN("x")[i * 128:(i + 1) * 128, :], w=[xts[i % 3]])

                for i in range(min(2, NT)):
                    load_x(i)
                for b in range(NBLK):
                    for j in range(4):
                        i = b * 4 + j
                        if i + 2 < NT:
                            load_x(i + 2)
                        norm_to_hT(xts[i % 3], wb, hT, hT[:, :, j * 128:(j + 1) * 128], (junk, ssq, rstd, hb))
                    if debug == "s2":
                        raise StopBuild()
                    def vg_task(tix, b=b):
                        j, rem = divmod(tix, 4)
                        which, n = divmod(rem, 2)
                        pp = psb[6 + (tix % 2)]
                        col = 2048 + which * 1024 + n * 512
                        mm_group(P, pp, pp[:], [(hT[:, k, j * 128:(j + 1) * 128], w_in[:, k, col:col + 512])
                                                for k in range(8)], r=[w_in, hT])
                        if which == 0:
                            P.op("act", ("copy", dict(out=vblk[:, j, n * 512:(n + 1) * 512], in_=pp[:])), r=[pp], w=[vblk])
                        else:
                            P.op("act", ("activation", dict(out=sgblk[:, j, n * 512:(n + 1) * 512], in_=pp[:], func=AF.Sigmoid)),
                                 r=[pp], w=[sgblk])
                    for hd in range(8):
                        sig, kk, ff, bcum, Ep, Em, negm = (sig_[hd % 2], kk_[hd % 2], ff_[hd % 2], bcum_[hd % 2], Ep_[hd % 2],
                                                          Em_[hd % 2], negm_[hd % 2])
                        qp = ps1()
                        mm_group(P, qp, qp[:], [(w_in[:, k, hd * 128:(hd + 1) * 128], hT[:, k, :]) for k in range(8)],
                                 r=[w_in, hT])
                        qsb = qsb_[hd % 2]
                        P.op("act", ("copy", dict(out=qsb[:], in_=qp[:])), r=[qp], w=[qsb])
                        fp = ps1()
                        mm_group(P, fp, fp[:], [(w_in[:, k, 1024 + hd * 128:1024 + (hd + 1) * 128], hT[:, k, :])
                                                for k in range(8)], r=[w_in, hT])
                        P.op("act", ("activation", dict(out=sig[:], in_=fp[:], func=AF.Sigmoid)), r=[fp], w=[sig])
                        vg_task(2 * hd)
                        vg_task(2 * hd + 1)
                        P.op("dve", ("tensor_scalar", dict(out=kk[:], in0=sig[:], scalar1=noml[:, hd:hd + 1],
                                                                    scalar2=oml[:, hd:hd + 1], op0=ALU.mult, op1=ALU.add)),
                             r=[sig, noml, oml], w=[kk])
                        P.op("dve", ("tensor_scalar", dict(out=ff[:], in0=sig[:], scalar1=oml[:, hd:hd + 1],
                                                                    scalar2=lb[:, hd:hd + 1], op0=ALU.mult, op1=ALU.add)),
                             r=[sig, oml, lb], w=[ff])
                        P.op("act", ("activation", dict(out=ff[:], in_=ff[:], func=AF.Ln)), r=[ff], w=[ff])

                        def scans(e, bcum=bcum, ff=ff):
                            ins = None
                            for c in range(4):
                                ins = e.tensor_tensor_scan(out=bcum[:, c * 128:(c + 1) * 128], data0=ones_f[:],
                                                           data1=ff[:, c * 128:(c + 1) * 128], initial=0.0,
                                                           op0=ALU.mult, op1=ALU.add)
                            return ins
                        P.op("dve", scans, r=[ff, ones_f], w=[bcum])
                        bc3 = bcum[:].rearrange("p (c t) -> p c t", c=4)
                        P.op("dve", ("tensor_scalar", dict(out=negm[:], in0=bc3[:, :, 63], scalar1=-1.0,
                                                                      scalar2=None, op0=ALU.mult)), r=[bcum], w=[negm])

                        def exps(e, hd=hd, bc3=bc3, bcum=bcum, Ep=Ep, Em=Em, negm=negm):
                            for c in range(4):
                                e.activation(out=Ep[:, c * 128:(c + 1) * 128], in_=bcum[:, c * 128:(c + 1) * 128],
                                             func=AF.Exp, bias=negm[:, c:c + 1], scale=1.0)
                                e.activation(out=Em[:, c * 128:(c + 1) * 128], in_=bcum[:, c * 128:(c + 1) * 128],
                                             func=AF.Exp, bias=bcum[:, c * 128 + 63:c * 128 + 64], scale=-1.0)
                            e.activation(out=e3b[:, :, hd], in_=bc3[:, :, 127], func=AF.Exp)
                            return e.activation(out=emb[:, :, hd], in_=bc3[:, :, 63], func=AF.Exp)
                        P.op("act", exps, r=[bcum, negm], w=[Ep, Em, e3b, emb])
                        Ep3 = Ep[:].rearrange("p (c t) -> p c t", c=4)
                        P.op("dve", ("tensor_copy", dict(out=e2b[:, :, hd], in_=Ep3[:, :, 127])),
                             r=[Ep], w=[e2b])
                        P.op("dve", ("tensor_tensor", dict(out=QT[:, hd, :], in0=qsb[:], in1=Ep[:], op=ALU.mult)),
                             r=[qsb, Ep], w=[QT])
                        P.op("dve", ("tensor_tensor", dict(out=KT[:, hd, :], in0=kk[:], in1=Em[:], op=ALU.mult)),
                             r=[kk, Em], w=[KT])
                    if debug == "s4":
                        raise StopBuild()
                    pa = [psb[4], psb[5]]
                    po = [psb[6], psb[7]]

                    def c_front(c, b=b):
                        i = b * 4 + c
                        tok = slice(c * 128, (c + 1) * 128)
                        Ktok, attn, Sp, ob, obT = Ktok_[i % 2], attn_[i % 2], Sp_[i % 2], ob_[i % 2], obT_[i % 2]
                        pk = ps1()
                        pkv = pk[:].bitcast(BF16).rearrange("p (k t) -> p k t", k=8)

                        def trk(e, pkv=pkv, tok=tok):
                            ins = None
                            for hd in range(8):
                                ins = e.transpose(out=pkv[:, hd, :], in_=KT[:, hd, tok], identity=ident[:])
                            return ins
                        P.op("pe", trk, r=[KT, ident], w=[pk])
                        P.op("act", ("copy", dict(out=Ktok[:], in_=pkv)), r=[pk], w=[Ktok])
                        pa = [psb[4], psb[5]]
                        for half in range(2):
                            def att(e, half=half, tok=tok):
                                ins = None
                                for q4 in range(4):
                                    hd = half * 4 + q4
                                    t0 = tok.start
                                    e.matmul(pa[half][:, q4 * 128 + 64:(q4 + 1) * 128], lhsT=KT[:, hd, tok], rhs=QT[:, hd, t0 + 64:t0 + 128],
                                             start=True, stop=True)
                                    ins = e.matmul(pa[half][0:64, q4 * 128:q4 * 128 + 64], lhsT=KT[:, hd, t0:t0 + 64],
                                                   rhs=QT[:, hd, t0:t0 + 64], start=True, stop=True)
                                return ins
                            P.op("pe", att, r=[KT, QT], w=[pa[half]])
                            P.op("dve", ("tensor_tensor", dict(
                                out=attn[:, half * 4:(half + 1) * 4, :],
                                in0=pa[half][:].rearrange("p (h t) -> p h t", h=4),
                                in1=mask01[:].unsqueeze(1).broadcast_to([128, 4, 128]), op=ALU.mult)),
                                r=[pa[half], mask01], w=[attn])
                    def c_mid(c, b=b):
                        i = b * 4 + c
                        tok = slice(c * 128, (c + 1) * 128)
                        Ktok, attn, Sp, ob, obT = Ktok_[i % 2], attn_[i % 2], Sp_[i % 2], ob_[i % 2], obT_[i % 2]
                        P.op("dve", ("tensor_tensor", dict(out=Sp[:], in0=S[:],
                                                                  in1=emb[:, c, :].unsqueeze(2).broadcast_to([128, 8, 128]),
                                                                  op=ALU.mult)), r=[S, emb], w=[Sp])
                        po = [psb[6], psb[7]]
                        for half in range(2):
                            def omm(e, half=half, tok=tok, c=c, attn=attn, Sp=Sp):
                                ins = None
                                for q4 in range(4):
                                    hd = half * 4 + q4
                                    e.matmul(po[half][:, q4 * 128:(q4 + 1) * 128], lhsT=attn[:, hd, :],
                                             rhs=vblk[:, c, hd * 128:(hd + 1) * 128], start=True, stop=False)
                                    ins = e.matmul(po[half][:, q4 * 128:(q4 + 1) * 128], lhsT=QT[:, hd, tok],
                                                   rhs=Sp[:, hd, :], start=False, stop=True)
                                return ins
                            P.op("pe", omm, r=[attn, vblk, QT, Sp], w=[po[half]])
                        pp2 = [ps1(), ps1()]
                        for half in range(2):
                            def pmm(e, half=half, c=c, pp2=pp2, Ktok=Ktok):
                                ins = None
                                for q4 in range(4):
                                    hd = half * 4 + q4
                                    ins = e.matmul(pp2[half][:, q4 * 128:(q4 + 1) * 128], lhsT=Ktok[:, hd, :],
                                                   rhs=vblk[:, c, hd * 128:(hd + 1) * 128], start=True, stop=True)
                                return ins
                            P.op("pe", pmm, r=[Ktok, vblk], w=[pp2[half]])
                        P.op("dve", ("tensor_tensor", dict(out=S3[:], in0=S[:],
                                                                   in1=e3b[:, c, :].unsqueeze(2).broadcast_to([128, 8, 128]),
                                                                   op=ALU.mult)), r=[S, e3b], w=[S3])
                        for half in range(2):
                            P.op("dve", ("tensor_tensor", dict(
                                out=S[:, half * 4:(half + 1) * 4, :],
                                in0=pp2[half][:].rearrange("p (h v) -> p h v", h=4),
                                in1=e2b[:, c, half * 4:(half + 1) * 4].unsqueeze(2).broadcast_to([128, 4, 128]),
                                op=ALU.mult)), r=[pp2[half], e2b], w=[S])
                        P.op("dve", ("tensor_tensor", dict(out=S[:], in0=S[:], in1=S3[:], op=ALU.add)), r=[S, S3], w=[S])
                    def c_tail(c, b=b):
                        i = b * 4 + c
                        tok = slice(c * 128, (c + 1) * 128)
                        Ktok, attn, Sp, ob, obT = Ktok_[i % 2], attn_[i % 2], Sp_[i % 2], ob_[i % 2], obT_[i % 2]
                        for half in range(2):
                            P.op("act", ("activation", dict(out=osq[:, half * 512:(half + 1) * 512], in_=po[half][:],
                                                                         func=AF.Square)), r=[po[half]], w=[osq])
                        P.op("dve", ("tensor_reduce", dict(out=oss[:], in_=osq[:].rearrange("p (h v) -> p h v", h=8),
                                                              axis=AX.X, op=ALU.add)), r=[osq], w=[oss])
                        P.op("dve", ("tensor_scalar", dict(out=orstd[:], in0=oss[:], scalar1=1.0 / 128, scalar2=EPS,
                                                              op0=ALU.mult, op1=ALU.add)), r=[oss], w=[orstd])
                        P.op("act", ("activation", dict(out=orstd[:], in_=orstd[:], func=AF.Sqrt)), r=[orstd], w=[orstd])
                        P.op("dve", ("reciprocal", dict(out=orstd[:], in_=orstd[:])), r=[orstd], w=[orstd])
                        for half in range(2):
                            P.op("dve", ("tensor_tensor", dict(
                                out=on[:, half * 512:(half + 1) * 512].rearrange("p (h v) -> p h v", h=4),
                                in0=po[half][:].rearrange("p (h v) -> p h v", h=4),
                                in1=orstd[:, half * 4:(half + 1) * 4].unsqueeze(2).broadcast_to([128, 4, 128]),
                                op=ALU.mult)), r=[po[half], orstd], w=[on])
                        P.op("dve", ("tensor_tensor", dict(out=on[:].rearrange("p (h v) -> p h v", h=8),
                                                               in0=on[:].rearrange("p (h v) -> p h v", h=8),
                                                               in1=gw[:].unsqueeze(1).broadcast_to([128, 8, 128]),
                                                               op=ALU.mult)), r=[on, gw], w=[on])
                        P.op("dve", ("tensor_tensor", dict(out=ob[:], in0=on[:], in1=sgblk[:, c, :], op=ALU.mult)),
                             r=[on, sgblk], w=[ob])
                        pt = ps1()
                        ptv = pt[:].bitcast(BF16).rearrange("p (k t) -> p k t", k=8)

                        def tro(e, ptv=ptv, ob=ob):
                            ins = None
                            for hd in range(8):
                                ins = e.transpose(out=ptv[:, hd, :], in_=ob[:, hd * 128:(hd + 1) * 128], identity=ident[:])
                            return ins
                        P.op("pe", tro, r=[ob, ident], w=[pt])
                        P.op("act", ("copy", dict(out=obT[:], in_=ptv)), r=[pt], w=[obT])
                        xo = x1[i % 2]
                        P.dma("sp", xo[:], IN("x")[i * 128:(i + 1) * 128, :], w=[xo])
                        for n in range(2):
                            py = ps1()
                            mm_group(P, py, py[:], [(obT[:, hd, :], w_out[:, hd, n * 512:(n + 1) * 512]) for hd in range(8)],
                                     r=[obT, w_out])
                            P.op("dve", ("tensor_tensor", dict(
                                out=xo[:, n * 512:(n + 1) * 512], in0=py[:], in1=xo[:, n * 512:(n + 1) * 512], op=ALU.add)),
                                r=[py, xo], w=[xo])
                        P.dma("sp", X1[i * 128:(i + 1) * 128, :], xo[:], r=[xo], w=[X1t[i]])

                    c_front(0)
                    for c in range(4):
                        c_mid(c)
                        if c + 1 < 4:
                            c_front(c + 1)
                        c_tail(c)

        def ffn_phase(tag, src_tiles, norm_w_row, experts, moe, dst_fn, final):
            NTT = len(src_tiles)
            P.barrier()
            PASS = min(16, NTT)
            with PhaseStack() as es2:
                def sb(name, shape, dt):
                    return T(es2.enter_context(nc.sbuf_tensor(tag + name, list(shape), dt)), name)
                wb = sb("wb", [128, D], F32)
                P.dma("sp", wb[:], norm_w_row.partition_broadcast(128), w=[wb])
                acc = [sb("acc%d" % i, [128, D], F32) for i in range(PASS)]
                hT = sb("hT", [128, 8, PASS * 128], BF16)
                junk = sb("junk", [128, D], BF16)
                ssq = sb("ssq", [128, 1], F32)
                rstd = sb("rstd", [128, 1], F32)
                hb = sb("hb", [128, D], BF16)
                wgs = [sb("wg%d" % i, [128, 8, 256], BF16) for i in range(2)]
                wus = [sb("wu%d" % i, [128, 8, 256], BF16) for i in range(2)]
                wds = [sb("wd%d" % i, [128, 2, D], BF16) for i in range(2)]
                sgl = [sb("sgl%d" % i, [128, 512], F32) for i in range(2)]
                Ab = [sb("A%d" % i, [128, 2, 512], BF16) for i in range(2)]
                if moe:
                    sel = sb("sel", [128, 2], F32)
                    P.dma("sp", sel[:], IN("sel").partition_broadcast(128), w=[sel])
                    xb = sb("xb", [128, D], F32)
                    wr = sb("wr", [128, 8, NE], F32)
                    P.dma("sp", wr[:], IN("moe_router").rearrange("(k p) n -> p k n", p=128), w=[wr])
                    hf = sb("hf", [128, D], F32)
                    hT32 = sb("hT32", [128, 8, 128], F32)
                    gates = sb("gates", [128, PASS, NE], F32)
                    lg = sb("lg", [128, NE], F32)
                    mx8 = sb("mx8", [128, 8], F32)
                    pe_ = sb("pexp", [128, NE], F32)
                    msk = sb("msk", [128, NE], F32)
                    den = sb("den", [128, 1], F32)
                if final:
                    fwb = sb("fwb", [128, D], F32)
                    P.dma("sp", fwb[:], IN("final_norm_w").partition_broadcast(128), w=[fwb])
                    ot = [sb("ot%d" % i, [128, D], F32) for i in range(2)]
                wcount = [0]
                for p0 in range(0, NTT, PASS):
                    npt = min(PASS, NTT - p0)
                    ntb = npt // 4
                    for j in range(npt):
                        i = p0 + j
                        srcs = src_tiles[i]
                        if not moe:
                            ap, tt = srcs[0]
                            P.dma("sp", acc[j][:], ap, r=[tt], w=[acc[j]])
                        else:
                            (apa, ta), (apb, tb_) = srcs
                            P.dma("sp", acc[j][:], apa, r=[ta], w=[acc[j]])
                            P.dma("sp", xb[:], apb, r=[tb_], w=[xb])
                            P.op("dve", ("tensor_scalar", dict(out=acc[j][:], in0=acc[j][:], scalar1=sel[:, 0:1],
                                                                      scalar2=None, op0=ALU.mult)), r=[acc[j], sel], w=[acc[j]])
                            P.op("dve", ("scalar_tensor_tensor", dict(out=acc[j][:], in0=xb[:], scalar=sel[:, 1:2],
                                                                             in1=acc[j][:], op0=ALU.mult, op1=ALU.add)),
                                 r=[xb, sel, acc[j]], w=[acc[j]])
                        norm_to_hT(acc[j], wb, hT, hT[:, :, j * 128:(j + 1) * 128], (junk, ssq, rstd, hb))
                        if moe:
                            P.op("dve", ("scalar_tensor_tensor", dict(out=hf[:], in0=acc[j][:], scalar=rstd[:], in1=wb[:],
                                                                      op0=ALU.mult, op1=ALU.mult)), r=[acc[j], rstd, wb], w=[hf])
                            for hh in range(2):
                                ptr = psb[4 + hh]

                                def trf(e, ptr=ptr, hh=hh):
                                    ins = None
                                    for k4 in range(4):
                                        k = hh * 4 + k4
                                        ins = e.transpose(out=ptr[:, k4 * 128:(k4 + 1) * 128], in_=hf[:, k * 128:(k + 1) * 128],
                                                          identity=ident_f[:])
                                    return ins
                                P.op("pe", trf, r=[hf, ident_f], w=[ptr])
                                P.op("act", ("copy", dict(out=hT32[:, hh * 4:(hh + 1) * 4, :],
                                                          in_=ptr[:].rearrange("p (k t) -> p k t", k=4))), r=[ptr], w=[hT32])
                            pl = ps1()
                            mm_group(P, pl, pl[:, 0:NE], [(hT32[:, k, :], wr[:, k, :]) for k in range(8)], r=[hT32, wr])
                            P.op("act", ("copy", dict(out=lg[:], in_=pl[:, 0:NE])), r=[pl], w=[lg])
                            P.op("dve", ("max", dict(out=mx8[:], in_=lg[:])), r=[lg], w=[mx8])
                            P.op("dve", ("tensor_scalar", dict(out=msk[:], in0=lg[:], scalar1=mx8[:, 1:2], scalar2=None,
                                                                  op0=ALU.is_ge)), r=[lg, mx8], w=[msk])
                            P.op("dve", ("tensor_scalar", dict(out=pe_[:], in0=lg[:], scalar1=mx8[:, 0:1], scalar2=None,
                                                                  op0=ALU.subtract)), r=[lg, mx8], w=[pe_])
                            P.op("act", ("activation", dict(out=pe_[:], in_=pe_[:], func=AF.Exp)), r=[pe_], w=[pe_])
                            P.op("dve", ("tensor_tensor", dict(out=pe_[:], in0=pe_[:], in1=msk[:], op=ALU.mult)),
                                 r=[pe_, msk], w=[pe_])
                            P.op("dve", ("tensor_reduce", dict(out=den[:], in_=pe_[:], axis=AX.X, op=ALU.add)),
                                 r=[pe_], w=[den])
                            P.op("dve", ("reciprocal", dict(out=den[:], in_=den[:])), r=[den], w=[den])
                            P.op("dve", ("tensor_scalar", dict(out=gates[:, j, :], in0=pe_[:], scalar1=den[:, 0:1],
                                                                      scalar2=None, op0=ALU.mult)), r=[pe_, den], w=[gates])
                    if debug == "f1":
                        raise StopBuild()
                    for ei, (wg_ap, wu_ap, wd_ap) in enumerate(experts):
                        for fb in range(NFC // 2):
                            if debug in ("f2", "f3", "f6") and fb == {"f2": 1, "f3": 3, "f6": 6}[debug]:
                                raise StopBuild()
                            wi = wcount[0] % 2
                            wcount[0] += 1
                            wg, wu, wd = wgs[wi], wus[wi], wds[wi]
                            f0 = fb * 256
                            P.dma("pool", wg[:], wg_ap[:, f0:f0 + 256].rearrange("(k p) n -> p k n", p=128), w=[wg])
                            P.dma("pool", wu[:], wu_ap[:, f0:f0 + 256].rearrange("(k p) n -> p k n", p=128), w=[wu])
                            P.dma("pool", wd[:], wd_ap[f0:f0 + 256, :].rearrange("(c p) n -> p c n", p=128), w=[wd])
                            for tb in range(ntb):
                                tk = slice(tb * 512, (tb + 1) * 512)
                                A = Ab[(wcount[0] * 4 + tb) % 2]
                                for cc in range(2):
                                    pg = ps1()
                                    mm_group(P, pg, pg[:], [(wg[:, k, cc * 128:(cc + 1) * 128], hT[:, k, tk]) for k in range(8)],
                                             r=[wg, hT])
                                    pu = ps1()
                                    mm_group(P, pu, pu[:], [(wu[:, k, cc * 128:(cc + 1) * 128], hT[:, k, tk]) for k in range(8)],
                                             r=[wu, hT])
                                    sg = sgl[cc]
                                    P.op("act", ("activation", dict(out=sg[:], in_=pg[:], func=AF.Silu)),
                                         r=[pg], w=[sg])
                                    P.op("dve", ("tensor_tensor", dict(out=A[:, cc, :], in0=pu[:], in1=sg[:],
                                                                                                     op=ALU.mult)),
                                         r=[pu, sg], w=[A])
                                for t4 in range(4):
                                    j = tb * 4 + t4
                                    for n in range(2):
                                        pd = psb[4 + (t4 * 2 + n) % 4]
                                        mm_group(P, pd, pd[:], [(A[:, cc, t4 * 128:(t4 + 1) * 128], wd[:, cc, n * 512:(n + 1) * 512])
                                                                for cc in range(2)], r=[A, wd])
                                        if moe:
                                            P.op("dve", ("scalar_tensor_tensor", dict(
                                                out=acc[j][:, n * 512:(n + 1) * 512], in0=pd[:], scalar=gates[:, j, ei:ei + 1],
                                                in1=acc[j][:, n * 512:(n + 1) * 512], op0=ALU.mult, op1=ALU.add)),
                                                r=[pd, gates, acc[j]], w=[acc[j]])
                                        else:
                                            P.op("dve", ("tensor_tensor", dict(
                                                out=acc[j][:, n * 512:(n + 1) * 512], in0=pd[:],
                                                in1=acc[j][:, n * 512:(n + 1) * 512], op=ALU.add)),
                                                r=[pd, acc[j]], w=[acc[j]])
                    for j in range(npt):
                        i = p0 + j
                        dap, dt_ = dst_fn(i)
                        if final:
                            o = ot[j % 2]
                            P.op("act", ("activation", dict(out=junk[:], in_=acc[j][:], func=AF.Square, accum_out=ssq[:])),
                                 r=[acc[j]], w=[junk, ssq])
                            P.op("dve", ("tensor_scalar", dict(out=rstd[:], in0=ssq[:], scalar1=1.0 / D, scalar2=EPS,
                                                                  op0=ALU.mult, op1=ALU.add)), r=[ssq], w=[rstd])
                            P.op("act", ("activation", dict(out=rstd[:], in_=rstd[:], func=AF.Sqrt)), r=[rstd], w=[rstd])
                            P.op("dve", ("reciprocal", dict(out=rstd[:], in_=rstd[:])), r=[rstd], w=[rstd])
                            P.op("dve", ("scalar_tensor_tensor", dict(out=o[:], in0=acc[j][:], scalar=rstd[:], in1=fwb[:],
                                                                                  op0=ALU.mult, op1=ALU.mult)),
                                 r=[acc[j], rstd, fwb], w=[o])
                            P.dma("sp", dap, o[:], r=[o], w=[dt_])
                        else:
                            P.dma("sp", dap, acc[j][:], r=[acc[j]], w=[dt_])

        ZS = nc.dram_tensor("ZS", [L, DIN], BF16, kind="Internal").ap()
        XT = nc.dram_tensor("XT", [L, DIN], BF16, kind="Internal").ap()
        BTK = nc.dram_tensor("BTK", [L, 512], BF16, kind="Internal").ap()
        BCD = nc.dram_tensor("BCD", [NT, 128, 1024], BF16, kind="Internal").ap()
        SMD = nc.dram_tensor("SMD", [L, 128], F32, kind="Internal").ap()
        CSD = nc.dram_tensor("CSD", [NT, SSD_H * 128], F32, kind="Internal").ap()
        CBD = nc.dram_tensor("CBD", [NT, 128, 512], F32, kind="Internal").ap()
        XDT = nc.dram_tensor("XDT", [L, DIN], BF16, kind="Internal").ap()
        XWD = nc.dram_tensor("XWD", [L, DIN], BF16, kind="Internal").ap()
        MD = nc.dram_tensor("MD", [NT, 128, SSD_H * 128], BF16, kind="Internal").ap()
        XDTt = [T(None, "XDT_%d" % i) for i in range(NT)]
        XWDt = [T(None, "XWD_%d" % i) for i in range(NT)]
        MDt = [T(None, "MD_%d" % i) for i in range(NT)]
        ZSt = [T(None, "ZS_%d" % i) for i in range(NT)]
        XTt = [T(None, "XT_%d" % i) for i in range(NT)]
        BTKt = [T(None, "BTK_%d" % i) for i in range(NT)]
        BCDt = [T(None, "BCD_%d" % i) for i in range(NT)]
        SMDt = [T(None, "SMD_%d" % i) for i in range(NT)]
        CSDt = [T(None, "CSD_%d" % i) for i in range(NT)]
        CBDt = [T(None, "CBD_%d" % i) for i in range(NT)]

        def phase3b():
            P.barrier()
            with PhaseStack() as es3:
                def sb(name, shape, dt):
                    return T(es3.enter_context(nc.sbuf_tensor("pb_" + name, list(shape), dt)), name)
                w_in = sb("win", [128, 8, SSD_IN], BF16)
                for k in range(8):
                    P.dma("pool", w_in[:, k, :], IN("ssd_w_in")[k * 128:(k + 1) * 128, :], w=[w_in])
                wb = sb("wb", [128, D], F32)
                P.dma("sp", wb[:], IN("mix_norm_w")[1].partition_broadcast(128), w=[wb])
                dtb = sb("dtb", [128, SSD_H], F32)
                P.dma("sp", dtb[:], IN("ssd_dt_bias").partition_broadcast(128), w=[dtb])
                ab = sb("ab", [128, SSD_H], F32)
                P.dma("sp", ab[:], IN("ssd_a_log").partition_broadcast(128), w=[ab])
                P.op("act", ("activation", dict(out=ab[:], in_=ab[:], func=AF.Exp)), r=[ab], w=[ab])
                P.op("dve", ("tensor_scalar", dict(out=ab[:], in0=ab[:], scalar1=-1.0, scalar2=None, op0=ALU.mult)), r=[ab], w=[ab])
                cwr = sb("cwr", [120, 128], F32)
                P.dma("sp", cwr[0:96, :], IN("ssd_conv_w").rearrange("j (c p) -> (j c) p", p=128), w=[cwr])
                P.dma("sp", cwr[96:120, :], IN("ssd_conv_b").rearrange("(c p) -> c p", p=128), w=[cwr])
                cw = sb("cw", [128, 120], F32)
                pc = ps1()
                P.op("pe", ("transpose", dict(out=pc[:, 0:120], in_=cwr[:], identity=ident_f[0:120, 0:120])), r=[cwr, ident_f], w=[pc])
                P.op("act", ("copy", dict(out=cw[:], in_=pc[:, 0:120])), r=[pc], w=[cw])
                diag = sb("diag", [128, 24, 4, 128], BF16)

                def mkdiag(e):
                    ins = None
                    for c in range(24):
                        for j in range(4):
                            ins = e.tensor_scalar(out=diag[:, c, j, :], in0=ident_f[:], scalar1=cw[:, j * 24 + c:j * 24 + c + 1],
                                                  scalar2=None, op0=ALU.mult)
                    return ins
                P.op("dve", mkdiag, r=[cw, ident_f], w=[diag])

                xts = [sb("xt%d" % i, [128, D], F32) for i in range(2)]
                junk = sb("junk", [128, D], BF16)
                ssq = sb("ssq", [128, 1], F32)
                rstd = sb("rstd", [128, 1], F32)
                hb = sb("hb", [128, D], BF16)
                hT = sb("hT", [128, 8, 512], BF16)
                szs = [sb("sz%d" % i, [128, DIN], BF16) for i in range(2)]
                xbc = sb("xbc", [128, 24, 515], BF16)
                xcT = sb("xcT", [128, 16, 512], BF16)
                BCT = sb("BCT", [128, 8, 512], BF16)
                bcts = [sb("bct0", [128, 8, 128], BF16)] * 2
                xtoks = [sb("xtok%d" % i, [128, DIN], BF16) for i in range(2)]
                btks = [sb("btk%d" % i, [128, 4, 128], BF16) for i in range(2)]
                sms = [sb("sm%d" % i, [128, 128], F32) for i in range(2)]
                da = sb("da", [128, SSD_H], F32)
                csTs = [sb("csT%d" % i, [32, 128], F32) for i in range(2)]
                cbms = [sb("cbm%d" % i, [128, 4, 128], F32) for i in range(2)]
                P.op("pool", ("memset", dict(ap=xbc[:], constant=0.0)), w=[xbc])
                for i in range(2):
                    P.op("pool", ("memset", dict(ap=sms[i][:], constant=0.0)), w=[sms[i]])

                for b in range(NBLK):
                    if b > 0:
                        P.op("pool", ("tensor_copy", dict(out=xbc[:, :, 0:3], in_=xbc[:, :, 512:515])), r=[xbc], w=[xbc])
                    for j in range(4):
                        i = b * 4 + j
                        xt = xts[i % 2]
                        P.dma("sp", xt[:], X2[i * 128:(i + 1) * 128, :], r=[X2t[i]], w=[xt])
                        norm_to_hT(xt, wb, hT, hT[:, :, j * 128:(j + 1) * 128], (junk, ssq, rstd, hb))
                    for j in range(4):
                        i = b * 4 + j
                        sz = szs[i % 2]
                        for n in range(4):
                            pz = ps1()
                            mm_group(P, pz, pz[:], [(hT[:, k, j * 128:(j + 1) * 128], w_in[:, k, n * 512:(n + 1) * 512]) for k in range(8)],
                                     r=[hT, w_in])
                            P.op("act", ("activation", dict(out=sz[:, n * 512:(n + 1) * 512], in_=pz[:], func=AF.Silu)), r=[pz], w=[sz])
                        P.dma("sp", ZS[i * 128:(i + 1) * 128, :], sz[:], r=[sz], w=[ZSt[i]])
                    for c in range(24):
                        pp = ps1()
                        mm_group(P, pp, pp[:], [(w_in[:, k, DIN + c * 128:DIN + (c + 1) * 128], hT[:, k, :]) for k in range(8)], r=[w_in, hT])
                        if c % 2 == 0:
                            P.op("act", ("copy", dict(out=xbc[:, c, 3:515], in_=pp[:])), r=[pp], w=[xbc])
                        else:
                            P.op("dve", ("tensor_copy", dict(out=xbc[:, c, 3:515], in_=pp[:])), r=[pp], w=[xbc])
                    for j in range(4):
                        i = b * 4 + j
                        sm = sms[i % 2]
                        csT = csTs[i % 2]
                        pd = ps1()
                        mm_group(P, pd, pd[:, 0:SSD_H], [(hT[:, k, j * 128:(j + 1) * 128], w_in[:, k, DIN + CONV:SSD_IN]) for k in range(8)],
                                 r=[hT, w_in])
                        P.op("dve", ("tensor_tensor", dict(out=sm[:, 0:32], in0=pd[:, 0:SSD_H], in1=dtb[:], op=ALU.add)), r=[pd, dtb], w=[sm])
                        P.op("act", ("activation", dict(out=sm[:, 0:32], in_=sm[:, 0:32], func=AF.Exp)), r=[sm], w=[sm])
                        P.op("act", ("activation", dict(out=sm[:, 0:32], in_=sm[:, 0:32], func=AF.Ln, bias=1.0, scale=1.0)), r=[sm], w=[sm])
                        P.op("dve", ("tensor_tensor", dict(out=da[:], in0=sm[:, 0:32], in1=ab[:], op=ALU.mult)), r=[sm, ab], w=[da])
                        pcs = ps1()
                        P.op("pe", ("matmul", dict(out=pcs[:, 0:SSD_H], lhsT=mask01[:], rhs=da[:], start=True, stop=True)),
                             r=[mask01, da], w=[pcs])
                        P.op("act", ("copy", dict(out=sm[:, 32:64], in_=pcs[:, 0:SSD_H])), r=[pcs], w=[sm])
                        P.op("act", ("activation", dict(out=sm[:, 64:96], in_=pcs[:, 0:SSD_H], func=AF.Exp)), r=[pcs], w=[sm])
                        pct = ps1()
                        P.op("pe", ("matmul", dict(out=pct[0:32, 0:128], lhsT=da[:], rhs=mask01[:], start=True, stop=True)),
                             r=[mask01, da], w=[pct])
                        P.op("act", ("copy", dict(out=csT[:], in_=pct[0:32, 0:128])), r=[pct], w=[csT])
                        P.dma("sp", CSD[i].rearrange("(h t) -> h t", t=128), csT[:], r=[csT], w=[CSDt[i]])
                        P.dma("sp", SMD[i * 128:(i + 1) * 128, :], sm[:], r=[sm], w=[SMDt[i]])
                    for c in range(24):
                        pp = ps1()
                        mm_group(P, pp, pp[:], [(diag[:, c, jj, :], xbc[:, c, jj:jj + 512]) for jj in range(4)], r=[diag, xbc])
                        dst_t = xcT if c < 16 else BCT
                        dst = xcT[:, c, :] if c < 16 else BCT[:, c - 16, :]
                        P.op("act", ("activation", dict(out=dst, in_=pp[:], func=AF.Silu, bias=cw[:, 96 + c:97 + c], scale=1.0)),
                             r=[pp, cw], w=[dst_t])
                    for j in range(4):
                        i = b * 4 + j
                        tk = slice(j * 128, (j + 1) * 128)
                        xtok = xtoks[i % 2]
                        btk = btks[i % 2]
                        bct = bcts[i % 2]
                        cbm = cbms[i % 2]
                        for q4 in range(2):
                            pt = ps1()
                            ptv = pt[:].bitcast(BF16).rearrange("p (k t) -> p k t", k=8)

                            def trx(e, ptv=ptv, q4=q4, tk=tk):
                                ins = None
                                for k in range(8):
                                    ins = e.transpose(out=ptv[:, k, :], in_=xcT[:, q4 * 8 + k, tk], identity=ident[:])
                                return ins
                            P.op("pe", trx, r=[xcT, ident], w=[pt])
                            if q4 == 0:
                                P.op("act", ("copy", dict(out=xtok[:, 0:1024].rearrange("p (k t) -> p k t", k=8), in_=ptv)), r=[pt], w=[xtok])
                            else:
                                P.op("dve", ("tensor_copy", dict(out=xtok[:, 1024:2048].rearrange("p (k t) -> p k t", k=8), in_=ptv)),
                                     r=[pt], w=[xtok])
                        pt = ps1()
                        ptv = pt[:].bitcast(BF16).rearrange("p (k t) -> p k t", k=8)

                        def trb(e, ptv=ptv, tk=tk):
                            ins = None
                            for g in range(4):
                                ins = e.transpose(out=ptv[:, g, :], in_=BCT[:, g, tk], identity=ident[:])
                            return ins
                        P.op("pe", trb, r=[BCT, ident], w=[pt])
                        P.op("act", ("copy", dict(out=btk[:], in_=ptv[:, 0:4, :])), r=[pt], w=[btk])
                        pcb = ps1()

                        def cbmm(e, pcb=pcb, tk=tk):
                            ins = None
                            for g in range(4):
                                ins = e.matmul(pcb[:, g * 128:(g + 1) * 128], lhsT=BCT[:, g, tk], rhs=BCT[:, 4 + g, tk], start=True, stop=True)
                            return ins
                        P.op("pe", cbmm, r=[BCT], w=[pcb])
                        P.op("dve", ("tensor_tensor", dict(out=cbm[:], in0=pcb[:].rearrange("p (g t) -> p g t", g=4),
                                                           in1=mask01[:].unsqueeze(1).broadcast_to([128, 4, 128]), op=ALU.mult)),
                             r=[pcb, mask01], w=[cbm])
                        P.dma("sp", XT[i * 128:(i + 1) * 128, :], xtok[:], r=[xtok], w=[XTt[i]])
                        P.dma("sp", BTK[i * 128:(i + 1) * 128, :], btk[:].rearrange("p g n -> p (g n)"), r=[btk], w=[BTKt[i]])
                        P.dma("sp", BCD[i].rearrange("p (g t) -> p g t", g=8), BCT[:, :, tk], r=[BCT], w=[BCDt[i]])
                        P.dma("sp", CBD[i], cbm[:].rearrange("p g t -> p (g t)"), r=[cbm], w=[CBDt[i]])

        def phase3m():
            P.barrier()
            with PhaseStack() as es3:
                def sb(name, shape, dt):
                    return T(es3.enter_context(nc.sbuf_tensor("pm_" + name, list(shape), dt)), name)
                NB3 = 3
                csBs = [sb("csB%d" % i, [128, SSD_H, 128], F32) for i in range(NB3)]
                sms = [sb("sm%d" % i, [128, 128], F32) for i in range(NB3)]
                cbms = [sb("cbm%d" % i, [128, 4, 128], F32) for i in range(NB3)]
                xtoks = [sb("xtok%d" % i, [128, DIN], BF16) for i in range(NB3)]
                xdts = [sb("xdt%d" % i, [128, DIN], BF16) for i in range(2)]
                xws = [sb("xw%d" % i, [128, DIN], BF16) for i in range(2)]
                wvs = [sb("wv%d" % i, [128, SSD_H], F32) for i in range(2)]
                LTs = [sb("LT%d" % i, [128, 8, 128], BF16) for i in range(2)]
                Mas = [sb("Ma%d" % i, [128, SSD_H, 128], BF16) for i in range(2)]

                def loads(i):
                    P.dma("sp", csBs[i % NB3][:].rearrange("p h t -> p (h t)"), CSD[i].partition_broadcast(128), r=[CSDt[i]],
                          w=[csBs[i % NB3]])
                    P.dma("sp", sms[i % NB3][:], SMD[i * 128:(i + 1) * 128, :], r=[SMDt[i]], w=[sms[i % NB3]])
                    P.dma("sp", cbms[i % NB3][:].rearrange("p g t -> p (g t)"), CBD[i], r=[CBDt[i]], w=[cbms[i % NB3]])
                    P.dma("sp", xtoks[i % NB3][:], XT[i * 128:(i + 1) * 128, :], r=[XTt[i]], w=[xtoks[i % NB3]])

                loads(0)
                if NT > 1:
                    loads(1)
                for i in range(NT):
                    if i + 2 < NT:
                        loads(i + 2)
                    csB, sm, cbm, xtok = csBs[i % NB3], sms[i % NB3], cbms[i % NB3], xtoks[i % NB3]
                    xdt, xw, wv, Ma = xdts[i % 2], xws[i % 2], wvs[i % 2], Mas[i % 2]
                    dt = sm[:, 0:32]
                    cs = sm[:, 32:64]
                    P.op("dve", ("tensor_tensor", dict(out=xdt[:].rearrange("p (h q) -> p h q", h=SSD_H),
                                                       in0=xtok[:].rearrange("p (h q) -> p h q", h=SSD_H),
                                                       in1=dt.unsqueeze(2).broadcast_to([128, SSD_H, 64]), op=ALU.mult)),
                         r=[xtok, sm], w=[xdt])
                    P.dma("act", XDT[i * 128:(i + 1) * 128, :], xdt[:], r=[xdt], w=[XDTt[i]])
                    P.op("dve", ("tensor_tensor", dict(out=wv[:], in0=csB[:, :, 127], in1=cs, op=ALU.subtract)), r=[csB, sm], w=[wv])
                    P.op("act", ("activation", dict(out=wv[:], in_=wv[:], func=AF.Exp)), r=[wv], w=[wv])
                    P.op("act", ("activation", dict(out=sm[:, 96:128], in_=csB[:, :, 127], func=AF.Exp)), r=[csB], w=[sm])
                    P.op("dve", ("tensor_tensor", dict(out=wv[:], in0=wv[:], in1=dt, op=ALU.mult)), r=[wv, sm], w=[wv])
                    P.op("pool", ("tensor_tensor", dict(out=xw[:].rearrange("p (h q) -> p h q", h=SSD_H),
                                                        in0=xtok[:].rearrange("p (h q) -> p h q", h=SSD_H),
                                                        in1=wv[:].unsqueeze(2).broadcast_to([128, SSD_H, 64]), op=ALU.mult)),
                         r=[xtok, wv], w=[xw])
                    P.dma("act", XWD[i * 128:(i + 1) * 128, :], xw[:], r=[xw], w=[XWDt[i]])
                    P.dma("act", SMD[i * 128:(i + 1) * 128, :], sm[:], r=[sm], w=[SMDt[i]])
                    for g in range(4):
                        LT = LTs[g % 2]
                        hs = slice(g * 8, (g + 1) * 8)

                        def dmin(e, g=g, csB=csB, cs=cs):
                            ins = None
                            for r8 in range(8):
                                h = g * 8 + r8
                                ins = e.tensor_scalar(out=csB[:, h, :], in0=csB[:, h, :], scalar1=cs[:, h:h + 1], scalar2=0.0,
                                                      op0=ALU.subtract, op1=ALU.min)
                            return ins
                        P.op("dve", dmin, r=[csB, sm, wv], w=[csB])
                        P.op("act", ("activation", dict(out=LT[:], in_=csB[:, hs, :], func=AF.Exp)), r=[csB], w=[LT])
                        P.op("dve", ("tensor_tensor", dict(out=Ma[:, hs, :], in0=LT[:],
                                                           in1=cbm[:, g, :].unsqueeze(1).broadcast_to([128, 8, 128]), op=ALU.mult)),
                             r=[LT, cbm], w=[Ma])
                    P.dma("act", MD[i], Ma[:].rearrange("p h t -> p (h t)"), r=[Ma], w=[MDt[i]])

        def phase3():
            P.barrier()
            with PhaseStack() as es3:
                def sb(name, shape, dt):
                    return T(es3.enter_context(nc.sbuf_tensor("p3_" + name, list(shape), dt)), name)
                w_out = sb("wout", [128, 16, D], BF16)
                for k in range(4):
                    P.dma("pool", w_out[:, k * 4:(k + 1) * 4, :],
                          IN("ssd_w_out")[k * 512:(k + 1) * 512, :].rearrange("(k p) n -> p k n", p=128), w=[w_out])
                nwb = sb("nwb", [128, DIN], BF16)
                P.dma("pool", nwb[:], IN("ssd_norm_w").partition_broadcast(128), w=[nwb])
                dsk = sb("dsk", [128, SSD_H], F32)
                P.dma("sp", dsk[:], IN("ssd_d").partition_broadcast(128), w=[dsk])
                Did = sb("Did", [128, SSD_H, 128], BF16)

                def mkdid(e):
                    ins = None
                    for h in range(SSD_H):
                        ins = e.tensor_scalar(out=Did[:, h, :], in0=ident_f[:], scalar1=dsk[:, h:h + 1], scalar2=None, op0=ALU.mult)
                    return ins
                P.op("dve", mkdid, r=[dsk, ident_f], w=[Did])
                NBUF = 3
                xt_ = [sb("xt%d" % i, [128, D], F32) for i in range(NBUF)]
                szs = [sb("sz%d" % i, [128, DIN], BF16) for i in range(NBUF)]
                xtoks = [sb("xtok%d" % i, [128, DIN], BF16) for i in range(NBUF)]
                bcts = [sb("bct%d" % i, [128, 8, 128], BF16) for i in range(NBUF)]
                btks = [sb("btk%d" % i, [128, 4, 128], BF16) for i in range(NBUF)]
                sms = [sb("sm%d" % i, [128, 128], F32) for i in range(NBUF)]
                Mts = [sb("Mt%d" % i, [128, SSD_H, 128], BF16) for i in range(NBUF)]
                xdts = [sb("xdt%d" % i, [128, DIN], BF16) for i in range(NBUF)]
                xws = [sb("xw%d" % i, [128, DIN], BF16) for i in range(NBUF)]
                t1 = [sb("t1_%d" % i, [128, 512], F32) for i in range(2)]
                t2 = [sb("t2_%d" % i, [128, 512], F32) for i in range(2)]
                gss = sb("gss", [128, 4], F32)
                grs = sb("grs", [128, 4], F32)
                ybs = [sb("yb%d" % i, [128, DIN], BF16) for i in range(2)]
                ybT = sb("ybT", [128, 16, 128], BF16)
                ST = sb("ST", [128, DIN], F32)
                STb = sb("STb", [128, DIN], BF16)
                P.op("pool", ("memset", dict(ap=ST[:], constant=0.0)), w=[ST])
                P.op("pool", ("memset", dict(ap=STb[:], constant=0.0)), w=[STb])

                def loads(i):
                    q = "act" if i % 2 == 0 else "sp"
                    P.dma("sp", xt_[i % NBUF][:], X2[i * 128:(i + 1) * 128, :], r=[X2t[i]], w=[xt_[i % NBUF]])
                    P.dma("sp", szs[i % NBUF][:], ZS[i * 128:(i + 1) * 128, :], r=[ZSt[i]], w=[szs[i % NBUF]])
                    P.dma("sp", xtoks[i % NBUF][:], XT[i * 128:(i + 1) * 128, :], r=[XTt[i]], w=[xtoks[i % NBUF]])
                    P.dma("sp", bcts[i % NBUF][:].rearrange("p g t -> p (g t)"), BCD[i], r=[BCDt[i]], w=[bcts[i % NBUF]])
                    P.dma("sp", btks[i % NBUF][:].rearrange("p g n -> p (g n)"), BTK[i * 128:(i + 1) * 128, :], r=[BTKt[i]], w=[btks[i % NBUF]])
                    P.dma("sp", sms[i % NBUF][:], SMD[i * 128:(i + 1) * 128, :], r=[SMDt[i]], w=[sms[i % NBUF]])
                    P.dma("sp", Mts[i % NBUF][:].rearrange("p h t -> p (h t)"), MD[i], r=[MDt[i]], w=[Mts[i % NBUF]])
                    P.dma("sp", xdts[i % NBUF][:], XDT[i * 128:(i + 1) * 128, :], r=[XDTt[i]], w=[xdts[i % NBUF]])
                    P.dma("sp", xws[i % NBUF][:], XWD[i * 128:(i + 1) * 128, :], r=[XWDt[i]], w=[xws[i % NBUF]])


                def bufs(i):
                    return dict(xt=xt_[i % NBUF], sz=szs[i % NBUF], xtok=xtoks[i % NBUF], BCT=bcts[i % NBUF], Btok=btks[i % NBUF],
                                sm=sms[i % NBUF], xdt=xdts[i % NBUF], xw=xws[i % NBUF], yb=ybs[i % 2], Mt=Mts[i % NBUF])

                def stageB(i):
                    B = bufs(i)
                    sm, xtok, xdt, sz, yb, BCT = B["sm"], B["xtok"], B["xdt"], B["sz"], B["yb"], B["BCT"]
                    ecs = sm[:, 64:96]
                    for g in range(4):
                        M = B["Mt"]
                        hs = slice(g * 8, (g + 1) * 8)
                        gsl = slice(g * 512, (g + 1) * 512)
                        pyi = psb[4 + (g % 2)]

                        def yimm(e, pyi=pyi, M=M, g=g, xdt=xdt, xtok=xtok):
                            ins = None
                            for r8 in range(8):
                                h = g * 8 + r8
                                e.matmul(pyi[:, r8 * 64:(r8 + 1) * 64], lhsT=M[:, h, :], rhs=xdt[:, h * 64:(h + 1) * 64],
                                         start=True, stop=False)
                                ins = e.matmul(pyi[:, r8 * 64:(r8 + 1) * 64], lhsT=Did[:, h, :], rhs=xtok[:, h * 64:(h + 1) * 64],
                                               start=False, stop=True)
                            return ins
                        P.op("pe", yimm, r=[M, xdt, Did, xtok], w=[pyi])
                        pyo = psb[6 + (g % 2)]
                        P.op("pe", ("matmul", dict(out=pyo[:], lhsT=BCT[:, 4 + g, :], rhs=STb[:, gsl], start=True, stop=True)),
                             r=[BCT, STb], w=[pyo])
                        a1 = t1[g % 2]
                        a2 = t2[g % 2]
                        P.op("dve", ("tensor_tensor", dict(out=a1[:].rearrange("p (h q) -> p h q", h=8),
                                                           in0=pyo[:].rearrange("p (h q) -> p h q", h=8),
                                                           in1=ecs[:, hs].unsqueeze(2).broadcast_to([128, 8, 64]), op=ALU.mult)),
                             r=[pyo, sm], w=[a1])
                        P.op("dve", ("tensor_tensor", dict(out=a1[:], in0=pyi[:], in1=a1[:], op=ALU.add)), r=[pyi, a1], w=[a1])
                        P.op("dve", ("tensor_tensor", dict(out=a1[:], in0=a1[:], in1=sz[:, gsl], op=ALU.mult)), r=[a1, sz], w=[a1])
                        P.op("act", ("activation", dict(out=a2[:], in_=a1[:], func=AF.Square, accum_out=gss[:, g:g + 1])),
                             r=[a1], w=[a2, gss])
                        P.op("dve", ("tensor_scalar", dict(out=grs[:, g:g + 1], in0=gss[:, g:g + 1], scalar1=1.0 / 512, scalar2=EPS,
                                                           op0=ALU.mult, op1=ALU.add)), r=[gss], w=[grs])
                        P.op("act", ("activation", dict(out=grs[:, g:g + 1], in_=grs[:, g:g + 1], func=AF.Sqrt)), r=[grs], w=[grs])
                        P.op("dve", ("reciprocal", dict(out=grs[:, g:g + 1], in_=grs[:, g:g + 1])), r=[grs], w=[grs])
                        P.op("dve", ("scalar_tensor_tensor", dict(out=yb[:, gsl], in0=a1[:], scalar=grs[:, g:g + 1], in1=nwb[:, gsl],
                                                                  op0=ALU.mult, op1=ALU.mult)), r=[a1, grs, nwb], w=[yb])

                def stageC(i):
                    B = bufs(i)
                    Btok, xw, sm = B["Btok"], B["xw"], B["sm"]
                    ecl = sm[:, 96:128]
                    for g in range(4):
                        gsl = slice(g * 512, (g + 1) * 512)
                        hs = slice(g * 8, (g + 1) * 8)
                        pst = ps1()
                        P.op("pe", ("matmul", dict(out=pst[:], lhsT=Btok[:, g, :], rhs=xw[:, gsl], start=True, stop=True)),
                             r=[Btok, xw], w=[pst])
                        P.op("dve", ("tensor_tensor", dict(out=ST[:, gsl].rearrange("p (h q) -> p h q", h=8),
                                                           in0=ST[:, gsl].rearrange("p (h q) -> p h q", h=8),
                                                           in1=ecl[:, hs].unsqueeze(2).broadcast_to([128, 8, 64]), op=ALU.mult)),
                             r=[ST, sm], w=[ST])
                        P.op("dve", ("tensor_tensor", dict(out=ST[:, gsl], in0=pst[:], in1=ST[:, gsl], op=ALU.add)), r=[pst, ST], w=[ST])
                        P.op("act", ("copy", dict(out=STb[:, gsl], in_=ST[:, gsl])), r=[ST], w=[STb])

                def stageD(i):
                    B = bufs(i)
                    xt, yb = B["xt"], B["yb"]
                    for q4 in range(2):
                        pt = ps1()
                        ptv = pt[:].bitcast(BF16).rearrange("p (k t) -> p k t", k=8)

                        def try_(e, ptv=ptv, q4=q4, yb=yb):
                            ins = None
                            for k in range(8):
                                c = q4 * 8 + k
                                ins = e.transpose(out=ptv[:, k, :], in_=yb[:, c * 128:(c + 1) * 128], identity=ident[:])
                            return ins
                        P.op("pe", try_, r=[yb, ident], w=[pt])
                        P.op("act", ("copy", dict(out=ybT[:, q4 * 8:(q4 + 1) * 8, :], in_=ptv)), r=[pt], w=[ybT])
                    for n in range(2):
                        py = ps1()
                        mm_group(P, py, py[:], [(ybT[:, c, :], w_out[:, c, n * 512:(n + 1) * 512]) for c in range(16)], r=[ybT, w_out])
                        P.op("dve", ("tensor_tensor", dict(out=xt[:, n * 512:(n + 1) * 512], in0=py[:], in1=xt[:, n * 512:(n + 1) * 512],
                                                           op=ALU.add)), r=[py, xt], w=[xt])
                    P.dma("sp", X3[i * 128:(i + 1) * 128, :], xt[:], r=[xt], w=[X3t[i]])

                loads(0)
                if NT > 1:
                    loads(1)
                for i in range(NT):
                    if i + 2 < NT:
                        loads(i + 2)
                    stageB(i)
                    stageC(i)
                    stageD(i)

        outT = [T(None, "out%d" % i) for i in range(NT // 2)]
        try:
            phase1()
            if PhaseStack.stopped or debug == "p1":
                raise StopBuild()
            ffn_phase("f0", [[(X1[i * 128:(i + 1) * 128, :], X1t[i])] for i in range(NT)], IN("ffn_norm_w")[0],
                      [(IN("ffn_w_gate"), IN("ffn_w_up"), IN("ffn_w_down"))], False,
                      lambda i: (X2[i * 128:(i + 1) * 128, :], X2t[i]), False)
            if PhaseStack.stopped or debug == "p2":
                raise StopBuild()
            phase3b()
            phase3m()
            phase3()
            if PhaseStack.stopped or debug == "p3":
                raise StopBuild()
            NH = NT // 2
            experts = [(IN("moe_w_gate")[e_], IN("moe_w_up")[e_], IN("moe_w_down")[e_]) for e_ in range(NE)]
            ffn_phase("m1", [[(X3[i * 128:(i + 1) * 128, :], X3t[i]), (X3[(NH + i) * 128:(NH + i + 1) * 128, :], X3t[NH + i])]
                             for i in range(NH)], IN("ffn_norm_w")[1], experts, True,
                      lambda i: (out[i * 128:(i + 1) * 128, :], outT[i]), True)
            P.final_wait("sp", outT)
        except StopBuild:
            pass
        if debug is not None:
            srcX, srcT = {"p1": (X1, X1t), "p2": (X2, X2t), "p3": (X3, X3t)}.get(debug, (X1, X1t))
            dts = [T(None, "dbg%d" % i) for i in range(NT * 4)]
            for i in range(NT):
                for q in range(4):
                    P.dma("sp", dbgt[:], srcX[i * 128:(i + 1) * 128, q * 256:(q + 1) * 256], r=[srcT[i]], w=[dbgt])
                    P.dma("sp", dbg[i * 128:(i + 1) * 128, q * 256:(q + 1) * 256], dbgt[:], r=[dbgt], w=[dts[i * 4 + q]])
            P.final_wait("sp", dts)
        P.emit()
    nc._in_names = list(_ins.keys())
    return nc


_CACHE = {}

SEQ = 8192
NCORES = 8


def kernel(**inputs):
    x = np.asarray(inputs["x"], dtype=np.float32)
    B = x.shape[0]
    L = x.shape[1]
    if L not in _CACHE:
        _CACHE[L] = build_program(L)
    nc = _CACHE[L]
    sq = {}
    for k, v in inputs.items():
        if k == "x":
            continue
        v = np.asarray(v, dtype=np.float32)
        if k in ("mix_norm_w", "ffn_norm_w", "final_norm_w", "hg_lb_logits"):
            sq[k] = np.ascontiguousarray(v)
        else:
            sq[k] = np.ascontiguousarray(v[0])
    in_maps = []
    for c in range(NCORES):
        b, half = c // 2, c % 2
        m = {"x": np.ascontiguousarray(x[b]), "sel": np.array([1.0 - half, float(half)], np.float32)}
        for k in nc._in_names:
            if k not in m:
                m[k] = sq[k]
        in_maps.append({k: m[k] for k in nc._in_names})
    res = run_bass_kernel_spmd(nc, in_maps, core_ids=list(range(NCORES)))
    outp = np.empty((B, L, D), np.float32)
    LH = L // 2
    for c in range(NCORES):
        b, half = c // 2, c % 2
        outp[b, half * LH:(half + 1) * LH] = res.results[c]["out"]
    return outp
```

```python
import numpy as np
from contextlib import ExitStack
import concourse.bass as bass
import concourse.mybir as mybir
from concourse.bass_utils import run_bass_kernel_spmd

F32, BF16 = mybir.dt.float32, mybir.dt.bfloat16
AF = mybir.ActivationFunctionType
ALU = mybir.AluOpType
AX = mybir.AxisListType

D = 1024
EPS = 1e-6
HG_H = 8
DFF = 2816
NFC = DFF // 128
NE = 8
DIN = 2048
SSD_H = 32
NG = 4
SSD_IN = 5152
CONV = 3072
SELF_SYNC = True


class T:
    def __init__(self, t, name=""):
        self.t = t
        self.name = name
        self.lw = None
        self.rd = {}

    def __getitem__(self, idx):
        return self.t[idx]


class Stream:
    def __init__(self, name):
        self.name = name
        self.ops = []
        self.seen = {}
        self.cnt = 0
        self.dcnt = None
        self.di = 0


class Prog:
    NDS = 12

    def __init__(self, nc, es):
        self.nc = nc
        self.es = es
        self.sems = {}
        self.streams = {}
        for n in ["pe", "act", "dve", "pool", "sp"]:
            st = Stream(n)
            self.streams[n] = st
            self.sems[n] = es.enter_context(nc.semaphore("s_" + n))
        for n in ["pool", "sp", "act"]:
            st = self.streams[n]
            st.dcnt = [0] * self.NDS
            for i in range(self.NDS):
                self.sems[(n, i)] = es.enter_context(nc.semaphore("d_%s%d" % (n, i)))

    def sb(self, name, shape, dt):
        return T(self.es.enter_context(self.nc.sbuf_tensor(name, list(shape), dt)), name)

    def ps(self, name, shape, dt):
        return T(self.es.enter_context(self.nc.psum_tensor(name, list(shape), dt)), name)

    def op(self, s, fn, r=(), w=(), dma=False):
        st = self.streams[s]
        waits = {}

        def addw(ev, raw):
            if ev is None:
                return
            key, val, owner, is_dma = ev
            if owner == s and not is_dma:
                if s == "pe" or not SELF_SYNC:
                    return
            if st.seen.get(key, 0) >= val:
                return
            if waits.get(key, 0) < val:
                waits[key] = val

        for t in r:
            addw(t.lw, True)
        for t in w:
            addw(t.lw, False)
            for key, (val, owner, is_dma) in t.rd.items():
                addw((key, val, owner, is_dma), False)
        if dma:
            i = st.di % self.NDS
            st.di += 1
            if st.dcnt[i] > 0 and st.seen.get((s, i), 0) < st.dcnt[i]:
                waits[(s, i)] = st.dcnt[i]
        for key, val in waits.items():
            st.seen[key] = val
        if dma:
            st.dcnt[i] += 16
            key = (s, i)
            ev = (key, st.dcnt[i], s, True)
            inc = 16
        else:
            st.cnt += 1
            key = s
            ev = (key, st.cnt, s, False)
            inc = 1
        st.ops.append((list(waits.items()), fn, key, inc))
        for t in w:
            t.lw = ev
            t.rd = {}
        for t in r:
            if t.lw is ev:
                continue
            old = t.rd.get(key)
            if old is None or old[0] < ev[1]:
                t.rd[key] = (ev[1], s, ev[3])
        return ev

    def dma(self, q, out, in_, r=(), w=(), **kw):
        return self.op(q, ("dma_start", dict(out=out, in_=in_, **kw)), r=r, w=w, dma=True)

    def barrier(self):
        targets = {}
        for n, st in self.streams.items():
            if st.cnt > 0:
                targets[n] = st.cnt
            if st.dcnt is not None:
                for i, v in enumerate(st.dcnt):
                    if v > 0:
                        targets[(n, i)] = v
        for n, st in self.streams.items():
            waits = {}
            for key, val in targets.items():
                if key == n and n == "pe":
                    continue
                if st.seen.get(key, 0) < val:
                    waits[key] = val
                    st.seen[key] = val
            st.ops.append((list(waits.items()), None, None, 0))

    def final_wait(self, s, tiles):
        st = self.streams[s]
        waits = {}
        for t in tiles:
            ev = t.lw
            if ev is not None and waits.get(ev[0], 0) < ev[1]:
                waits[ev[0]] = ev[1]
        st.ops.append((list(waits.items()), None, None, 0))

    def emit(self):
        nc = self.nc
        names = {"pe": "tensor", "act": "scalar", "dve": "vector", "pool": "gpsimd", "sp": "sync"}
        with nc.Block() as block:
            for s, attr in names.items():
                st = self.streams[s]

                def body(e, st=st):
                    for waits, fn, key, inc in st.ops:
                        for k, v in waits:
                            e.wait_ge(self.sems[k], v)
                        if fn is None:
                            continue
                        if isinstance(fn, tuple):
                            ins = getattr(e, fn[0])(**fn[1])
                        else:
                            ins = fn(e)
                        ins.then_inc(self.sems[key], inc)

                getattr(block, attr)(body)


def mm_group(P, out_t, out_ap, pairs, r):
    def fn(e):
        n = len(pairs)
        ins = None
        for i, (l, rr) in enumerate(pairs):
            ins = e.matmul(out_ap, lhsT=l, rhs=rr, start=(i == 0), stop=(i == n - 1))
        return ins

    return P.op("pe", fn, r=r, w=[out_t])


class StopBuild(Exception):
    pass


class PhaseStack(ExitStack):
    stopped = False

    def __exit__(self, et, ev, tb):
        if et is StopBuild:
            PhaseStack.stopped = True
            super().__exit__(None, None, None)
            return True
        return super().__exit__(et, ev, tb)


def build_program(L, debug=None):
    nc = bass.Bass("TRN2", target_bir_lowering=False)
    es = ExitStack()
    PhaseStack.stopped = False
    NT = L // 128
    NBLK = L // 512
    LH = L // 2

    _ins = {}
    SHAPES = {
        "x": [L, D], "sel": [2], "mix_norm_w": [2, D], "ffn_norm_w": [2, D], "final_norm_w": [D],
        "hg_w_in": [D, 4096], "hg_lb_logits": [2, D], "hg_norm_w": [128], "hg_w_out": [D, D],
        "ssd_w_in": [D, SSD_IN], "ssd_conv_w": [4, CONV], "ssd_conv_b": [CONV], "ssd_dt_bias": [SSD_H],
        "ssd_a_log": [SSD_H], "ssd_d": [SSD_H], "ssd_norm_w": [DIN], "ssd_w_out": [DIN, D],
        "ffn_w_gate": [D, DFF], "ffn_w_up": [D, DFF], "ffn_w_down": [DFF, D], "moe_router": [D, NE],
        "moe_w_gate": [NE, D, DFF], "moe_w_up": [NE, D, DFF], "moe_w_down": [NE, DFF, D],
    }

    def IN(name):
        if name not in _ins:
            _ins[name] = nc.dram_tensor(name, list(SHAPES[name]), F32, kind="ExternalInput").ap()
        return _ins[name]

    out = nc.dram_tensor("out", [LH, D], F32, kind="ExternalOutput").ap()
    dbg = None
    if debug is not None:
        dbg = nc.dram_tensor("dbg", [L, D], F32, kind="ExternalOutput").ap()

    X1 = nc.dram_tensor("X1", [L, D], F32, kind="Internal").ap()
    X2 = nc.dram_tensor("X2", [L, D], F32, kind="Internal").ap()
    X3 = nc.dram_tensor("X3", [L, D], F32, kind="Internal").ap()
    X1t = [T(None, "X1_%d" % i) for i in range(NT)]
    X2t = [T(None, "X2_%d" % i) for i in range(NT)]
    X3t = [T(None, "X3_%d" % i) for i in range(NT)]

    P = Prog(nc, es)
    with es:
        if debug is not None:
            dbgt = P.sb("dbgt", [128, 256], F32)
        ident_f = P.sb("ident_f", [128, 128], F32)
        ident = P.sb("ident", [128, 128], BF16)
        ones_f = P.sb("ones_f", [128, 128], F32)
        mask01 = P.sb("mask01", [128, 128], F32)
        P.op("pool", ("memset", dict(ap=ones_f[:], constant=1.0)), w=[ones_f])
        P.op("pool", ("affine_select", dict(out=ident_f[:], in_=ones_f[:], pattern=[[1, 128]],
                                               compare_op=ALU.is_equal, fill=0.0, base=0,
                                               channel_multiplier=-1)), r=[ones_f], w=[ident_f])
        P.op("pool", ("affine_select", dict(out=mask01[:], in_=ones_f[:], pattern=[[1, 128]],
                                               compare_op=ALU.is_ge, fill=0.0, base=0,
                                               channel_multiplier=-1)), r=[ones_f], w=[mask01])
        P.op("dve", ("tensor_copy", dict(out=ident[:], in_=ident_f[:])), r=[ident_f], w=[ident])

        psb = [P.ps("psb%d" % i, [128, 512], F32) for i in range(8)]
        ps_rr = [0]

        def ps1():
            t = psb[ps_rr[0] % 4]
            ps_rr[0] += 1
            return t

        def norm_to_hT(xt, wb, hT_t, hT_ap, tmp, eng_copy="act"):
            junk, ssq, rstd, hb = tmp
            P.op("act", ("activation", dict(out=junk[:], in_=xt[:], func=AF.Square, accum_out=ssq[:])),
                 r=[xt], w=[junk, ssq])
            P.op("dve", ("tensor_scalar", dict(out=rstd[:], in0=ssq[:], scalar1=1.0 / D, scalar2=EPS,
                                                  op0=ALU.mult, op1=ALU.add)), r=[ssq], w=[rstd])
            P.op("act", ("activation", dict(out=rstd[:], in_=rstd[:], func=AF.Ln)), r=[rstd], w=[rstd])
            P.op("act", ("activation", dict(out=rstd[:], in_=rstd[:], func=AF.Exp, scale=-0.5)), r=[rstd], w=[rstd])
            P.op("dve", ("scalar_tensor_tensor", dict(out=hb[:], in0=xt[:], scalar=rstd[:], in1=wb[:],
                                                         op0=ALU.mult, op1=ALU.mult)), r=[xt, rstd, wb], w=[hb])
            pt = ps1()
            ptv = pt[:].bitcast(BF16).rearrange("p (k t) -> p k t", k=8)

            def tr(e):
                ins = None
                for k in range(8):
                    ins = e.transpose(out=ptv[:, k, :], in_=hb[:, k * 128:(k + 1) * 128], identity=ident[:])
                return ins
            P.op("pe", tr, r=[hb, ident], w=[pt])
            if eng_copy == "act":
                P.op("act", ("copy", dict(out=hT_ap, in_=ptv)), r=[pt], w=[hT_t])
            else:
                P.op("dve", ("tensor_copy", dict(out=hT_ap, in_=ptv)), r=[pt], w=[hT_t])

        def phase1():
            with PhaseStack() as es1:
                def sb(name, shape, dt):
                    return T(es1.enter_context(nc.sbuf_tensor(name, list(shape), dt)), name)
                w_in = sb("hg_win", [128, 8, 4096], BF16)
                w_out = sb("hg_wout", [128, 8, 1024], BF16)
                for k in range(8):
                    P.dma("pool", w_in[:, k, :], IN("hg_w_in")[k * 128:(k + 1) * 128, :], w=[w_in])
                P.dma("pool", w_out[:], IN("hg_w_out").rearrange("(k p) n -> p k n", p=128), w=[w_out])
                wb = sb("p1_wb", [128, D], F32)
                P.dma("sp", wb[:], IN("mix_norm_w")[0].partition_broadcast(128), w=[wb])
                gw = sb("p1_gw", [128, 128], F32)
                P.dma("sp", gw[:], IN("hg_norm_w").partition_broadcast(128), w=[gw])
                lbl = sb("p1_lbl", [128, 2, 8], F32)
                P.dma("sp", lbl[:], IN("hg_lb_logits").rearrange("r (h p) -> p r h", p=128), w=[lbl],
                      allow_slow_non_contiguous=True)
                lb = sb("p1_lb", [128, 8], F32)
                oml = sb("p1_oml", [128, 8], F32)
                noml = sb("p1_noml", [128, 8], F32)
                P.op("dve", ("tensor_tensor", dict(out=lb[:], in0=lbl[:, 0, :], in1=lbl[:, 1, :], op=ALU.subtract)),
                     r=[lbl], w=[lb])
                P.op("act", ("activation", dict(out=lb[:], in_=lb[:], func=AF.Sigmoid)), r=[lb], w=[lb])
                P.op("dve", ("tensor_scalar", dict(out=oml[:], in0=lb[:], scalar1=-1.0, scalar2=1.0,
                                                      op0=ALU.mult, op1=ALU.add)), r=[lb], w=[oml])
                P.op("dve", ("tensor_scalar", dict(out=noml[:], in0=lb[:], scalar1=-1.0, scalar2=None,
                                                      op0=ALU.add)), r=[lb], w=[noml])

                xts = [sb("p1_xt%d" % i, [128, D], F32) for i in range(3)]
                junk = sb("p1_junk", [128, D], BF16)
                ssq = sb("p1_ssq", [128, 1], F32)
                rstd = sb("p1_rstd", [128, 1], F32)
                hb = sb("p1_hb", [128, D], BF16)
                hT = sb("p1_hT", [128, 8, 512], BF16)
                QT = sb("p1_QT", [128, 8, 512], BF16)
                KT = sb("p1_KT", [128, 8, 512], BF16)
                vblk = sb("p1_v", [128, 4, D], BF16)
                sgblk = sb("p1_sg", [128, 4, D], BF16)
                sig_ = [sb("p1_sig%d" % i, [128, 512], F32) for i in range(2)]
                kk_ = [sb("p1_kk%d" % i, [128, 512], F32) for i in range(2)]
                ff_ = [sb("p1_ff%d" % i, [128, 512], F32) for i in range(2)]
                bcum_ = [sb("p1_bcum%d" % i, [128, 512], F32) for i in range(2)]
                Ep_ = [sb("p1_Ep%d" % i, [128, 512], F32) for i in range(2)]
                Em_ = [sb("p1_Em%d" % i, [128, 512], F32) for i in range(2)]
                negm_ = [sb("p1_negm%d" % i, [128, 4], F32) for i in range(2)]
                qsb_ = [sb("p1_qsb%d" % i, [128, 512], F32) for i in range(2)]
                e2b = sb("p1_e2", [128, 4, 8], F32)
                e3b = sb("p1_e3", [128, 4, 8], F32)
                emb = sb("p1_em", [128, 4, 8], F32)
                S = sb("p1_S", [128, 8, 128], F32)
                S3 = sb("p1_S3", [128, 8, 128], F32)
                Sp_ = [sb("p1_Sp%d" % i, [128, 8, 128], BF16) for i in range(2)]
                Ktok_ = [sb("p1_Ktok%d" % i, [128, 8, 128], BF16) for i in range(2)]
                attn_ = [sb("p1_attn%d" % i, [128, 8, 128], BF16) for i in range(2)]
                oss = sb("p1_oss", [128, 8], F32)
                orstd = sb("p1_orstd", [128, 8], F32)
                on = sb("p1_on", [128, D], F32)
                osq = on
                ob_ = [sb("p1_ob%d" % i, [128, D], BF16) for i in range(2)]
                obT_ = [sb("p1_obT0", [128, 8, 128], BF16)] * 2
                x1 = [sb("p1_x1_0", [128, D], F32)] * 2
                P.op("pool", ("memset", dict(ap=S[:], constant=0.0)), w=[S])
                P.op("dve", ("memset", dict(ap=psb[4][:], constant=0.0)), w=[psb[4]])
                P.op("dve", ("memset", dict(ap=psb[5][:], constant=0.0)), w=[psb[5]])

                if debug == "s1":
                    raise StopBuild()

                def load_x(i):
                    P.dma("sp", xts[i % 3][:], IN("x")[i * 128:(i + 1) * 128, :], w=[xts[i % 3]])

                for i in range(min(2, NT)):
                    load_x(i)
                for b in range(NBLK):
                    for j in range(4):
                        i = b * 4 + j
                        if i + 2 < NT:
                            load_x(i + 2)
                        norm_to_hT(xts[i % 3], wb, hT, hT[:, :, j * 128:(j + 1) * 128], (junk, ssq, rstd, hb))
                    if debug == "s2":
                        raise StopBuild()
                    def vg_task(tix, b=b):
                        which, rem = divmod(tix, 8)
                        j, n = divmod(rem, 2)
                        pp = psb[6 + (tix % 2)]
                        col = 2048 + which * 1024 + n * 512
                        mm_group(P, pp, pp[:], [(hT[:, k, j * 128:(j + 1) * 128], w_in[:, k, col:col + 512])
                                                for k in range(8)], r=[w_in, hT])
                        if which == 0:
                            P.op("act", ("copy", dict(out=vblk[:, j, n * 512:(n + 1) * 512], in_=pp[:])), r=[pp], w=[vblk])
                        else:
                            P.op("act", ("activation", dict(out=sgblk[:, j, n * 512:(n + 1) * 512], in_=pp[:], func=AF.Sigmoid)),
                                 r=[pp], w=[sgblk])
                    for hd in range(8):
                        sig, kk, ff, bcum, Ep, Em, negm = (sig_[hd % 2], kk_[hd % 2], ff_[hd % 2], bcum_[hd % 2], Ep_[hd % 2],
                                                          Em_[hd % 2], negm_[hd % 2])
                        qp = ps1()
                        mm_group(P, qp, qp[:], [(w_in[:, k, hd * 128:(hd + 1) * 128], hT[:, k, :]) for k in range(8)],
                                 r=[w_in, hT])
                        qsb = qsb_[hd % 2]
                        P.op("act", ("copy", dict(out=qsb[:], in_=qp[:])), r=[qp], w=[qsb])
                        fp = ps1()
                        mm_group(P, fp, fp[:], [(w_in[:, k, 1024 + hd * 128:1024 + (hd + 1) * 128], hT[:, k, :])
                                                for k in range(8)], r=[w_in, hT])
                        P.op("act", ("activation", dict(out=sig[:], in_=fp[:], func=AF.Exp, scale=-1.0)), r=[fp], w=[sig])
                        P.op("act", ("activation", dict(out=kk[:], in_=sig[:], func=AF.Ln, bias=1.0, scale=1.0)), r=[sig], w=[kk])
                        P.op("act", ("activation", dict(out=ff[:], in_=sig[:], func=AF.Ln, bias=1.0, scale=lb[:, hd:hd + 1])),
                             r=[sig, lb], w=[ff])
                        P.op("dve", ("tensor_tensor", dict(out=ff[:], in0=ff[:], in1=kk[:], op=ALU.subtract)), r=[ff, kk], w=[ff])
                        P.op("act", ("activation", dict(out=kk[:], in_=ff[:], func=AF.Exp)), r=[ff], w=[kk])
                        P.op("dve", ("tensor_scalar", dict(out=kk[:], in0=kk[:], scalar1=-1.0, scalar2=1.0, op0=ALU.mult, op1=ALU.add)),
                             r=[kk], w=[kk])
                        vg_task(hd)
                        def scans(e, bcum=bcum, ff=ff):
                            ins = None
                            for c in range(4):
                                ins = e.tensor_tensor_scan(out=bcum[:, c * 128:(c + 1) * 128], data0=ones_f[:],
                                                           data1=ff[:, c * 128:(c + 1) * 128], initial=0.0,
                                                           op0=ALU.mult, op1=ALU.add)
                            return ins
                        P.op("dve", scans, r=[ff, ones_f], w=[bcum])
                        bc3 = bcum[:].rearrange("p (c t) -> p c t", c=4)
                        P.op("dve", ("tensor_scalar", dict(out=negm[:], in0=bc3[:, :, 63], scalar1=-1.0,
                                                                      scalar2=None, op0=ALU.mult)), r=[bcum], w=[negm])

                        def exps(e, hd=hd, bc3=bc3, bcum=bcum, Ep=Ep, Em=Em, negm=negm):
                            for c in range(4):
                                e.activation(out=Ep[:, c * 128:(c + 1) * 128], in_=bcum[:, c * 128:(c + 1) * 128],
                                             func=AF.Exp, bias=negm[:, c:c + 1], scale=1.0)
                                e.activation(out=Em[:, c * 128:(c + 1) * 128], in_=bcum[:, c * 128:(c + 1) * 128],
                                             func=AF.Exp, bias=bcum[:, c * 128 + 63:c * 128 + 64], scale=-1.0)
                            e.activation(out=e3b[:, :, hd], in_=bc3[:, :, 127], func=AF.Exp)
                            return e.activation(out=emb[:, :, hd], in_=bc3[:, :, 63], func=AF.Exp)
                        P.op("act", exps, r=[bcum, negm], w=[Ep, Em, e3b, emb])
                        Ep3 = Ep[:].rearrange("p (c t) -> p c t", c=4)
                        P.op("dve", ("tensor_copy", dict(out=e2b[:, :, hd], in_=Ep3[:, :, 127])),
                             r=[Ep], w=[e2b])
                        P.op("dve", ("tensor_tensor", dict(out=QT[:, hd, :], in0=qsb[:], in1=Ep[:], op=ALU.mult)),
                             r=[qsb, Ep], w=[QT])
                        P.op("dve", ("tensor_tensor", dict(out=KT[:, hd, :], in0=kk[:], in1=Em[:], op=ALU.mult)),
                             r=[kk, Em], w=[KT])
                    if debug == "s4":
                        raise StopBuild()
                    for tix in range(8, 16):
                        vg_task(tix)
                    pa = [psb[4], psb[5]]
                    po = [psb[6], psb[7]]

                    def c_front(c, b=b):
                        i = b * 4 + c
                        tok = slice(c * 128, (c + 1) * 128)
                        Ktok, attn, Sp, ob, obT = Ktok_[i % 2], attn_[i % 2], Sp_[i % 2], ob_[i % 2], obT_[i % 2]
                        pk = ps1()
                        pkv = pk[:].bitcast(BF16).rearrange("p (k t) -> p k t", k=8)

                        def trk(e, pkv=pkv, tok=tok):
                            ins = None
                            for hd in range(8):
                                ins = e.transpose(out=pkv[:, hd, :], in_=KT[:, hd, tok], identity=ident[:])
                            return ins
                        P.op("pe", trk, r=[KT, ident], w=[pk])
                        P.op("act", ("copy", dict(out=Ktok[:], in_=pkv)), r=[pk], w=[Ktok])
                        pa = [psb[4], psb[5]]
                        for half in range(2):
                            def att(e, half=half, tok=tok):
                                ins = None
                                for q4 in range(4):
                                    hd = half * 4 + q4
                                    t0 = tok.start
                                    e.matmul(pa[half][:, q4 * 128 + 64:(q4 + 1) * 128], lhsT=KT[:, hd, tok], rhs=QT[:, hd, t0 + 64:t0 + 128],
                                             start=True, stop=True)
                                    ins = e.matmul(pa[half][0:64, q4 * 128:q4 * 128 + 64], lhsT=KT[:, hd, t0:t0 + 64],
                                                   rhs=QT[:, hd, t0:t0 + 64], start=True, stop=True)
                                return ins
                            P.op("pe", att, r=[KT, QT], w=[pa[half]])
                            P.op("dve", ("tensor_tensor", dict(
                                out=attn[:, half * 4:(half + 1) * 4, :],
                                in0=pa[half][:].rearrange("p (h t) -> p h t", h=4),
                                in1=mask01[:].unsqueeze(1).broadcast_to([128, 4, 128]), op=ALU.mult)),
                                r=[pa[half], mask01], w=[attn])
                    def c_mid(c, b=b):
                        i = b * 4 + c
                        tok = slice(c * 128, (c + 1) * 128)
                        Ktok, attn, Sp, ob, obT = Ktok_[i % 2], attn_[i % 2], Sp_[i % 2], ob_[i % 2], obT_[i % 2]
                        P.op("dve", ("tensor_tensor", dict(out=Sp[:], in0=S[:],
                                                                  in1=emb[:, c, :].unsqueeze(2).broadcast_to([128, 8, 128]),
                                                                  op=ALU.mult)), r=[S, emb], w=[Sp])
                        po = [psb[6], psb[7]]
                        for half in range(2):
                            def omm(e, half=half, tok=tok, c=c, attn=attn, Sp=Sp):
                                ins = None
                                for q4 in range(4):
                                    hd = half * 4 + q4
                                    e.matmul(po[half][:, q4 * 128:(q4 + 1) * 128], lhsT=attn[:, hd, :],
                                             rhs=vblk[:, c, hd * 128:(hd + 1) * 128], start=True, stop=False)
                                    ins = e.matmul(po[half][:, q4 * 128:(q4 + 1) * 128], lhsT=QT[:, hd, tok],
                                                   rhs=Sp[:, hd, :], start=False, stop=True)
                                return ins
                            P.op("pe", omm, r=[attn, vblk, QT, Sp], w=[po[half]])
                        pp2 = [ps1(), ps1()]
                        for half in range(2):
                            def pmm(e, half=half, c=c, pp2=pp2, Ktok=Ktok):
                                ins = None
                                for q4 in range(4):
                                    hd = half * 4 + q4
                                    ins = e.matmul(pp2[half][:, q4 * 128:(q4 + 1) * 128], lhsT=Ktok[:, hd, :],
                                                   rhs=vblk[:, c, hd * 128:(hd + 1) * 128], start=True, stop=True)
                                return ins
                            P.op("pe", pmm, r=[Ktok, vblk], w=[pp2[half]])
                        P.op("dve", ("tensor_tensor", dict(out=S3[:], in0=S[:],
                                                                   in1=e3b[:, c, :].unsqueeze(2).broadcast_to([128, 8, 128]),
                                                                   op=ALU.mult)), r=[S, e3b], w=[S3])
                        for half in range(2):
                            P.op("dve", ("tensor_tensor", dict(
                                out=S[:, half * 4:(half + 1) * 4, :],
                                in0=pp2[half][:].rearrange("p (h v) -> p h v", h=4),
                                in1=e2b[:, c, half * 4:(half + 1) * 4].unsqueeze(2).broadcast_to([128, 4, 128]),
                                op=ALU.mult)), r=[pp2[half], e2b], w=[S])
                        P.op("dve", ("tensor_tensor", dict(out=S[:], in0=S[:], in1=S3[:], op=ALU.add)), r=[S, S3], w=[S])
                    def c_tail(c, b=b):
                        i = b * 4 + c
                        tok = slice(c * 128, (c + 1) * 128)
                        Ktok, attn, Sp, ob, obT = Ktok_[i % 2], attn_[i % 2], Sp_[i % 2], ob_[i % 2], obT_[i % 2]
                        for half in range(2):
                            P.op("act", ("activation", dict(out=osq[:, half * 512:(half + 1) * 512], in_=po[half][:],
                                                                         func=AF.Square)), r=[po[half]], w=[osq])
                        P.op("dve", ("tensor_reduce", dict(out=oss[:], in_=osq[:].rearrange("p (h v) -> p h v", h=8),
                                                              axis=AX.X, op=ALU.add)), r=[osq], w=[oss])
                        P.op("dve", ("tensor_scalar", dict(out=orstd[:], in0=oss[:], scalar1=1.0 / 128, scalar2=EPS,
                                                              op0=ALU.mult, op1=ALU.add)), r=[oss], w=[orstd])
                        P.op("act", ("activation", dict(out=orstd[:], in_=orstd[:], func=AF.Ln)), r=[orstd], w=[orstd])
                        P.op("act", ("activation", dict(out=orstd[:], in_=orstd[:], func=AF.Exp, scale=-0.5)), r=[orstd], w=[orstd])
                        for half in range(2):
                            P.op("dve", ("tensor_tensor", dict(
                                out=on[:, half * 512:(half + 1) * 512].rearrange("p (h v) -> p h v", h=4),
                                in0=po[half][:].rearrange("p (h v) -> p h v", h=4),
                                in1=orstd[:, half * 4:(half + 1) * 4].unsqueeze(2).broadcast_to([128, 4, 128]),
                                op=ALU.mult)), r=[po[half], orstd], w=[on])
                        P.op("dve", ("tensor_tensor", dict(out=on[:].rearrange("p (h v) -> p h v", h=8),
                                                               in0=on[:].rearrange("p (h v) -> p h v", h=8),
                                                               in1=gw[:].unsqueeze(1).broadcast_to([128, 8, 128]),
                                                               op=ALU.mult)), r=[on, gw], w=[on])
                        P.op("dve", ("tensor_tensor", dict(out=ob[:], in0=on[:], in1=sgblk[:, c, :], op=ALU.mult)),
                             r=[on, sgblk], w=[ob])
                        pt = ps1()
                        ptv = pt[:].bitcast(BF16).rearrange("p (k t) -> p k t", k=8)

                        def tro(e, ptv=ptv, ob=ob):
                            ins = None
                            for hd in range(8):
                                ins = e.transpose(out=ptv[:, hd, :], in_=ob[:, hd * 128:(hd + 1) * 128], identity=ident[:])
                            return ins
                        P.op("pe", tro, r=[ob, ident], w=[pt])
                        P.op("act", ("copy", dict(out=obT[:], in_=ptv)), r=[pt], w=[obT])
                        xo = x1[i % 2]
                        P.dma("sp", xo[:], IN("x")[i * 128:(i + 1) * 128, :], w=[xo])
                        for n in range(2):
                            py = ps1()
                            mm_group(P, py, py[:], [(obT[:, hd, :], w_out[:, hd, n * 512:(n + 1) * 512]) for hd in range(8)],
                                     r=[obT, w_out])
                            P.op("dve", ("tensor_tensor", dict(
                                out=xo[:, n * 512:(n + 1) * 512], in0=py[:], in1=xo[:, n * 512:(n + 1) * 512], op=ALU.add)),
                                r=[py, xo], w=[xo])
                        P.dma("sp", X1[i * 128:(i + 1) * 128, :], xo[:], r=[xo], w=[X1t[i]])

                    c_front(0)
                    for c in range(4):
                        c_mid(c)
                        if c + 1 < 4:
                            c_front(c + 1)
                        c_tail(c)

        def ffn_phase(tag, src_tiles, norm_w_row, experts, moe, dst_fn, final):
            NTT = len(src_tiles)
            P.barrier()
            PASS = min(16, NTT)
            with PhaseStack() as es2:
                def sb(name, shape, dt):
                    return T(es2.enter_context(nc.sbuf_tensor(tag + name, list(shape), dt)), name)
                wb = sb("wb", [128, D], F32)
                P.dma("sp", wb[:], norm_w_row.partition_broadcast(128), w=[wb])
                acc = [sb("acc%d" % i, [128, D], F32) for i in range(PASS)]
                hT = sb("hT", [128, 8, PASS * 128], BF16)
                junk = sb("junk", [128, D], BF16)
                ssq = sb("ssq", [128, 1], F32)
                rstd = sb("rstd", [128, 1], F32)
                hb = sb("hb", [128, D], BF16)
                wgs = [sb("wg%d" % i, [128, 8, 256], BF16) for i in range(2)]
                wus = [sb("wu%d" % i, [128, 8, 256], BF16) for i in range(2)]
                wds = [sb("wd%d" % i, [128, 2, D], BF16) for i in range(2)]
                sgl = [sb("sgl%d" % i, [128, 512], F32) for i in range(2)]
                Ab = [sb("A%d" % i, [128, 2, 512], BF16) for i in range(2)]
                if moe:
                    sel = sb("sel", [128, 2], F32)
                    P.dma("sp", sel[:], IN("sel").partition_broadcast(128), w=[sel])
                    xb = sb("xb", [128, D], F32)
                    wr = sb("wr", [128, 8, NE], F32)
                    P.dma("sp", wr[:], IN("moe_router").rearrange("(k p) n -> p k n", p=128), w=[wr])
                    hf = sb("hf", [128, D], F32)
                    hT32 = sb("hT32", [128, 8, 128], F32)
                    gates = sb("gates", [128, PASS, NE], F32)
                    lg = sb("lg", [128, NE], F32)
                    mx8 = sb("mx8", [128, 8], F32)
                    pe_ = sb("pexp", [128, NE], F32)
                    msk = sb("msk", [128, NE], F32)
                    den = sb("den", [128, 1], F32)
                if final:
                    fwb = sb("fwb", [128, D], F32)
                    P.dma("sp", fwb[:], IN("final_norm_w").partition_broadcast(128), w=[fwb])
                    ot = [sb("ot%d" % i, [128, D], F32) for i in range(2)]
                wcount = [0]
                for p0 in range(0, NTT, PASS):
                    npt = min(PASS, NTT - p0)
                    ntb = npt // 4
                    for j in range(npt):
                        i = p0 + j
                        srcs = src_tiles[i]
                        if not moe:
                            ap, tt = srcs[0]
                            P.dma("sp", acc[j][:], ap, r=[tt], w=[acc[j]])
                        else:
                            (apa, ta), (apb, tb_) = srcs
                            P.dma("sp", acc[j][:], apa, r=[ta], w=[acc[j]])
                            P.dma("sp", xb[:], apb, r=[tb_], w=[xb])
                            P.op("dve", ("tensor_scalar", dict(out=acc[j][:], in0=acc[j][:], scalar1=sel[:, 0:1],
                                                                      scalar2=None, op0=ALU.mult)), r=[acc[j], sel], w=[acc[j]])
                            P.op("dve", ("scalar_tensor_tensor", dict(out=acc[j][:], in0=xb[:], scalar=sel[:, 1:2],
                                                                             in1=acc[j][:], op0=ALU.mult, op1=ALU.add)),
                                 r=[xb, sel, acc[j]], w=[acc[j]])
                        norm_to_hT(acc[j], wb, hT, hT[:, :, j * 128:(j + 1) * 128], (junk, ssq, rstd, hb))
                        if moe:
                            P.op("dve", ("scalar_tensor_tensor", dict(out=hf[:], in0=acc[j][:], scalar=rstd[:], in1=wb[:],
                                                                      op0=ALU.mult, op1=ALU.mult)), r=[acc[j], rstd, wb], w=[hf])
                            for hh in range(2):
                                ptr = psb[4 + hh]

                                def trf(e, ptr=ptr, hh=hh):
                                    ins = None
                                    for k4 in range(4):
                                        k = hh * 4 + k4
                                        ins = e.transpose(out=ptr[:, k4 * 128:(k4 + 1) * 128], in_=hf[:, k * 128:(k + 1) * 128],
                                                          identity=ident_f[:])
                                    return ins
                                P.op("pe", trf, r=[hf, ident_f], w=[ptr])
                                P.op("act", ("copy", dict(out=hT32[:, hh * 4:(hh + 1) * 4, :],
                                                          in_=ptr[:].rearrange("p (k t) -> p k t", k=4))), r=[ptr], w=[hT32])
                            pl = ps1()
                            mm_group(P, pl, pl[:, 0:NE], [(hT32[:, k, :], wr[:, k, :]) for k in range(8)], r=[hT32, wr])
                            P.op("act", ("copy", dict(out=lg[:], in_=pl[:, 0:NE])), r=[pl], w=[lg])
                            P.op("dve", ("max", dict(out=mx8[:], in_=lg[:])), r=[lg], w=[mx8])
                            P.op("dve", ("tensor_scalar", dict(out=msk[:], in0=lg[:], scalar1=mx8[:, 1:2], scalar2=None,
                                                                  op0=ALU.is_ge)), r=[lg, mx8], w=[msk])
                            P.op("dve", ("tensor_scalar", dict(out=pe_[:], in0=lg[:], scalar1=mx8[:, 0:1], scalar2=None,
                                                                  op0=ALU.subtract)), r=[lg, mx8], w=[pe_])
                            P.op("act", ("activation", dict(out=pe_[:], in_=pe_[:], func=AF.Exp)), r=[pe_], w=[pe_])
                            P.op("dve", ("tensor_tensor", dict(out=pe_[:], in0=pe_[:], in1=msk[:], op=ALU.mult)),
                                 r=[pe_, msk], w=[pe_])
                            P.op("dve", ("tensor_reduce", dict(out=den[:], in_=pe_[:], axis=AX.X, op=ALU.add)),
                                 r=[pe_], w=[den])
                            P.op("dve", ("reciprocal", dict(out=den[:], in_=den[:])), r=[den], w=[den])
                            P.op("dve", ("tensor_scalar", dict(out=gates[:, j, :], in0=pe_[:], scalar1=den[:, 0:1],
                                                                      scalar2=None, op0=ALU.mult)), r=[pe_, den], w=[gates])
                    if debug == "f1":
                        raise StopBuild()
                    for ei, (wg_ap, wu_ap, wd_ap) in enumerate(experts):
                        for fb in range(NFC // 2):
                            if debug in ("f2", "f3", "f6") and fb == {"f2": 1, "f3": 3, "f6": 6}[debug]:
                                raise StopBuild()
                            wi = wcount[0] % 2
                            wcount[0] += 1
                            wg, wu, wd = wgs[wi], wus[wi], wds[wi]
                            f0 = fb * 256
                            P.dma("pool", wg[:], wg_ap[:, f0:f0 + 256].rearrange("(k p) n -> p k n", p=128), w=[wg])
                            P.dma("pool", wu[:], wu_ap[:, f0:f0 + 256].rearrange("(k p) n -> p k n", p=128), w=[wu])
                            P.dma("pool", wd[:], wd_ap[f0:f0 + 256, :].rearrange("(c p) n -> p c n", p=128), w=[wd])
                            for tb in range(ntb):
                                tk = slice(tb * 512, (tb + 1) * 512)
                                A = Ab[(wcount[0] * 4 + tb) % 2]
                                for cc in range(2):
                                    pg = ps1()
                                    mm_group(P, pg, pg[:], [(wg[:, k, cc * 128:(cc + 1) * 128], hT[:, k, tk]) for k in range(8)],
                                             r=[wg, hT])
                                    pu = ps1()
                                    mm_group(P, pu, pu[:], [(wu[:, k, cc * 128:(cc + 1) * 128], hT[:, k, tk]) for k in range(8)],
                                             r=[wu, hT])
                                    sg = sgl[cc]
                                    P.op("act", ("activation", dict(out=sg[:], in_=pg[:], func=AF.Silu)),
                                         r=[pg], w=[sg])
                                    P.op("dve", ("tensor_tensor", dict(out=A[:, cc, :], in0=pu[:], in1=sg[:],
                                                                                                     op=ALU.mult)),
                                         r=[pu, sg], w=[A])
                                for t4 in range(4):
                                    j = tb * 4 + t4
                                    for n in range(2):
                                        pd = psb[4 + (t4 * 2 + n) % 4]
                                        mm_group(P, pd, pd[:], [(A[:, cc, t4 * 128:(t4 + 1) * 128], wd[:, cc, n * 512:(n + 1) * 512])
                                                                for cc in range(2)], r=[A, wd])
                                        if moe:
                                            P.op("dve", ("scalar_tensor_tensor", dict(
                                                out=acc[j][:, n * 512:(n + 1) * 512], in0=pd[:], scalar=gates[:, j, ei:ei + 1],
                                                in1=acc[j][:, n * 512:(n + 1) * 512], op0=ALU.mult, op1=ALU.add)),
                                                r=[pd, gates, acc[j]], w=[acc[j]])
                                        else:
                                            P.op("dve", ("tensor_tensor", dict(
                                                out=acc[j][:, n * 512:(n + 1) * 512], in0=pd[:],
                                                in1=acc[j][:, n * 512:(n + 1) * 512], op=ALU.add)),
                                                r=[pd, acc[j]], w=[acc[j]])
                    for j in range(npt):
                        i = p0 + j
                        dap, dt_ = dst_fn(i)
                        if final:
                            o = ot[j % 2]
                            P.op("act", ("activation", dict(out=junk[:], in_=acc[j][:], func=AF.Square, accum_out=ssq[:])),
                                 r=[acc[j]], w=[junk, ssq])
                            P.op("dve", ("tensor_scalar", dict(out=rstd[:], in0=ssq[:], scalar1=1.0 / D, scalar2=EPS,
                                                                  op0=ALU.mult, op1=ALU.add)), r=[ssq], w=[rstd])
                            P.op("act", ("activation", dict(out=rstd[:], in_=rstd[:], func=AF.Sqrt)), r=[rstd], w=[rstd])
                            P.op("dve", ("reciprocal", dict(out=rstd[:], in_=rstd[:])), r=[rstd], w=[rstd])
                            P.op("dve", ("scalar_tensor_tensor", dict(out=o[:], in0=acc[j][:], scalar=rstd[:], in1=fwb[:],
                                                                                  op0=ALU.mult, op1=ALU.mult)),
                                 r=[acc[j], rstd, fwb], w=[o])
                            P.dma("sp", dap, o[:], r=[o], w=[dt_])
                        else:
                            P.dma("sp", dap, acc[j][:], r=[acc[j]], w=[dt_])

        ZS = nc.dram_tensor("ZS", [L, DIN], BF16, kind="Internal").ap()
        XT = nc.dram_tensor("XT", [L, DIN], BF16, kind="Internal").ap()
        BTK = nc.dram_tensor("BTK", [L, 512], BF16, kind="Internal").ap()
        BCD = nc.dram_tensor("BCD", [NT, 128, 1024], BF16, kind="Internal").ap()
        SMD = nc.dram_tensor("SMD", [L, 128], F32, kind="Internal").ap()
        CSD = nc.dram_tensor("CSD", [NT, SSD_H * 128], F32, kind="Internal").ap()
        CBD = nc.dram_tensor("CBD", [NT, 128, 512], F32, kind="Internal").ap()
        XDT = nc.dram_tensor("XDT", [L, DIN], BF16, kind="Internal").ap()
        XWD = nc.dram_tensor("XWD", [L, DIN], BF16, kind="Internal").ap()
        MD = nc.dram_tensor("MD", [NT, 128, SSD_H * 128], BF16, kind="Internal").ap()
        XDTt = [T(None, "XDT_%d" % i) for i in range(NT)]
        XWDt = [T(None, "XWD_%d" % i) for i in range(NT)]
        MDt = [T(None, "MD_%d" % i) for i in range(NT)]
        ZSt = [T(None, "ZS_%d" % i) for i in range(NT)]
        XTt = [T(None, "XT_%d" % i) for i in range(NT)]
        BTKt = [T(None, "BTK_%d" % i) for i in range(NT)]
        BCDt = [T(None, "BCD_%d" % i) for i in range(NT)]
        SMDt = [T(None, "SMD_%d" % i) for i in range(NT)]
        CSDt = [T(None, "CSD_%d" % i) for i in range(NT)]
        CBDt = [T(None, "CBD_%d" % i) for i in range(NT)]

        def phase3b():
            P.barrier()
            with PhaseStack() as es3:
                def sb(name, shape, dt):
                    return T(es3.enter_context(nc.sbuf_tensor("pb_" + name, list(shape), dt)), name)
                w_in = sb("win", [128, 8, SSD_IN], BF16)
                for k in range(8):
                    P.dma("pool", w_in[:, k, :], IN("ssd_w_in")[k * 128:(k + 1) * 128, :], w=[w_in])
                wb = sb("wb", [128, D], F32)
                P.dma("sp", wb[:], IN("mix_norm_w")[1].partition_broadcast(128), w=[wb])
                dtb = sb("dtb", [128, SSD_H], F32)
                P.dma("sp", dtb[:], IN("ssd_dt_bias").partition_broadcast(128), w=[dtb])
                ab = sb("ab", [128, SSD_H], F32)
                P.dma("sp", ab[:], IN("ssd_a_log").partition_broadcast(128), w=[ab])
                P.op("act", ("activation", dict(out=ab[:], in_=ab[:], func=AF.Exp)), r=[ab], w=[ab])
                P.op("dve", ("tensor_scalar", dict(out=ab[:], in0=ab[:], scalar1=-1.0, scalar2=None, op0=ALU.mult)), r=[ab], w=[ab])
                cwr = sb("cwr", [120, 128], F32)
                P.dma("sp", cwr[0:96, :], IN("ssd_conv_w").rearrange("j (c p) -> (j c) p", p=128), w=[cwr])
                P.dma("sp", cwr[96:120, :], IN("ssd_conv_b").rearrange("(c p) -> c p", p=128), w=[cwr])
                cw = sb("cw", [128, 120], F32)
                pc = ps1()
                P.op("pe", ("transpose", dict(out=pc[:, 0:120], in_=cwr[:], identity=ident_f[0:120, 0:120])), r=[cwr, ident_f], w=[pc])
                P.op("act", ("copy", dict(out=cw[:], in_=pc[:, 0:120])), r=[pc], w=[cw])
                diag = sb("diag", [128, 24, 4, 128], BF16)

                def mkdiag(e):
                    ins = None
                    for c in range(24):
                        for j in range(4):
                            ins = e.tensor_scalar(out=diag[:, c, j, :], in0=ident_f[:], scalar1=cw[:, j * 24 + c:j * 24 + c + 1],
                                                  scalar2=None, op0=ALU.mult)
                    return ins
                P.op("dve", mkdiag, r=[cw, ident_f], w=[diag])

                xts = [sb("xt%d" % i, [128, D], F32) for i in range(2)]
                junk = sb("junk", [128, D], BF16)
                ssq = sb("ssq", [128, 1], F32)
                rstd = sb("rstd", [128, 1], F32)
                hb = sb("hb", [128, D], BF16)
                hT = sb("hT", [128, 8, 512], BF16)
                szs = [sb("sz%d" % i, [128, DIN], BF16) for i in range(2)]
                xbc = sb("xbc", [128, 24, 515], BF16)
                xcT = sb("xcT", [128, 16, 512], BF16)
                BCT = sb("BCT", [128, 8, 512], BF16)
                bcts = [sb("bct0", [128, 8, 128], BF16)] * 2
                xtoks = [sb("xtok%d" % i, [128, DIN], BF16) for i in range(2)]
                btks = [sb("btk%d" % i, [128, 4, 128], BF16) for i in range(2)]
                sms = [sb("sm%d" % i, [128, 128], F32) for i in range(2)]
                da = sb("da", [128, SSD_H], F32)
                csTs = [sb("csT%d" % i, [32, 128], F32) for i in range(2)]
                cbms = [sb("cbm%d" % i, [128, 4, 128], F32) for i in range(2)]
                P.op("pool", ("memset", dict(ap=xbc[:], constant=0.0)), w=[xbc])
                for i in range(2):
                    P.op("pool", ("memset", dict(ap=sms[i][:], constant=0.0)), w=[sms[i]])

                for b in range(NBLK):
                    if b > 0:
                        P.op("pool", ("tensor_copy", dict(out=xbc[:, :, 0:3], in_=xbc[:, :, 512:515])), r=[xbc], w=[xbc])
                    for j in range(4):
                        i = b * 4 + j
                        xt = xts[i % 2]
                        P.dma("sp", xt[:], X2[i * 128:(i + 1) * 128, :], r=[X2t[i]], w=[xt])
                        norm_to_hT(xt, wb, hT, hT[:, :, j * 128:(j + 1) * 128], (junk, ssq, rstd, hb))
                    for j in range(4):
                        i = b * 4 + j
                        sz = szs[i % 2]
                        for n in range(4):
                            pz = ps1()
                            mm_group(P, pz, pz[:], [(hT[:, k, j * 128:(j + 1) * 128], w_in[:, k, n * 512:(n + 1) * 512]) for k in range(8)],
                                     r=[hT, w_in])
                            P.op("act", ("activation", dict(out=sz[:, n * 512:(n + 1) * 512], in_=pz[:], func=AF.Silu)), r=[pz], w=[sz])
                        P.dma("sp", ZS[i * 128:(i + 1) * 128, :], sz[:], r=[sz], w=[ZSt[i]])
                    for c in range(24):
                        pp = ps1()
                        mm_group(P, pp, pp[:], [(w_in[:, k, DIN + c * 128:DIN + (c + 1) * 128], hT[:, k, :]) for k in range(8)], r=[w_in, hT])
                        if c % 2 == 0:
                            P.op("act", ("copy", dict(out=xbc[:, c, 3:515], in_=pp[:])), r=[pp], w=[xbc])
                        else:
                            P.op("dve", ("tensor_copy", dict(out=xbc[:, c, 3:515], in_=pp[:])), r=[pp], w=[xbc])
                    for j in range(4):
                        i = b * 4 + j
                        sm = sms[i % 2]
                        csT = csTs[i % 2]
                        pd = ps1()
                        mm_group(P, pd, pd[:, 0:SSD_H], [(hT[:, k, j * 128:(j + 1) * 128], w_in[:, k, DIN + CONV:SSD_IN]) for k in range(8)],
                                 r=[hT, w_in])
                        P.op("dve", ("tensor_tensor", dict(out=sm[:, 0:32], in0=pd[:, 0:SSD_H], in1=dtb[:], op=ALU.add)), r=[pd, dtb], w=[sm])
                        P.op("act", ("activation", dict(out=sm[:, 0:32], in_=sm[:, 0:32], func=AF.Exp)), r=[sm], w=[sm])
                        P.op("act", ("activation", dict(out=sm[:, 0:32], in_=sm[:, 0:32], func=AF.Ln, bias=1.0, scale=1.0)), r=[sm], w=[sm])
                        P.op("dve", ("tensor_tensor", dict(out=da[:], in0=sm[:, 0:32], in1=ab[:], op=ALU.mult)), r=[sm, ab], w=[da])
                        pcs = ps1()
                        P.op("pe", ("matmul", dict(out=pcs[:, 0:SSD_H], lhsT=mask01[:], rhs=da[:], start=True, stop=True)),
                             r=[mask01, da], w=[pcs])
                        P.op("act", ("copy", dict(out=sm[:, 32:64], in_=pcs[:, 0:SSD_H])), r=[pcs], w=[sm])
                        P.op("act", ("activation", dict(out=sm[:, 64:96], in_=pcs[:, 0:SSD_H], func=AF.Exp)), r=[pcs], w=[sm])
                        pct = ps1()
                        P.op("pe", ("matmul", dict(out=pct[0:32, 0:128], lhsT=da[:], rhs=mask01[:], start=True, stop=True)),
                             r=[mask01, da], w=[pct])
                        P.op("act", ("copy", dict(out=csT[:], in_=pct[0:32, 0:128])), r=[pct], w=[csT])
                        P.dma("sp", CSD[i].rearrange("(h t) -> h t", t=128), csT[:], r=[csT], w=[CSDt[i]])
                        P.dma("sp", SMD[i * 128:(i + 1) * 128, :], sm[:], r=[sm], w=[SMDt[i]])
                    for c in range(24):
                        pp = ps1()
                        mm_group(P, pp, pp[:], [(diag[:, c, jj, :], xbc[:, c, jj:jj + 512]) for jj in range(4)], r=[diag, xbc])
                        dst_t = xcT if c < 16 else BCT
                        dst = xcT[:, c, :] if c < 16 else BCT[:, c - 16, :]
                        P.op("act", ("activation", dict(out=dst, in_=pp[:], func=AF.Silu, bias=cw[:, 96 + c:97 + c], scale=1.0)),
                             r=[pp, cw], w=[dst_t])
                    for j in range(4):
                        i = b * 4 + j
                        tk = slice(j * 128, (j + 1) * 128)
                        xtok = xtoks[i % 2]
                        btk = btks[i % 2]
                        bct = bcts[i % 2]
                        cbm = cbms[i % 2]
                        for q4 in range(2):
                            pt = ps1()
                            ptv = pt[:].bitcast(BF16).rearrange("p (k t) -> p k t", k=8)

                            def trx(e, ptv=ptv, q4=q4, tk=tk):
                                ins = None
                                for k in range(8):
                                    ins = e.transpose(out=ptv[:, k, :], in_=xcT[:, q4 * 8 + k, tk], identity=ident[:])
                                return ins
                            P.op("pe", trx, r=[xcT, ident], w=[pt])
                            if q4 == 0:
                                P.op("act", ("copy", dict(out=xtok[:, 0:1024].rearrange("p (k t) -> p k t", k=8), in_=ptv)), r=[pt], w=[xtok])
                            else:
                                P.op("dve", ("tensor_copy", dict(out=xtok[:, 1024:2048].rearrange("p (k t) -> p k t", k=8), in_=ptv)),
                                     r=[pt], w=[xtok])
                        pt = ps1()
                        ptv = pt[:].bitcast(BF16).rearrange("p (k t) -> p k t", k=8)

                        def trb(e, ptv=ptv, tk=tk):
                            ins = None
                            for g in range(4):
                                ins = e.transpose(out=ptv[:, g, :], in_=BCT[:, g, tk], identity=ident[:])
                            return ins
                        P.op("pe", trb, r=[BCT, ident], w=[pt])
                        P.op("act", ("copy", dict(out=btk[:], in_=ptv[:, 0:4, :])), r=[pt], w=[btk])
                        pcb = ps1()

                        def cbmm(e, pcb=pcb, tk=tk):
                            ins = None
                            for g in range(4):
                                ins = e.matmul(pcb[:, g * 128:(g + 1) * 128], lhsT=BCT[:, g, tk], rhs=BCT[:, 4 + g, tk], start=True, stop=True)
                            return ins
                        P.op("pe", cbmm, r=[BCT], w=[pcb])
                        P.op("dve", ("tensor_tensor", dict(out=cbm[:], in0=pcb[:].rearrange("p (g t) -> p g t", g=4),
                                                           in1=mask01[:].unsqueeze(1).broadcast_to([128, 4, 128]), op=ALU.mult)),
                             r=[pcb, mask01], w=[cbm])
                        P.dma("sp", XT[i * 128:(i + 1) * 128, :], xtok[:], r=[xtok], w=[XTt[i]])
                        P.dma("sp", BTK[i * 128:(i + 1) * 128, :], btk[:].rearrange("p g n -> p (g n)"), r=[btk], w=[BTKt[i]])
                        P.dma("sp", BCD[i].rearrange("p (g t) -> p g t", g=8), BCT[:, :, tk], r=[BCT], w=[BCDt[i]])
                        P.dma("sp", CBD[i], cbm[:].rearrange("p g t -> p (g t)"), r=[cbm], w=[CBDt[i]])

        def phase3m():
            P.barrier()
            with PhaseStack() as es3:
                def sb(name, shape, dt):
                    return T(es3.enter_context(nc.sbuf_tensor("pm_" + name, list(shape), dt)), name)
                NB3 = 3
                csBs = [sb("csB%d" % i, [128, SSD_H, 128], F32) for i in range(NB3)]
                sms = [sb("sm%d" % i, [128, 128], F32) for i in range(NB3)]
                cbms = [sb("cbm%d" % i, [128, 4, 128], F32) for i in range(NB3)]
                xtoks = [sb("xtok%d" % i, [128, DIN], BF16) for i in range(NB3)]
                xdts = [sb("xdt%d" % i, [128, DIN], BF16) for i in range(2)]
                xws = [sb("xw%d" % i, [128, DIN], BF16) for i in range(2)]
                wvs = [sb("wv%d" % i, [128, SSD_H], F32) for i in range(2)]
                LTs = [sb("LT%d" % i, [128, 8, 128], BF16) for i in range(2)]
                Mas = [sb("Ma%d" % i, [128, SSD_H, 128], BF16) for i in range(2)]

                def loads(i):
                    P.dma("sp", csBs[i % NB3][:].rearrange("p h t -> p (h t)"), CSD[i].partition_broadcast(128), r=[CSDt[i]],
                          w=[csBs[i % NB3]])
                    P.dma("sp", sms[i % NB3][:], SMD[i * 128:(i + 1) * 128, :], r=[SMDt[i]], w=[sms[i % NB3]])
                    P.dma("sp", cbms[i % NB3][:].rearrange("p g t -> p (g t)"), CBD[i], r=[CBDt[i]], w=[cbms[i % NB3]])
                    P.dma("sp", xtoks[i % NB3][:], XT[i * 128:(i + 1) * 128, :], r=[XTt[i]], w=[xtoks[i % NB3]])

                loads(0)
                if NT > 1:
                    loads(1)
                for i in range(NT):
                    if i + 2 < NT:
                        loads(i + 2)
                    csB, sm, cbm, xtok = csBs[i % NB3], sms[i % NB3], cbms[i % NB3], xtoks[i % NB3]
                    xdt, xw, wv, Ma = xdts[i % 2], xws[i % 2], wvs[i % 2], Mas[i % 2]
                    dt = sm[:, 0:32]
                    cs = sm[:, 32:64]
                    P.op("dve", ("tensor_tensor", dict(out=xdt[:].rearrange("p (h q) -> p h q", h=SSD_H),
                                                       in0=xtok[:].rearrange("p (h q) -> p h q", h=SSD_H),
                                                       in1=dt.unsqueeze(2).broadcast_to([128, SSD_H, 64]), op=ALU.mult)),
                         r=[xtok, sm], w=[xdt])
                    P.dma("act", XDT[i * 128:(i + 1) * 128, :], xdt[:], r=[xdt], w=[XDTt[i]])
                    P.op("dve", ("tensor_tensor", dict(out=wv[:], in0=csB[:, :, 127], in1=cs, op=ALU.subtract)), r=[csB, sm], w=[wv])
                    P.op("act", ("activation", dict(out=wv[:], in_=wv[:], func=AF.Exp)), r=[wv], w=[wv])
                    P.op("act", ("activation", dict(out=sm[:, 96:128], in_=csB[:, :, 127], func=AF.Exp)), r=[csB], w=[sm])
                    P.op("dve", ("tensor_tensor", dict(out=wv[:], in0=wv[:], in1=dt, op=ALU.mult)), r=[wv, sm], w=[wv])
                    P.op("pool", ("tensor_tensor", dict(out=xw[:].rearrange("p (h q) -> p h q", h=SSD_H),
                                                        in0=xtok[:].rearrange("p (h q) -> p h q", h=SSD_H),
                                                        in1=wv[:].unsqueeze(2).broadcast_to([128, SSD_H, 64]), op=ALU.mult)),
                         r=[xtok, wv], w=[xw])
                    P.dma("act", XWD[i * 128:(i + 1) * 128, :], xw[:], r=[xw], w=[XWDt[i]])
                    P.dma("act", SMD[i * 128:(i + 1) * 128, :], sm[:], r=[sm], w=[SMDt[i]])
                    for g in range(4):
                        LT = LTs[g % 2]
                        hs = slice(g * 8, (g + 1) * 8)

                        def dmin(e, g=g, csB=csB, cs=cs):
                            ins = None
                            for r8 in range(8):
                                h = g * 8 + r8
                                ins = e.tensor_scalar(out=csB[:, h, :], in0=csB[:, h, :], scalar1=cs[:, h:h + 1], scalar2=0.0,
                                                      op0=ALU.subtract, op1=ALU.min)
                            return ins
                        P.op("dve", dmin, r=[csB, sm, wv], w=[csB])
                        P.op("act", ("activation", dict(out=LT[:], in_=csB[:, hs, :], func=AF.Exp)), r=[csB], w=[LT])
                        P.op("dve", ("tensor_tensor", dict(out=Ma[:, hs, :], in0=LT[:],
                                                           in1=cbm[:, g, :].unsqueeze(1).broadcast_to([128, 8, 128]), op=ALU.mult)),
                             r=[LT, cbm], w=[Ma])
                    P.dma("act", MD[i], Ma[:].rearrange("p h t -> p (h t)"), r=[Ma], w=[MDt[i]])

        def phase3():
            P.barrier()
            with PhaseStack() as es3:
                def sb(name, shape, dt):
                    return T(es3.enter_context(nc.sbuf_tensor("p3_" + name, list(shape), dt)), name)
                w_out = sb("wout", [128, 16, D], BF16)
                for k in range(4):
                    P.dma("pool", w_out[:, k * 4:(k + 1) * 4, :],
                          IN("ssd_w_out")[k * 512:(k + 1) * 512, :].rearrange("(k p) n -> p k n", p=128), w=[w_out])
                nwb = sb("nwb", [128, DIN], BF16)
                P.dma("pool", nwb[:], IN("ssd_norm_w").partition_broadcast(128), w=[nwb])
                dsk = sb("dsk", [128, SSD_H], F32)
                P.dma("sp", dsk[:], IN("ssd_d").partition_broadcast(128), w=[dsk])
                Did = sb("Did", [128, SSD_H, 128], BF16)

                def mkdid(e):
                    ins = None
                    for h in range(SSD_H):
                        ins = e.tensor_scalar(out=Did[:, h, :], in0=ident_f[:], scalar1=dsk[:, h:h + 1], scalar2=None, op0=ALU.mult)
                    return ins
                P.op("dve", mkdid, r=[dsk, ident_f], w=[Did])
                NBUF = 3
                xt_ = [sb("xt%d" % i, [128, D], F32) for i in range(NBUF)]
                szs = [sb("sz%d" % i, [128, DIN], BF16) for i in range(NBUF)]
                xtoks = [sb("xtok%d" % i, [128, DIN], BF16) for i in range(NBUF)]
                bcts = [sb("bct%d" % i, [128, 8, 128], BF16) for i in range(NBUF)]
                btks = [sb("btk%d" % i, [128, 4, 128], BF16) for i in range(NBUF)]
                sms = [sb("sm%d" % i, [128, 128], F32) for i in range(NBUF)]
                Mts = [sb("Mt%d" % i, [128, SSD_H, 128], BF16) for i in range(NBUF)]
                xdts = [sb("xdt%d" % i, [128, DIN], BF16) for i in range(NBUF)]
                xws = [sb("xw%d" % i, [128, DIN], BF16) for i in range(NBUF)]
                t1 = [sb("t1_%d" % i, [128, 512], F32) for i in range(2)]
                t2 = [sb("t2_%d" % i, [128, 512], F32) for i in range(2)]
                gss = sb("gss", [128, 4], F32)
                grs = sb("grs", [128, 4], F32)
                ybs = [sb("yb%d" % i, [128, DIN], BF16) for i in range(2)]
                ybT = sb("ybT", [128, 16, 128], BF16)
                ST = sb("ST", [128, DIN], F32)
                STb = sb("STb", [128, DIN], BF16)
                P.op("pool", ("memset", dict(ap=ST[:], constant=0.0)), w=[ST])
                P.op("pool", ("memset", dict(ap=STb[:], constant=0.0)), w=[STb])

                def loads(i):
                    q = "act" if i % 2 == 0 else "sp"
                    P.dma("sp", xt_[i % NBUF][:], X2[i * 128:(i + 1) * 128, :], r=[X2t[i]], w=[xt_[i % NBUF]])
                    P.dma("sp", szs[i % NBUF][:], ZS[i * 128:(i + 1) * 128, :], r=[ZSt[i]], w=[szs[i % NBUF]])
                    P.dma("sp", xtoks[i % NBUF][:], XT[i * 128:(i + 1) * 128, :], r=[XTt[i]], w=[xtoks[i % NBUF]])
                    P.dma("sp", bcts[i % NBUF][:].rearrange("p g t -> p (g t)"), BCD[i], r=[BCDt[i]], w=[bcts[i % NBUF]])
                    P.dma("sp", btks[i % NBUF][:].rearrange("p g n -> p (g n)"), BTK[i * 128:(i + 1) * 128, :], r=[BTKt[i]], w=[btks[i % NBUF]])
                    P.dma("sp", sms[i % NBUF][:], SMD[i * 128:(i + 1) * 128, :], r=[SMDt[i]], w=[sms[i % NBUF]])
                    P.dma("sp", Mts[i % NBUF][:].rearrange("p h t -> p (h t)"), MD[i], r=[MDt[i]], w=[Mts[i % NBUF]])
                    P.dma("sp", xdts[i % NBUF][:], XDT[i * 128:(i + 1) * 128, :], r=[XDTt[i]], w=[xdts[i % NBUF]])
                    P.dma("sp", xws[i % NBUF][:], XWD[i * 128:(i + 1) * 128, :], r=[XWDt[i]], w=[xws[i % NBUF]])


                def bufs(i):
                    return dict(xt=xt_[i % NBUF], sz=szs[i % NBUF], xtok=xtoks[i % NBUF], BCT=bcts[i % NBUF], Btok=btks[i % NBUF],
                                sm=sms[i % NBUF], xdt=xdts[i % NBUF], xw=xws[i % NBUF], yb=ybs[i % 2], Mt=Mts[i % NBUF])

                def stageB(i):
                    B = bufs(i)
                    sm, xtok, xdt, sz, yb, BCT = B["sm"], B["xtok"], B["xdt"], B["sz"], B["yb"], B["BCT"]
                    ecs = sm[:, 64:96]
                    for g in range(4):
                        M = B["Mt"]
                        hs = slice(g * 8, (g + 1) * 8)
                        gsl = slice(g * 512, (g + 1) * 512)
                        pyi = psb[4 + (g % 2)]

                        def yimm(e, pyi=pyi, M=M, g=g, xdt=xdt, xtok=xtok):
                            ins = None
                            for r8 in range(8):
                                h = g * 8 + r8
                                e.matmul(pyi[:, r8 * 64:(r8 + 1) * 64], lhsT=M[:, h, :], rhs=xdt[:, h * 64:(h + 1) * 64],
                                         start=True, stop=False)
                                ins = e.matmul(pyi[:, r8 * 64:(r8 + 1) * 64], lhsT=Did[:, h, :], rhs=xtok[:, h * 64:(h + 1) * 64],
                                               start=False, stop=True)
                            return ins
                        P.op("pe", yimm, r=[M, xdt, Did, xtok], w=[pyi])
                        pyo = psb[6 + (g % 2)]
                        P.op("pe", ("matmul", dict(out=pyo[:], lhsT=BCT[:, 4 + g, :], rhs=STb[:, gsl], start=True, stop=True)),
                             r=[BCT, STb], w=[pyo])
                        a1 = t1[g % 2]
                        a2 = t2[g % 2]
                        P.op("dve", ("tensor_tensor", dict(out=a1[:].rearrange("p (h q) -> p h q", h=8),
                                                           in0=pyo[:].rearrange("p (h q) -> p h q", h=8),
                                                           in1=ecs[:, hs].unsqueeze(2).broadcast_to([128, 8, 64]), op=ALU.mult)),
                             r=[pyo, sm], w=[a1])
                        P.op("dve", ("tensor_tensor", dict(out=a1[:], in0=pyi[:], in1=a1[:], op=ALU.add)), r=[pyi, a1], w=[a1])
                        P.op("dve", ("tensor_tensor", dict(out=a1[:], in0=a1[:], in1=sz[:, gsl], op=ALU.mult)), r=[a1, sz], w=[a1])
                        P.op("act", ("activation", dict(out=a2[:], in_=a1[:], func=AF.Square, accum_out=gss[:, g:g + 1])),
                             r=[a1], w=[a2, gss])
                        P.op("dve", ("tensor_scalar", dict(out=grs[:, g:g + 1], in0=gss[:, g:g + 1], scalar1=1.0 / 512, scalar2=EPS,
                                                           op0=ALU.mult, op1=ALU.add)), r=[gss], w=[grs])
                        P.op("act", ("activation", dict(out=grs[:, g:g + 1], in_=grs[:, g:g + 1], func=AF.Sqrt)), r=[grs], w=[grs])
                        P.op("dve", ("reciprocal", dict(out=grs[:, g:g + 1], in_=grs[:, g:g + 1])), r=[grs], w=[grs])
                        P.op("dve", ("scalar_tensor_tensor", dict(out=yb[:, gsl], in0=a1[:], scalar=grs[:, g:g + 1], in1=nwb[:, gsl],
                                                                  op0=ALU.mult, op1=ALU.mult)), r=[a1, grs, nwb], w=[yb])

                def stageC(i):
                    B = bufs(i)
                    Btok, xw, sm = B["Btok"], B["xw"], B["sm"]
                    ecl = sm[:, 96:128]
                    for g in range(4):
                        gsl = slice(g * 512, (g + 1) * 512)
                        hs = slice(g * 8, (g + 1) * 8)
                        pst = ps1()
                        P.op("pe", ("matmul", dict(out=pst[:], lhsT=Btok[:, g, :], rhs=xw[:, gsl], start=True, stop=True)),
                             r=[Btok, xw], w=[pst])
                        P.op("dve", ("tensor_tensor", dict(out=ST[:, gsl].rearrange("p (h q) -> p h q", h=8),
                                                           in0=ST[:, gsl].rearrange("p (h q) -> p h q", h=8),
                                                           in1=ecl[:, hs].unsqueeze(2).broadcast_to([128, 8, 64]), op=ALU.mult)),
                             r=[ST, sm], w=[ST])
                        P.op("dve", ("tensor_tensor", dict(out=ST[:, gsl], in0=pst[:], in1=ST[:, gsl], op=ALU.add)), r=[pst, ST], w=[ST])
                        P.op("act", ("copy", dict(out=STb[:, gsl], in_=ST[:, gsl])), r=[ST], w=[STb])

                def stageD(i):
                    B = bufs(i)
                    xt, yb = B["xt"], B["yb"]
                    for q4 in range(2):
                        pt = ps1()
                        ptv = pt[:].bitcast(BF16).rearrange("p (k t) -> p k t", k=8)

                        def try_(e, ptv=ptv, q4=q4, yb=yb):
                            ins = None
                            for k in range(8):
                                c = q4 * 8 + k
                                ins = e.transpose(out=ptv[:, k, :], in_=yb[:, c * 128:(c + 1) * 128], identity=ident[:])
                            return ins
                        P.op("pe", try_, r=[yb, ident], w=[pt])
                        P.op("act", ("copy", dict(out=ybT[:, q4 * 8:(q4 + 1) * 8, :], in_=ptv)), r=[pt], w=[ybT])
                    for n in range(2):
                        py = ps1()
                        mm_group(P, py, py[:], [(ybT[:, c, :], w_out[:, c, n * 512:(n + 1) * 512]) for c in range(16)], r=[ybT, w_out])
                        P.op("dve", ("tensor_tensor", dict(out=xt[:, n * 512:(n + 1) * 512], in0=py[:], in1=xt[:, n * 512:(n + 1) * 512],
                                                           op=ALU.add)), r=[py, xt], w=[xt])
                    P.dma("sp", X3[i * 128:(i + 1) * 128, :], xt[:], r=[xt], w=[X3t[i]])

                loads(0)
                if NT > 1:
                    loads(1)
                for i in range(NT):
                    if i + 2 < NT:
                        loads(i + 2)
                    stageB(i)
                    stageC(i)
                    stageD(i)

        outT = [T(None, "out%d" % i) for i in range(NT // 2)]
        try:
            phase1()
            if PhaseStack.stopped or debug == "p1":
                raise StopBuild()
            ffn_phase("f0", [[(X1[i * 128:(i + 1) * 128, :], X1t[i])] for i in range(NT)], IN("ffn_norm_w")[0],
                      [(IN("ffn_w_gate"), IN("ffn_w_up"), IN("ffn_w_down"))], False,
                      lambda i: (X2[i * 128:(i + 1) * 128, :], X2t[i]), False)
            if PhaseStack.stopped or debug == "p2":
                raise StopBuild()
            phase3b()
            phase3m()
            phase3()
            if PhaseStack.stopped or debug == "p3":
                raise StopBuild()
            NH = NT // 2
            experts = [(IN("moe_w_gate")[e_], IN("moe_w_up")[e_], IN("moe_w_down")[e_]) for e_ in range(NE)]
            ffn_phase("m1", [[(X3[i * 128:(i + 1) * 128, :], X3t[i]), (X3[(NH + i) * 128:(NH + i + 1) * 128, :], X3t[NH + i])]
                             for i in range(NH)], IN("ffn_norm_w")[1], experts, True,
                      lambda i: (out[i * 128:(i + 1) * 128, :], outT[i]), True)
            P.final_wait("sp", outT)
        except StopBuild:
            pass
        if debug is not None:
            srcX, srcT = {"p1": (X1, X1t), "p2": (X2, X2t), "p3": (X3, X3t)}.get(debug, (X1, X1t))
            dts = [T(None, "dbg%d" % i) for i in range(NT * 4)]
            for i in range(NT):
                for q in range(4):
                    P.dma("sp", dbgt[:], srcX[i * 128:(i + 1) * 128, q * 256:(q + 1) * 256], r=[srcT[i]], w=[dbgt])
                    P.dma("sp", dbg[i * 128:(i + 1) * 128, q * 256:(q + 1) * 256], dbgt[:], r=[dbgt], w=[dts[i * 4 + q]])
            P.final_wait("sp", dts)
        P.emit()
    nc._in_names = list(_ins.keys())
    return nc


_CACHE = {}

SEQ = 8192
NCORES = 8


def kernel(**inputs):
    x = np.asarray(inputs["x"], dtype=np.float32)
    B = x.shape[0]
    L = x.shape[1]
    if L not in _CACHE:
        _CACHE[L] = build_program(L)
    nc = _CACHE[L]
    sq = {}
    for k, v in inputs.items():
        if k == "x":
            continue
        v = np.asarray(v, dtype=np.float32)
        if k in ("mix_norm_w", "ffn_norm_w", "final_norm_w", "hg_lb_logits"):
            sq[k] = np.ascontiguousarray(v)
        else:
            sq[k] = np.ascontiguousarray(v[0])
    in_maps = []
    for c in range(NCORES):
        b, half = c // 2, c % 2
        m = {"x": np.ascontiguousarray(x[b]), "sel": np.array([1.0 - half, float(half)], np.float32)}
        for k in nc._in_names:
            if k not in m:
                m[k] = sq[k]
        in_maps.append({k: m[k] for k in nc._in_names})
    res = run_bass_kernel_spmd(nc, in_maps, core_ids=list(range(NCORES)))
    outp = np.empty((B, L, D), np.float32)
    LH = L // 2
    for c in range(NCORES):
        b, half = c // 2, c % 2
        outp[b, half * LH:(half + 1) * LH] = res.results[c]["out"]
    return outp
```

```python
import numpy as np
from contextlib import ExitStack
import concourse.bass as bass
import concourse.mybir as mybir
from concourse.bass_utils import run_bass_kernel_spmd

F32, BF16 = mybir.dt.float32, mybir.dt.bfloat16
AF = mybir.ActivationFunctionType
ALU = mybir.AluOpType
AX = mybir.AxisListType

D = 1024
EPS = 1e-6
HG_H = 8
DFF = 2816
NFC = DFF // 128
NE = 8
DIN = 2048
SSD_H = 32
NG = 4
SSD_IN = 5152
CONV = 3072
SELF_SYNC = True


class T:
    def __init__(self, t, name=""):
        self.t = t
        self.name = name
        self.lw = None
        self.rd = {}

    def __getitem__(self, idx):
        return self.t[idx]


class Stream:
    def __init__(self, name):
        self.name = name
        self.ops = []
        self.seen = {}
        self.cnt = 0
        self.dcnt = None
        self.di = 0


class Prog:
    NDS = 12

    def __init__(self, nc, es):
        self.nc = nc
        self.es = es
        self.sems = {}
        self.streams = {}
        for n in ["pe", "act", "dve", "pool", "sp"]:
            st = Stream(n)
            self.streams[n] = st
            self.sems[n] = es.enter_context(nc.semaphore("s_" + n))
        for n in ["pool", "sp", "act"]:
            st = self.streams[n]
            st.dcnt = [0] * self.NDS
            for i in range(self.NDS):
                self.sems[(n, i)] = es.enter_context(nc.semaphore("d_%s%d" % (n, i)))

    def sb(self, name, shape, dt):
        return T(self.es.enter_context(self.nc.sbuf_tensor(name, list(shape), dt)), name)

    def ps(self, name, shape, dt):
        return T(self.es.enter_context(self.nc.psum_tensor(name, list(shape), dt)), name)

    def op(self, s, fn, r=(), w=(), dma=False):
        st = self.streams[s]
        waits = {}

        def addw(ev, raw):
            if ev is None:
                return
            key, val, owner, is_dma = ev
            if owner == s and not is_dma:
                if s == "pe" or not SELF_SYNC:
                    return
            if st.seen.get(key, 0) >= val:
                return
            if waits.get(key, 0) < val:
                waits[key] = val

        for t in r:
            addw(t.lw, True)
        for t in w:
            addw(t.lw, False)
            for key, (val, owner, is_dma) in t.rd.items():
                addw((key, val, owner, is_dma), False)
        if dma:
            i = st.di % self.NDS
            st.di += 1
            if st.dcnt[i] > 0 and st.seen.get((s, i), 0) < st.dcnt[i]:
                waits[(s, i)] = st.dcnt[i]
        for key, val in waits.items():
            st.seen[key] = val
        if dma:
            st.dcnt[i] += 16
            key = (s, i)
            ev = (key, st.dcnt[i], s, True)
            inc = 16
        else:
            st.cnt += 1
            key = s
            ev = (key, st.cnt, s, False)
            inc = 1
        st.ops.append((list(waits.items()), fn, key, inc))
        for t in w:
            t.lw = ev
            t.rd = {}
        for t in r:
            if t.lw is ev:
                continue
            old = t.rd.get(key)
            if old is None or old[0] < ev[1]:
                t.rd[key] = (ev[1], s, ev[3])
        return ev

    def dma(self, q, out, in_, r=(), w=(), **kw):
        return self.op(q, ("dma_start", dict(out=out, in_=in_, **kw)), r=r, w=w, dma=True)

    def barrier(self):
        targets = {}
        for n, st in self.streams.items():
            if st.cnt > 0:
                targets[n] = st.cnt
            if st.dcnt is not None:
                for i, v in enumerate(st.dcnt):
                    if v > 0:
                        targets[(n, i)] = v
        for n, st in self.streams.items():
            waits = {}
            for key, val in targets.items():
                if key == n and n == "pe":
                    continue
                if st.seen.get(key, 0) < val:
                    waits[key] = val
                    st.seen[key] = val
            st.ops.append((list(waits.items()), None, None, 0))

    def final_wait(self, s, tiles):
        st = self.streams[s]
        waits = {}
        for t in tiles:
            ev = t.lw
            if ev is not None and waits.get(ev[0], 0) < ev[1]:
                waits[ev[0]] = ev[1]
        st.ops.append((list(waits.items()), None, None, 0))

    def emit(self):
        nc = self.nc
        names = {"pe": "tensor", "act": "scalar", "dve": "vector", "pool": "gpsimd", "sp": "sync"}
        with nc.Block() as block:
            for s, attr in names.items():
                st = self.streams[s]

                def body(e, st=st):
                    for waits, fn, key, inc in st.ops:
                        for k, v in waits:
                            e.wait_ge(self.sems[k], v)
                        if fn is None:
                            continue
                        if isinstance(fn, tuple):
                            ins = getattr(e, fn[0])(**fn[1])
                        else:
                            ins = fn(e)
                        ins.then_inc(self.sems[key], inc)

                getattr(block, attr)(body)


def mm_group(P, out_t, out_ap, pairs, r):
    def fn(e):
        n = len(pairs)
        ins = None
        for i, (l, rr) in enumerate(pairs):
            ins = e.matmul(out_ap, lhsT=l, rhs=rr, start=(i == 0), stop=(i == n - 1))
        return ins

    return P.op("pe", fn, r=r, w=[out_t])


class StopBuild(Exception):
    pass


class PhaseStack(ExitStack):
    stopped = False

    def __exit__(self, et, ev, tb):
        if et is StopBuild:
            PhaseStack.stopped = True
            super().__exit__(None, None, None)
            return True
        return super().__exit__(et, ev, tb)


def build_program(L, debug=None):
    nc = bass.Bass("TRN2", target_bir_lowering=False)
    es = ExitStack()
    PhaseStack.stopped = False
    NT = L // 128
    NBLK = L // 512
    LH = L // 2

    _ins = {}
    SHAPES = {
        "x": [L, D], "sel": [2], "mix_norm_w": [2, D], "ffn_norm_w": [2, D], "final_norm_w": [D],
        "hg_w_in": [D, 4096], "hg_lb_logits": [2, D], "hg_norm_w": [128], "hg_w_out": [D, D],
        "ssd_w_in": [D, SSD_IN], "ssd_conv_w": [4, CONV], "ssd_conv_b": [CONV], "ssd_dt_bias": [SSD_H],
        "ssd_a_log": [SSD_H], "ssd_d": [SSD_H], "ssd_norm_w": [DIN], "ssd_w_out": [DIN, D],
        "ffn_w_gate": [D, DFF], "ffn_w_up": [D, DFF], "ffn_w_down": [DFF, D], "moe_router": [D, NE],
        "moe_w_gate": [NE, D, DFF], "moe_w_up": [NE, D, DFF], "moe_w_down": [NE, DFF, D],
    }

    def IN(name):
        if name not in _ins:
            _ins[name] = nc.dram_tensor(name, list(SHAPES[name]), F32, kind="ExternalInput").ap()
        return _ins[name]

    out = nc.dram_tensor("out", [LH, D], F32, kind="ExternalOutput").ap()
    dbg = None
    if debug is not None:
        dbg = nc.dram_tensor("dbg", [L, D], F32, kind="ExternalOutput").ap()

    X1 = nc.dram_tensor("X1", [L, D], F32, kind="Internal").ap()
    X2 = nc.dram_tensor("X2", [L, D], F32, kind="Internal").ap()
    X3 = nc.dram_tensor("X3", [L, D], F32, kind="Internal").ap()
    X1t = [T(None, "X1_%d" % i) for i in range(NT)]
    X2t = [T(None, "X2_%d" % i) for i in range(NT)]
    X3t = [T(None, "X3_%d" % i) for i in range(NT)]

    P = Prog(nc, es)
    with es:
        if debug is not None:
            dbgt = P.sb("dbgt", [128, 256], F32)
        ident_f = P.sb("ident_f", [128, 128], F32)
        ident = P.sb("ident", [128, 128], BF16)
        ones_f = P.sb("ones_f", [128, 128], F32)
        mask01 = P.sb("mask01", [128, 128], F32)
        P.op("pool", ("memset", dict(ap=ones_f[:], constant=1.0)), w=[ones_f])
        P.op("pool", ("affine_select", dict(out=ident_f[:], in_=ones_f[:], pattern=[[1, 128]],
                                               compare_op=ALU.is_equal, fill=0.0, base=0,
                                               channel_multiplier=-1)), r=[ones_f], w=[ident_f])
        P.op("pool", ("affine_select", dict(out=mask01[:], in_=ones_f[:], pattern=[[1, 128]],
                                               compare_op=ALU.is_ge, fill=0.0, base=0,
                                               channel_multiplier=-1)), r=[ones_f], w=[mask01])
        P.op("dve", ("tensor_copy", dict(out=ident[:], in_=ident_f[:])), r=[ident_f], w=[ident])

        psb = [P.ps("psb%d" % i, [128, 512], F32) for i in range(8)]
        ps_rr = [0]

        def ps1():
            t = psb[ps_rr[0] % 4]
            ps_rr[0] += 1
            return t

        def norm_to_hT(xt, wb, hT_t, hT_ap, tmp, eng_copy="act"):
            junk, ssq, rstd, hb = tmp
            P.op("act", ("activation", dict(out=junk[:], in_=xt[:], func=AF.Square, accum_out=ssq[:])),
                 r=[xt], w=[junk, ssq])
            P.op("dve", ("tensor_scalar", dict(out=rstd[:], in0=ssq[:], scalar1=1.0 / D, scalar2=EPS,
                                                  op0=ALU.mult, op1=ALU.add)), r=[ssq], w=[rstd])
            P.op("act", ("activation", dict(out=rstd[:], in_=rstd[:], func=AF.Ln)), r=[rstd], w=[rstd])
            P.op("act", ("activation", dict(out=rstd[:], in_=rstd[:], func=AF.Exp, scale=-0.5)), r=[rstd], w=[rstd])
            P.op("dve", ("scalar_tensor_tensor", dict(out=hb[:], in0=xt[:], scalar=rstd[:], in1=wb[:],
                                                         op0=ALU.mult, op1=ALU.mult)), r=[xt, rstd, wb], w=[hb])
            pt = ps1()
            ptv = pt[:].bitcast(BF16).rearrange("p (k t) -> p k t", k=8)

            def tr(e):
                ins = None
                for k in range(8):
                    ins = e.transpose(out=ptv[:, k, :], in_=hb[:, k * 128:(k + 1) * 128], identity=ident[:])
                return ins
            P.op("pe", tr, r=[hb, ident], w=[pt])
            if eng_copy == "act":
                P.op("act", ("copy", dict(out=hT_ap, in_=ptv)), r=[pt], w=[hT_t])
            else:
                P.op("dve", ("tensor_copy", dict(out=hT_ap, in_=ptv)), r=[pt], w=[hT_t])

        def phase1():
            with PhaseStack() as es1:
                def sb(name, shape, dt):
                    return T(es1.enter_context(nc.sbuf_tensor(name, list(shape), dt)), name)
                w_in = sb("hg_win", [128, 8, 4096], BF16)
                w_out = sb("hg_wout", [128, 8, 1024], BF16)
                for k in range(8):
                    P.dma("pool", w_in[:, k, :], IN("hg_w_in")[k * 128:(k + 1) * 128, :], w=[w_in])
                P.dma("pool", w_out[:], IN("hg_w_out").rearrange("(k p) n -> p k n", p=128), w=[w_out])
                wb = sb("p1_wb", [128, D], F32)
                P.dma("sp", wb[:], IN("mix_norm_w")[0].partition_broadcast(128), w=[wb])
                gw = sb("p1_gw", [128, 128], F32)
                P.dma("sp", gw[:], IN("hg_norm_w").partition_broadcast(128), w=[gw])
                lbl = sb("p1_lbl", [128, 2, 8], F32)
                P.dma("sp", lbl[:], IN("hg_lb_logits").rearrange("r (h p) -> p r h", p=128), w=[lbl],
                      allow_slow_non_contiguous=True)
                lb = sb("p1_lb", [128, 8], F32)
                oml = sb("p1_oml", [128, 8], F32)
                noml = sb("p1_noml", [128, 8], F32)
                P.op("dve", ("tensor_tensor", dict(out=lb[:], in0=lbl[:, 0, :], in1=lbl[:, 1, :], op=ALU.subtract)),
                     r=[lbl], w=[lb])
                P.op("act", ("activation", dict(out=lb[:], in_=lb[:], func=AF.Sigmoid)), r=[lb], w=[lb])
                P.op("dve", ("tensor_scalar", dict(out=oml[:], in0=lb[:], scalar1=-1.0, scalar2=1.0,
                                                      op0=ALU.mult, op1=ALU.add)), r=[lb], w=[oml])
                P.op("dve", ("tensor_scalar", dict(out=noml[:], in0=lb[:], scalar1=-1.0, scalar2=None,
                                                      op0=ALU.add)), r=[lb], w=[noml])

                xts = [sb("p1_xt%d" % i, [128, D], F32) for i in range(3)]
                junk = sb("p1_junk", [128, D], BF16)
                ssq = sb("p1_ssq", [128, 1], F32)
                rstd = sb("p1_rstd", [128, 1], F32)
                hb = sb("p1_hb", [128, D], BF16)
                hT = sb("p1_hT", [128, 8, 512], BF16)
                QT = sb("p1_QT", [128, 8, 512], BF16)
                KT = sb("p1_KT", [128, 8, 512], BF16)
                vblk = sb("p1_v", [128, 4, D], BF16)
                sgblk = sb("p1_sg", [128, 4, D], BF16)
                sig_ = [sb("p1_sig%d" % i, [128, 512], F32) for i in range(2)]
                kk_ = [sb("p1_kk%d" % i, [128, 512], F32) for i in range(2)]
                ff_ = [sb("p1_ff%d" % i, [128, 512], F32) for i in range(2)]
                bcum_ = [sb("p1_bcum%d" % i, [128, 512], F32) for i in range(2)]
                Ep_ = [sb("p1_Ep%d" % i, [128, 512], F32) for i in range(2)]
                Em_ = [sb("p1_Em%d" % i, [128, 512], F32) for i in range(2)]
                negm_ = [sb("p1_negm%d" % i, [128, 4], F32) for i in range(2)]
                qsb_ = [sb("p1_qsb%d" % i, [128, 512], F32) for i in range(2)]
                e2b = sb("p1_e2", [128, 4, 8], F32)
                e3b = sb("p1_e3", [128, 4, 8], F32)
                emb = sb("p1_em", [128, 4, 8], F32)
                S = sb("p1_S", [128, 8, 128], F32)
                S3 = sb("p1_S3", [128, 8, 128], F32)
                Sp_ = [sb("p1_Sp%d" % i, [128, 8, 128], BF16) for i in range(2)]
                Ktok_ = [sb("p1_Ktok%d" % i, [128, 8, 128], BF16) for i in range(2)]
                attn_ = [sb("p1_attn%d" % i, [128, 8, 128], BF16) for i in range(2)]
                oss = sb("p1_oss", [128, 8], F32)
                orstd = sb("p1_orstd", [128, 8], F32)
                on = sb("p1_on", [128, D], F32)
                osq = on
                ob_ = [sb("p1_ob%d" % i, [128, D], BF16) for i in range(2)]
                obT_ = [sb("p1_obT0", [128, 8, 128], BF16)] * 2
                x1 = [sb("p1_x1_0", [128, D], F32)] * 2
                P.op("pool", ("memset", dict(ap=S[:], constant=0.0)), w=[S])
                P.op("dve", ("memset", dict(ap=psb[4][:], constant=0.0)), w=[psb[4]])
                P.op("dve", ("memset", dict(ap=psb[5][:], constant=0.0)), w=[psb[5]])

                if debug == "s1":
                    raise StopBuild()

                def load_x(i):
                    P.dma("sp", xts[i % 3][:], IN("x")[i * 128:(i + 1) * 128, :], w=[xts[i % 3]])

                for i in range(min(2, NT)):
                    load_x(i)
                for b in range(NBLK):
                    for j in range(4):
                        i = b * 4 + j
                        if i + 2 < NT:
                            load_x(i + 2)
                        norm_to_hT(xts[i % 3], wb, hT, hT[:, :, j * 128:(j + 1) * 128], (junk, ssq, rstd, hb))
                    if debug == "s2":
                        raise StopBuild()
                    def vg_task(tix, b=b):
                        which, rem = divmod(tix, 8)
                        j, n = divmod(rem, 2)
                        pp = psb[6 + (tix % 2)]
                        col = 2048 + which * 1024 + n * 512
                        mm_group(P, pp, pp[:], [(hT[:, k, j * 128:(j + 1) * 128], w_in[:, k, col:col + 512])
                                                for k in range(8)], r=[w_in, hT])
                        if which == 0:
                            P.op("act", ("copy", dict(out=vblk[:, j, n * 512:(n + 1) * 512], in_=pp[:])), r=[pp], w=[vblk])
                        else:
                            P.op("act", ("activation", dict(out=sgblk[:, j, n * 512:(n + 1) * 512], in_=pp[:], func=AF.Sigmoid)),
                                 r=[pp], w=[sgblk])
                    for hd in range(8):
                        sig, kk, ff, bcum, Ep, Em, negm = (sig_[hd % 2], kk_[hd % 2], ff_[hd % 2], bcum_[hd % 2], Ep_[hd % 2],
                                                          Em_[hd % 2], negm_[hd % 2])
                        qp = ps1()
                        mm_group(P, qp, qp[:], [(w_in[:, k, hd * 128:(hd + 1) * 128], hT[:, k, :]) for k in range(8)],
                                 r=[w_in, hT])
                        qsb = qsb_[hd % 2]
                        P.op("act", ("copy", dict(out=qsb[:], in_=qp[:])), r=[qp], w=[qsb])
                        fp = ps1()
                        mm_group(P, fp, fp[:], [(w_in[:, k, 1024 + hd * 128:1024 + (hd + 1) * 128], hT[:, k, :])
                                                for k in range(8)], r=[w_in, hT])
                        P.op("act", ("activation", dict(out=sig[:], in_=fp[:], func=AF.Exp, scale=-1.0)), r=[fp], w=[sig])
                        P.op("act", ("activation", dict(out=kk[:], in_=sig[:], func=AF.Ln, bias=1.0, scale=1.0)), r=[sig], w=[kk])
                        P.op("act", ("activation", dict(out=ff[:], in_=sig[:], func=AF.Ln, bias=1.0, scale=lb[:, hd:hd + 1])),
                             r=[sig, lb], w=[ff])
                        P.op("dve", ("tensor_tensor", dict(out=ff[:], in0=ff[:], in1=kk[:], op=ALU.subtract)), r=[ff, kk], w=[ff])
                        P.op("act", ("activation", dict(out=kk[:], in_=ff[:], func=AF.Exp)), r=[ff], w=[kk])
                        P.op("dve", ("tensor_scalar", dict(out=kk[:], in0=kk[:], scalar1=-1.0, scalar2=1.0, op0=ALU.mult, op1=ALU.add)),
                             r=[kk], w=[kk])
                        vg_task(hd)
                        def scans(e, bcum=bcum, ff=ff):
                            ins = None
                            for c in range(4):
                                ins = e.tensor_tensor_scan(out=bcum[:, c * 128:(c + 1) * 128], data0=ones_f[:],
                                                           data1=ff[:, c * 128:(c + 1) * 128], initial=0.0,
                                                           op0=ALU.mult, op1=ALU.add)
                            return ins
                        P.op("dve", scans, r=[ff, ones_f], w=[bcum])
                        bc3 = bcum[:].rearrange("p (c t) -> p c t", c=4)
                        P.op("dve", ("tensor_scalar", dict(out=negm[:], in0=bc3[:, :, 63], scalar1=-1.0,
                                                                      scalar2=None, op0=ALU.mult)), r=[bcum], w=[negm])

                        def exps(e, hd=hd, bc3=bc3, bcum=bcum, Ep=Ep, Em=Em, negm=negm):
                            for c in range(4):
                                e.activation(out=Ep[:, c * 128:(c + 1) * 128], in_=bcum[:, c * 128:(c + 1) * 128],
                                             func=AF.Exp, bias=negm[:, c:c + 1], scale=1.0)
                                e.activation(out=Em[:, c * 128:(c + 1) * 128], in_=bcum[:, c * 128:(c + 1) * 128],
                                             func=AF.Exp, bias=bcum[:, c * 128 + 63:c * 128 + 64], scale=-1.0)
                            e.activation(out=e3b[:, :, hd], in_=bc3[:, :, 127], func=AF.Exp)
                            return e.activation(out=emb[:, :, hd], in_=bc3[:, :, 63], func=AF.Exp)
                        P.op("act", exps, r=[bcum, negm], w=[Ep, Em, e3b, emb])
                        Ep3 = Ep[:].rearrange("p (c t) -> p c t", c=4)
                        P.op("dve", ("tensor_copy", dict(out=e2b[:, :, hd], in_=Ep3[:, :, 127])),
                             r=[Ep], w=[e2b])
                        P.op("dve", ("tensor_tensor", dict(out=QT[:, hd, :], in0=qsb[:], in1=Ep[:], op=ALU.mult)),
                             r=[qsb, Ep], w=[QT])
                        P.op("dve", ("tensor_tensor", dict(out=KT[:, hd, :], in0=kk[:], in1=Em[:], op=ALU.mult)),
                             r=[kk, Em], w=[KT])
                    if debug == "s4":
                        raise StopBuild()
                    for tix in range(8, 16):
                        vg_task(tix)
                    pa = [psb[4], psb[5]]
                    po = [psb[6], psb[7]]

                    def c_front(c, b=b):
                        i = b * 4 + c
                        tok = slice(c * 128, (c + 1) * 128)
                        Ktok, attn, Sp, ob, obT = Ktok_[i % 2], attn_[i % 2], Sp_[i % 2], ob_[i % 2], obT_[i % 2]
                        pk = ps1()
                        pkv = pk[:].bitcast(BF16).rearrange("p (k t) -> p k t", k=8)

                        def trk(e, pkv=pkv, tok=tok):
                            ins = None
                            for hd in range(8):
                                ins = e.transpose(out=pkv[:, hd, :], in_=KT[:, hd, tok], identity=ident[:])
                            return ins
                        P.op("pe", trk, r=[KT, ident], w=[pk])
                        P.op("act", ("copy", dict(out=Ktok[:], in_=pkv)), r=[pk], w=[Ktok])
                        pa = [psb[4], psb[5]]
                        for half in range(2):
                            def att(e, half=half, tok=tok):
                                ins = None
                                for q4 in range(4):
                                    hd = half * 4 + q4
                                    t0 = tok.start
                                    e.matmul(pa[half][:, q4 * 128 + 64:(q4 + 1) * 128], lhsT=KT[:, hd, tok], rhs=QT[:, hd, t0 + 64:t0 + 128],
                                             start=True, stop=True)
                                    ins = e.matmul(pa[half][0:64, q4 * 128:q4 * 128 + 64], lhsT=KT[:, hd, t0:t0 + 64],
                                                   rhs=QT[:, hd, t0:t0 + 64], start=True, stop=True)
                                return ins
                            P.op("pe", att, r=[KT, QT], w=[pa[half]])
                            P.op("dve", ("tensor_tensor", dict(
                                out=attn[:, half * 4:(half + 1) * 4, :],
                                in0=pa[half][:].rearrange("p (h t) -> p h t", h=4),
                                in1=mask01[:].unsqueeze(1).broadcast_to([128, 4, 128]), op=ALU.mult)),
                                r=[pa[half], mask01], w=[attn])
                    def c_mid(c, b=b):
                        i = b * 4 + c
                        tok = slice(c * 128, (c + 1) * 128)
                        Ktok, attn, Sp, ob, obT = Ktok_[i % 2], attn_[i % 2], Sp_[i % 2], ob_[i % 2], obT_[i % 2]
                        P.op("dve", ("tensor_tensor", dict(out=Sp[:], in0=S[:],
                                                                  in1=emb[:, c, :].unsqueeze(2).broadcast_to([128, 8, 128]),
                                                                  op=ALU.mult)), r=[S, emb], w=[Sp])
                        po = [psb[6], psb[7]]
                        for half in range(2):
                            def omm(e, half=half, tok=tok, c=c, attn=attn, Sp=Sp):
                                ins = None
                                for q4 in range(4):
                                    hd = half * 4 + q4
                                    e.matmul(po[half][:, q4 * 128:(q4 + 1) * 128], lhsT=attn[:, hd, :],
                                             rhs=vblk[:, c, hd * 128:(hd + 1) * 128], start=True, stop=False)
                                    ins = e.matmul(po[half][:, q4 * 128:(q4 + 1) * 128], lhsT=QT[:, hd, tok],
                                                   rhs=Sp[:, hd, :], start=False, stop=True)
                                return ins
                            P.op("pe", omm, r=[attn, vblk, QT, Sp], w=[po[half]])
                        pp2 = [ps1(), ps1()]
                        for half in range(2):
                            def pmm(e, half=half, c=c, pp2=pp2, Ktok=Ktok):
                                ins = None
                                for q4 in range(4):
                                    hd = half * 4 + q4
                                    ins = e.matmul(pp2[half][:, q4 * 128:(q4 + 1) * 128], lhsT=Ktok[:, hd, :],
                                                   rhs=vblk[:, c, hd * 128:(hd + 1) * 128], start=True, stop=True)
                                return ins
                            P.op("pe", pmm, r=[Ktok, vblk], w=[pp2[half]])
                        P.op("dve", ("tensor_tensor", dict(out=S3[:], in0=S[:],
                                                                   in1=e3b[:, c, :].unsqueeze(2).broadcast_to([128, 8, 128]),
                                                                   op=ALU.mult)), r=[S, e3b], w=[S3])
                        for half in range(2):
                            P.op("dve", ("tensor_tensor", dict(
                                out=S[:, half * 4:(half + 1) * 4, :],
                                in0=pp2[half][:].rearrange("p (h v) -> p h v", h=4),
                                in1=e2b[:, c, half * 4:(half + 1) * 4].unsqueeze(2).broadcast_to([128, 4, 128]),
                                op=ALU.mult)), r=[pp2[half], e2b], w=[S])
                        P.op("dve", ("tensor_tensor", dict(out=S[:], in0=S[:], in1=S3[:], op=ALU.add)), r=[S, S3], w=[S])
                    def c_tail(c, b=b):
                        i = b * 4 + c
                        tok = slice(c * 128, (c + 1) * 128)
                        Ktok, attn, Sp, ob, obT = Ktok_[i % 2], attn_[i % 2], Sp_[i % 2], ob_[i % 2], obT_[i % 2]
                        for half in range(2):
                            P.op("act", ("activation", dict(out=osq[:, half * 512:(half + 1) * 512], in_=po[half][:],
                                                                         func=AF.Square)), r=[po[half]], w=[osq])
                        P.op("dve", ("tensor_reduce", dict(out=oss[:], in_=osq[:].rearrange("p (h v) -> p h v", h=8),
                                                              axis=AX.X, op=ALU.add)), r=[osq], w=[oss])
                        P.op("dve", ("tensor_scalar", dict(out=orstd[:], in0=oss[:], scalar1=1.0 / 128, scalar2=EPS,
                                                              op0=ALU.mult, op1=ALU.add)), r=[oss], w=[orstd])
                        P.op("act", ("activation", dict(out=orstd[:], in_=orstd[:], func=AF.Ln)), r=[orstd], w=[orstd])
                        P.op("act", ("activation", dict(out=orstd[:], in_=orstd[:], func=AF.Exp, scale=-0.5)), r=[orstd], w=[orstd])
                        for half in range(2):
                            P.op("dve", ("tensor_tensor", dict(
                                out=on[:, half * 512:(half + 1) * 512].rearrange("p (h v) -> p h v", h=4),
                                in0=po[half][:].rearrange("p (h v) -> p h v", h=4),
                                in1=orstd[:, half * 4:(half + 1) * 4].unsqueeze(2).broadcast_to([128, 4, 128]),
                                op=ALU.mult)), r=[po[half], orstd], w=[on])
                        P.op("dve", ("tensor_tensor", dict(out=on[:].rearrange("p (h v) -> p h v", h=8),
                                                               in0=on[:].rearrange("p (h v) -> p h v", h=8),
                                                               in1=gw[:].unsqueeze(1).broadcast_to([128, 8, 128]),
                                                               op=ALU.mult)), r=[on, gw], w=[on])
                        P.op("dve", ("tensor_tensor", dict(out=ob[:], in0=on[:], in1=sgblk[:, c, :], op=ALU.mult)),
                             r=[on, sgblk], w=[ob])
                        pt = ps1()
                        ptv = pt[:].bitcast(BF16).rearrange("p (k t) -> p k t", k=8)

                        def tro(e, ptv=ptv, ob=ob):
                            ins = None
                            for hd in range(8):
                                ins = e.transpose(out=ptv[:, hd, :], in_=ob[:, hd * 128:(hd + 1) * 128], identity=ident[:])
                            return ins
                        P.op("pe", tro, r=[ob, ident], w=[pt])
                        P.op("act", ("copy", dict(out=obT[:], in_=ptv)), r=[pt], w=[obT])
                        xo = x1[i % 2]
                        P.dma("sp", xo[:], IN("x")[i * 128:(i + 1) * 128, :], w=[xo])
                        for n in range(2):
                            py = ps1()
                            mm_group(P, py, py[:], [(obT[:, hd, :], w_out[:, hd, n * 512:(n + 1) * 512]) for hd in range(8)],
                                     r=[obT, w_out])
                            P.op("dve", ("tensor_tensor", dict(
                                out=xo[:, n * 512:(n + 1) * 512], in0=py[:], in1=xo[:, n * 512:(n + 1) * 512], op=ALU.add)),
                                r=[py, xo], w=[xo])
                        P.dma("sp", X1[i * 128:(i + 1) * 128, :], xo[:], r=[xo], w=[X1t[i]])

                    c_front(0)
                    for c in range(4):
                        c_mid(c)
                        if c + 1 < 4:
                            c_front(c + 1)
                        c_tail(c)

        def ffn_phase(tag, src_tiles, norm_w_row, experts, moe, dst_fn, final):
            NTT = len(src_tiles)
            P.barrier()
            PASS = min(16, NTT)
            with PhaseStack() as es2:
                def sb(name, shape, dt):
                    return T(es2.enter_context(nc.sbuf_tensor(tag + name, list(shape), dt)), name)
                wb = sb("wb", [128, D], F32)
                P.dma("sp", wb[:], norm_w_row.partition_broadcast(128), w=[wb])
                acc = [sb("acc%d" % i, [128, D], F32) for i in range(PASS)]
                hT = sb("hT", [128, 8, PASS * 128], BF16)
                junk = sb("junk", [128, D], BF16)
                ssq = sb("ssq", [128, 1], F32)
                rstd = sb("rstd", [128, 1], F32)
                hb = sb("hb", [128, D], BF16)
                wgs = [sb("wg%d" % i, [128, 8, 256], BF16) for i in range(2)]
                wus = [sb("wu%d" % i, [128, 8, 256], BF16) for i in range(2)]
                wds = [sb("wd%d" % i, [128, 2, D], BF16) for i in range(2)]
                sgl = [sb("sgl%d" % i, [128, 512], F32) for i in range(2)]
                Ab = [sb("A%d" % i, [128, 2, 512], BF16) for i in range(2)]
                if moe:
                    sel = sb("sel", [128, 2], F32)
                    P.dma("sp", sel[:], IN("sel").partition_broadcast(128), w=[sel])
                    xbs = [sb("xb%d" % i, [128, D], F32) for i in range(2)]
                    wr = sb("wr", [128, 8, NE], F32)
                    P.dma("sp", wr[:], IN("moe_router").rearrange("(k p) n -> p k n", p=128), w=[wr])
                    hfs = [sb("hf%d" % i, [128, D], F32) for i in range(2)]
                    hT32s = [sb("hT32_%d" % i, [128, 8, 128], F32) for i in range(2)]
                    gates = sb("gates", [128, PASS, NE], F32)
                    lg = sb("lg", [128, NE], F32)
                    mx8 = sb("mx8", [128, 8], F32)
                    pe_ = sb("pexp", [128, NE], F32)
                    msk = sb("msk", [128, NE], F32)
                    den = sb("den", [128, 1], F32)
                if final:
                    fwb = sb("fwb", [128, D], F32)
                    P.dma("sp", fwb[:], IN("final_norm_w").partition_broadcast(128), w=[fwb])
                    ot = [sb("ot%d" % i, [128, D], F32) for i in range(2)]
                wcount = [0]
                for p0 in range(0, NTT, PASS):
                    npt = min(PASS, NTT - p0)
                    ntb = npt // 4
                    for j in range(npt):
                        i = p0 + j
                        srcs = src_tiles[i]
                        if moe:
                            xb, hf, hT32 = xbs[j % 2], hfs[j % 2], hT32s[j % 2]
                        if not moe:
                            ap, tt = srcs[0]
                            P.dma("sp", acc[j][:], ap, r=[tt], w=[acc[j]])
                        else:
                            (apa, ta), (apb, tb_) = srcs
                            P.dma("sp", acc[j][:], apa, r=[ta], w=[acc[j]])
                            P.dma("sp", xb[:], apb, r=[tb_], w=[xb])
                            P.op("dve", ("tensor_scalar", dict(out=acc[j][:], in0=acc[j][:], scalar1=sel[:, 0:1],
                                                                      scalar2=None, op0=ALU.mult)), r=[acc[j], sel], w=[acc[j]])
                            P.op("dve", ("scalar_tensor_tensor", dict(out=acc[j][:], in0=xb[:], scalar=sel[:, 1:2],
                                                                             in1=acc[j][:], op0=ALU.mult, op1=ALU.add)),
                                 r=[xb, sel, acc[j]], w=[acc[j]])
                        norm_to_hT(acc[j], wb, hT, hT[:, :, j * 128:(j + 1) * 128], (junk, ssq, rstd, hb))
                        if moe:
                            P.op("dve", ("scalar_tensor_tensor", dict(out=hf[:], in0=acc[j][:], scalar=rstd[:], in1=wb[:],
                                                                      op0=ALU.mult, op1=ALU.mult)), r=[acc[j], rstd, wb], w=[hf])
                            for hh in range(2):
                                ptr = psb[4 + hh]

                                def trf(e, ptr=ptr, hh=hh, hf=hf):
                                    ins = None
                                    for k4 in range(4):
                                        k = hh * 4 + k4
                                        ins = e.transpose(out=ptr[:, k4 * 128:(k4 + 1) * 128], in_=hf[:, k * 128:(k + 1) * 128],
                                                          identity=ident_f[:])
                                    return ins
                                P.op("pe", trf, r=[hf, ident_f], w=[ptr])
                                P.op("act", ("copy", dict(out=hT32[:, hh * 4:(hh + 1) * 4, :],
                                                          in_=ptr[:].rearrange("p (k t) -> p k t", k=4))), r=[ptr], w=[hT32])
                            pl = ps1()
                            mm_group(P, pl, pl[:, 0:NE], [(hT32[:, k, :], wr[:, k, :]) for k in range(8)], r=[hT32, wr])
                            P.op("act", ("copy", dict(out=lg[:], in_=pl[:, 0:NE])), r=[pl], w=[lg])
                            P.op("dve", ("max", dict(out=mx8[:], in_=lg[:])), r=[lg], w=[mx8])
                            P.op("dve", ("tensor_scalar", dict(out=msk[:], in0=lg[:], scalar1=mx8[:, 1:2], scalar2=None,
                                                                  op0=ALU.is_ge)), r=[lg, mx8], w=[msk])
                            P.op("dve", ("tensor_scalar", dict(out=pe_[:], in0=lg[:], scalar1=mx8[:, 0:1], scalar2=None,
                                                                  op0=ALU.subtract)), r=[lg, mx8], w=[pe_])
                            P.op("act", ("activation", dict(out=pe_[:], in_=pe_[:], func=AF.Exp)), r=[pe_], w=[pe_])
                            P.op("dve", ("tensor_tensor", dict(out=pe_[:], in0=pe_[:], in1=msk[:], op=ALU.mult)),
                                 r=[pe_, msk], w=[pe_])
                            P.op("dve", ("tensor_reduce", dict(out=den[:], in_=pe_[:], axis=AX.X, op=ALU.add)),
                                 r=[pe_], w=[den])
                            P.op("dve", ("reciprocal", dict(out=den[:], in_=den[:])), r=[den], w=[den])
                            P.op("dve", ("tensor_scalar", dict(out=gates[:, j, :], in0=pe_[:], scalar1=den[:, 0:1],
                                                                      scalar2=None, op0=ALU.mult)), r=[pe_, den], w=[gates])
                    if debug == "f1":
                        raise StopBuild()
                    for ei, (wg_ap, wu_ap, wd_ap) in enumerate(experts):
                        for fb in range(NFC // 2):
                            if debug in ("f2", "f3", "f6") and fb == {"f2": 1, "f3": 3, "f6": 6}[debug]:
                                raise StopBuild()
                            wi = wcount[0] % 2
                            wcount[0] += 1
                            wg, wu, wd = wgs[wi], wus[wi], wds[wi]
                            f0 = fb * 256
                            P.dma("pool", wg[:], wg_ap[:, f0:f0 + 256].rearrange("(k p) n -> p k n", p=128), w=[wg])
                            P.dma("pool", wu[:], wu_ap[:, f0:f0 + 256].rearrange("(k p) n -> p k n", p=128), w=[wu])
                            P.dma("pool", wd[:], wd_ap[f0:f0 + 256, :].rearrange("(c p) n -> p c n", p=128), w=[wd])
                            for tb in range(ntb):
                                tk = slice(tb * 512, (tb + 1) * 512)
                                A = Ab[(wcount[0] * 4 + tb) % 2]
                                for cc in range(2):
                                    pg = ps1()
                                    mm_group(P, pg, pg[:], [(wg[:, k, cc * 128:(cc + 1) * 128], hT[:, k, tk]) for k in range(8)],
                                             r=[wg, hT])
                                    pu = ps1()
                                    mm_group(P, pu, pu[:], [(wu[:, k, cc * 128:(cc + 1) * 128], hT[:, k, tk]) for k in range(8)],
                                             r=[wu, hT])
                                    sg = sgl[cc]
                                    P.op("act", ("activation", dict(out=sg[:], in_=pg[:], func=AF.Silu)),
                                         r=[pg], w=[sg])
                                    P.op("dve", ("tensor_tensor", dict(out=A[:, cc, :], in0=pu[:], in1=sg[:],
                                                                                                     op=ALU.mult)),
                                         r=[pu, sg], w=[A])
                                for t4 in range(4):
                                    j = tb * 4 + t4
                                    for n in range(2):
                                        pd = psb[4 + (t4 * 2 + n) % 4]
                                        mm_group(P, pd, pd[:], [(A[:, cc, t4 * 128:(t4 + 1) * 128], wd[:, cc, n * 512:(n + 1) * 512])
                                                                for cc in range(2)], r=[A, wd])
                                        if moe:
                                            P.op("dve", ("scalar_tensor_tensor", dict(
                                                out=acc[j][:, n * 512:(n + 1) * 512], in0=pd[:], scalar=gates[:, j, ei:ei + 1],
                                                in1=acc[j][:, n * 512:(n + 1) * 512], op0=ALU.mult, op1=ALU.add)),
                                                r=[pd, gates, acc[j]], w=[acc[j]])
                                        else:
                                            P.op("dve", ("tensor_tensor", dict(
                                                out=acc[j][:, n * 512:(n + 1) * 512], in0=pd[:],
                                                in1=acc[j][:, n * 512:(n + 1) * 512], op=ALU.add)),
                                                r=[pd, acc[j]], w=[acc[j]])
                    for j in range(npt):
                        i = p0 + j
                        dap, dt_ = dst_fn(i)
                        if final:
                            o = ot[j % 2]
                            P.op("act", ("activation", dict(out=junk[:], in_=acc[j][:], func=AF.Square, accum_out=ssq[:])),
                                 r=[acc[j]], w=[junk, ssq])
                            P.op("dve", ("tensor_scalar", dict(out=rstd[:], in0=ssq[:], scalar1=1.0 / D, scalar2=EPS,
                                                                  op0=ALU.mult, op1=ALU.add)), r=[ssq], w=[rstd])
                            P.op("act", ("activation", dict(out=rstd[:], in_=rstd[:], func=AF.Sqrt)), r=[rstd], w=[rstd])
                            P.op("dve", ("reciprocal", dict(out=rstd[:], in_=rstd[:])), r=[rstd], w=[rstd])
                            P.op("dve", ("scalar_tensor_tensor", dict(out=o[:], in0=acc[j][:], scalar=rstd[:], in1=fwb[:],
                                                                                  op0=ALU.mult, op1=ALU.mult)),
                                 r=[acc[j], rstd, fwb], w=[o])
                            P.dma("sp", dap, o[:], r=[o], w=[dt_])
                        else:
                            P.dma("sp", dap, acc[j][:], r=[acc[j]], w=[dt_])

        ZS = nc.dram_tensor("ZS", [L, DIN], BF16, kind="Internal").ap()
        XT = nc.dram_tensor("XT", [L, DIN], BF16, kind="Internal").ap()
        BTK = nc.dram_tensor("BTK", [L, 512], BF16, kind="Internal").ap()
        BCD = nc.dram_tensor("BCD", [NT, 128, 1024], BF16, kind="Internal").ap()
        SMD = nc.dram_tensor("SMD", [L, 128], F32, kind="Internal").ap()
        CSD = nc.dram_tensor("CSD", [NT, SSD_H * 128], F32, kind="Internal").ap()
        CBD = nc.dram_tensor("CBD", [NT, 128, 512], F32, kind="Internal").ap()
        XDT = nc.dram_tensor("XDT", [L, DIN], BF16, kind="Internal").ap()
        XWD = nc.dram_tensor("XWD", [L, DIN], BF16, kind="Internal").ap()
        MD = nc.dram_tensor("MD", [NT, 128, SSD_H * 128], BF16, kind="Internal").ap()
        XDTt = [T(None, "XDT_%d" % i) for i in range(NT)]
        XWDt = [T(None, "XWD_%d" % i) for i in range(NT)]
        MDt = [T(None, "MD_%d" % i) for i in range(NT)]
        ZSt = [T(None, "ZS_%d" % i) for i in range(NT)]
        XTt = [T(None, "XT_%d" % i) for i in range(NT)]
        BTKt = [T(None, "BTK_%d" % i) for i in range(NT)]
        BCDt = [T(None, "BCD_%d" % i) for i in range(NT)]
        SMDt = [T(None, "SMD_%d" % i) for i in range(NT)]
        CSDt = [T(None, "CSD_%d" % i) for i in range(NT)]
        CBDt = [T(None, "CBD_%d" % i) for i in range(NT)]

        def phase3b():
            P.barrier()
            with PhaseStack() as es3:
                def sb(name, shape, dt):
                    return T(es3.enter_context(nc.sbuf_tensor("pb_" + name, list(shape), dt)), name)
                w_in = sb("win", [128, 8, SSD_IN], BF16)
                for k in range(8):
                    P.dma("pool", w_in[:, k, :], IN("ssd_w_in")[k * 128:(k + 1) * 128, :], w=[w_in])
                wb = sb("wb", [128, D], F32)
                P.dma("sp", wb[:], IN("mix_norm_w")[1].partition_broadcast(128), w=[wb])
                dtb = sb("dtb", [128, SSD_H], F32)
                P.dma("sp", dtb[:], IN("ssd_dt_bias").partition_broadcast(128), w=[dtb])
                ab = sb("ab", [128, SSD_H], F32)
                P.dma("sp", ab[:], IN("ssd_a_log").partition_broadcast(128), w=[ab])
                P.op("act", ("activation", dict(out=ab[:], in_=ab[:], func=AF.Exp)), r=[ab], w=[ab])
                P.op("dve", ("tensor_scalar", dict(out=ab[:], in0=ab[:], scalar1=-1.0, scalar2=None, op0=ALU.mult)), r=[ab], w=[ab])
                cwr = sb("cwr", [120, 128], F32)
                P.dma("sp", cwr[0:96, :], IN("ssd_conv_w").rearrange("j (c p) -> (j c) p", p=128), w=[cwr])
                P.dma("sp", cwr[96:120, :], IN("ssd_conv_b").rearrange("(c p) -> c p", p=128), w=[cwr])
                cw = sb("cw", [128, 120], F32)
                pc = ps1()
                P.op("pe", ("transpose", dict(out=pc[:, 0:120], in_=cwr[:], identity=ident_f[0:120, 0:120])), r=[cwr, ident_f], w=[pc])
                P.op("act", ("copy", dict(out=cw[:], in_=pc[:, 0:120])), r=[pc], w=[cw])
                diag = sb("diag", [128, 24, 4, 128], BF16)

                def mkdiag(e):
                    ins = None
                    for c in range(24):
                        for j in range(4):
                            ins = e.tensor_scalar(out=diag[:, c, j, :], in0=ident_f[:], scalar1=cw[:, j * 24 + c:j * 24 + c + 1],
                                                  scalar2=None, op0=ALU.mult)
                    return ins
                P.op("dve", mkdiag, r=[cw, ident_f], w=[diag])

                xts = [sb("xt%d" % i, [128, D], F32) for i in range(2)]
                junk = sb("junk", [128, D], BF16)
                ssq = sb("ssq", [128, 1], F32)
                rstd = sb("rstd", [128, 1], F32)
                hb = sb("hb", [128, D], BF16)
                hT = sb("hT", [128, 8, 512], BF16)
                szs = [sb("sz%d" % i, [128, DIN], BF16) for i in range(2)]
                xbc = sb("xbc", [128, 24, 515], BF16)
                xcT = sb("xcT", [128, 16, 512], BF16)
                BCT = sb("BCT", [128, 8, 512], BF16)
                bcts = [sb("bct0", [128, 8, 128], BF16)] * 2
                xtoks = [sb("xtok%d" % i, [128, DIN], BF16) for i in range(2)]
                btks = [sb("btk%d" % i, [128, 4, 128], BF16) for i in range(2)]
                sms = [sb("sm%d" % i, [128, 128], F32) for i in range(2)]
                da = sb("da", [128, SSD_H], F32)
                csTs = [sb("csT%d" % i, [32, 128], F32) for i in range(2)]
                cbms = [sb("cbm%d" % i, [128, 4, 128], F32) for i in range(2)]
                P.op("pool", ("memset", dict(ap=xbc[:], constant=0.0)), w=[xbc])
                for i in range(2):
                    P.op("pool", ("memset", dict(ap=sms[i][:], constant=0.0)), w=[sms[i]])

                for b in range(NBLK):
                    if b > 0:
                        P.op("pool", ("tensor_copy", dict(out=xbc[:, :, 0:3], in_=xbc[:, :, 512:515])), r=[xbc], w=[xbc])
                    for j in range(4):
                        i = b * 4 + j
                        xt = xts[i % 2]
                        P.dma("sp", xt[:], X2[i * 128:(i + 1) * 128, :], r=[X2t[i]], w=[xt])
                        norm_to_hT(xt, wb, hT, hT[:, :, j * 128:(j + 1) * 128], (junk, ssq, rstd, hb))
                    for j in range(4):
                        i = b * 4 + j
                        sz = szs[i % 2]
                        for n in range(4):
                            pz = ps1()
                            mm_group(P, pz, pz[:], [(hT[:, k, j * 128:(j + 1) * 128], w_in[:, k, n * 512:(n + 1) * 512]) for k in range(8)],
                                     r=[hT, w_in])
                            P.op("act", ("activation", dict(out=sz[:, n * 512:(n + 1) * 512], in_=pz[:], func=AF.Silu)), r=[pz], w=[sz])
                        P.dma("sp", ZS[i * 128:(i + 1) * 128, :], sz[:], r=[sz], w=[ZSt[i]])
                    for c in range(24):
                        pp = ps1()
                        mm_group(P, pp, pp[:], [(w_in[:, k, DIN + c * 128:DIN + (c + 1) * 128], hT[:, k, :]) for k in range(8)], r=[w_in, hT])
                        if c % 2 == 0:
                            P.op("act", ("copy", dict(out=xbc[:, c, 3:515], in_=pp[:])), r=[pp], w=[xbc])
                        else:
                            P.op("dve", ("tensor_copy", dict(out=xbc[:, c, 3:515], in_=pp[:])), r=[pp], w=[xbc])
                    for j in range(4):
                        i = b * 4 + j
                        sm = sms[i % 2]
                        csT = csTs[i % 2]
                        pd = ps1()
                        mm_group(P, pd, pd[:, 0:SSD_H], [(hT[:, k, j * 128:(j + 1) * 128], w_in[:, k, DIN + CONV:SSD_IN]) for k in range(8)],
                                 r=[hT, w_in])
                        P.op("dve", ("tensor_tensor", dict(out=sm[:, 0:32], in0=pd[:, 0:SSD_H], in1=dtb[:], op=ALU.add)), r=[pd, dtb], w=[sm])
                        P.op("act", ("activation", dict(out=sm[:, 0:32], in_=sm[:, 0:32], func=AF.Exp)), r=[sm], w=[sm])
                        P.op("act", ("activation", dict(out=sm[:, 0:32], in_=sm[:, 0:32], func=AF.Ln, bias=1.0, scale=1.0)), r=[sm], w=[sm])
                        P.op("dve", ("tensor_tensor", dict(out=da[:], in0=sm[:, 0:32], in1=ab[:], op=ALU.mult)), r=[sm, ab], w=[da])
                        pcs = ps1()
                        P.op("pe", ("matmul", dict(out=pcs[:, 0:SSD_H], lhsT=mask01[:], rhs=da[:], start=True, stop=True)),
                             r=[mask01, da], w=[pcs])
                        P.op("act", ("copy", dict(out=sm[:, 32:64], in_=pcs[:, 0:SSD_H])), r=[pcs], w=[sm])
                        P.op("act", ("activation", dict(out=sm[:, 64:96], in_=pcs[:, 0:SSD_H], func=AF.Exp)), r=[pcs], w=[sm])
                        pct = ps1()
                        P.op("pe", ("matmul", dict(out=pct[0:32, 0:128], lhsT=da[:], rhs=mask01[:], start=True, stop=True)),
                             r=[mask01, da], w=[pct])
                        P.op("act", ("copy", dict(out=csT[:], in_=pct[0:32, 0:128])), r=[pct], w=[csT])
                        P.dma("sp", CSD[i].rearrange("(h t) -> h t", t=128), csT[:], r=[csT], w=[CSDt[i]])
                        P.dma("sp", SMD[i * 128:(i + 1) * 128, :], sm[:], r=[sm], w=[SMDt[i]])
                    for c in range(24):
                        pp = ps1()
                        mm_group(P, pp, pp[:], [(diag[:, c, jj, :], xbc[:, c, jj:jj + 512]) for jj in range(4)], r=[diag, xbc])
                        dst_t = xcT if c < 16 else BCT
                        dst = xcT[:, c, :] if c < 16 else BCT[:, c - 16, :]
                        P.op("act", ("activation", dict(out=dst, in_=pp[:], func=AF.Silu, bias=cw[:, 96 + c:97 + c], scale=1.0)),
                             r=[pp, cw], w=[dst_t])
                    for j in range(4):
                        i = b * 4 + j
                        tk = slice(j * 128, (j + 1) * 128)
                        xtok = xtoks[i % 2]
                        btk = btks[i % 2]
                        bct = bcts[i % 2]
                        cbm = cbms[i % 2]
                        for q4 in range(2):
                            pt = ps1()
                            ptv = pt[:].bitcast(BF16).rearrange("p (k t) -> p k t", k=8)

                            def trx(e, ptv=ptv, q4=q4, tk=tk):
                                ins = None
                                for k in range(8):
                                    ins = e.transpose(out=ptv[:, k, :], in_=xcT[:, q4 * 8 + k, tk], identity=ident[:])
                                return ins
                            P.op("pe", trx, r=[xcT, ident], w=[pt])
                            if q4 == 0:
                                P.op("act", ("copy", dict(out=xtok[:, 0:1024].rearrange("p (k t) -> p k t", k=8), in_=ptv)), r=[pt], w=[xtok])
                            else:
                                P.op("dve", ("tensor_copy", dict(out=xtok[:, 1024:2048].rearrange("p (k t) -> p k t", k=8), in_=ptv)),
                                     r=[pt], w=[xtok])
                        pt = ps1()
                        ptv = pt[:].bitcast(BF16).rearrange("p (k t) -> p k t", k=8)

                        def trb(e, ptv=ptv, tk=tk):
                            ins = None
                            for g in range(4):
                                ins = e.transpose(out=ptv[:, g, :], in_=BCT[:, g, tk], identity=ident[:])
                            return ins
                        P.op("pe", trb, r=[BCT, ident], w=[pt])
                        P.op("act", ("copy", dict(out=btk[:], in_=ptv[:, 0:4, :])), r=[pt], w=[btk])
                        pcb = ps1()

                        def cbmm(e, pcb=pcb, tk=tk):
                            ins = None
                            for g in range(4):
                                ins = e.matmul(pcb[:, g * 128:(g + 1) * 128], lhsT=BCT[:, g, tk], rhs=BCT[:, 4 + g, tk], start=True, stop=True)
                            return ins
                        P.op("pe", cbmm, r=[BCT], w=[pcb])
                        P.op("dve", ("tensor_tensor", dict(out=cbm[:], in0=pcb[:].rearrange("p (g t) -> p g t", g=4),
                                                           in1=mask01[:].unsqueeze(1).broadcast_to([128, 4, 128]), op=ALU.mult)),
                             r=[pcb, mask01], w=[cbm])
                        P.dma("sp", XT[i * 128:(i + 1) * 128, :], xtok[:], r=[xtok], w=[XTt[i]])
                        P.dma("sp", BTK[i * 128:(i + 1) * 128, :], btk[:].rearrange("p g n -> p (g n)"), r=[btk], w=[BTKt[i]])
                        P.dma("sp", BCD[i].rearrange("p (g t) -> p g t", g=8), BCT[:, :, tk], r=[BCT], w=[BCDt[i]])
                        P.dma("sp", CBD[i], cbm[:].rearrange("p g t -> p (g t)"), r=[cbm], w=[CBDt[i]])

        def phase3m():
            P.barrier()
            with PhaseStack() as es3:
                def sb(name, shape, dt):
                    return T(es3.enter_context(nc.sbuf_tensor("pm_" + name, list(shape), dt)), name)
                NB3 = 3
                csBs = [sb("csB%d" % i, [128, SSD_H, 128], F32) for i in range(NB3)]
                sms = [sb("sm%d" % i, [128, 128], F32) for i in range(NB3)]
                cbms = [sb("cbm%d" % i, [128, 4, 128], F32) for i in range(NB3)]
                xtoks = [sb("xtok%d" % i, [128, DIN], BF16) for i in range(NB3)]
                xdts = [sb("xdt%d" % i, [128, DIN], BF16) for i in range(2)]
                xws = [sb("xw%d" % i, [128, DIN], BF16) for i in range(2)]
                wvs = [sb("wv%d" % i, [128, SSD_H], F32) for i in range(2)]
                LTs = [sb("LT%d" % i, [128, 8, 128], BF16) for i in range(2)]
                Mas = [sb("Ma%d" % i, [128, SSD_H, 128], BF16) for i in range(2)]

                def loads(i):
                    P.dma("sp", csBs[i % NB3][:].rearrange("p h t -> p (h t)"), CSD[i].partition_broadcast(128), r=[CSDt[i]],
                          w=[csBs[i % NB3]])
                    P.dma("sp", sms[i % NB3][:], SMD[i * 128:(i + 1) * 128, :], r=[SMDt[i]], w=[sms[i % NB3]])
                    P.dma("sp", cbms[i % NB3][:].rearrange("p g t -> p (g t)"), CBD[i], r=[CBDt[i]], w=[cbms[i % NB3]])
                    P.dma("sp", xtoks[i % NB3][:], XT[i * 128:(i + 1) * 128, :], r=[XTt[i]], w=[xtoks[i % NB3]])

                loads(0)
                if NT > 1:
                    loads(1)
                for i in range(NT):
                    if i + 2 < NT:
                        loads(i + 2)
                    csB, sm, cbm, xtok = csBs[i % NB3], sms[i % NB3], cbms[i % NB3], xtoks[i % NB3]
                    xdt, xw, wv, Ma = xdts[i % 2], xws[i % 2], wvs[i % 2], Mas[i % 2]
                    dt = sm[:, 0:32]
                    cs = sm[:, 32:64]
                    P.op("dve", ("tensor_tensor", dict(out=xdt[:].rearrange("p (h q) -> p h q", h=SSD_H),
                                                       in0=xtok[:].rearrange("p (h q) -> p h q", h=SSD_H),
                                                       in1=dt.unsqueeze(2).broadcast_to([128, SSD_H, 64]), op=ALU.mult)),
                         r=[xtok, sm], w=[xdt])
                    P.dma("act", XDT[i * 128:(i + 1) * 128, :], xdt[:], r=[xdt], w=[XDTt[i]])
                    P.op("dve", ("tensor_tensor", dict(out=wv[:], in0=csB[:, :, 127], in1=cs, op=ALU.subtract)), r=[csB, sm], w=[wv])
                    P.op("act", ("activation", dict(out=wv[:], in_=wv[:], func=AF.Exp)), r=[wv], w=[wv])
                    P.op("act", ("activation", dict(out=sm[:, 96:128], in_=csB[:, :, 127], func=AF.Exp)), r=[csB], w=[sm])
                    P.op("dve", ("tensor_tensor", dict(out=wv[:], in0=wv[:], in1=dt, op=ALU.mult)), r=[wv, sm], w=[wv])
                    P.op("pool", ("tensor_tensor", dict(out=xw[:].rearrange("p (h q) -> p h q", h=SSD_H),
                                                        in0=xtok[:].rearrange("p (h q) -> p h q", h=SSD_H),
                                                        in1=wv[:].unsqueeze(2).broadcast_to([128, SSD_H, 64]), op=ALU.mult)),
                         r=[xtok, wv], w=[xw])
                    P.dma("act", XWD[i * 128:(i + 1) * 128, :], xw[:], r=[xw], w=[XWDt[i]])
                    P.dma("act", SMD[i * 128:(i + 1) * 128, :], sm[:], r=[sm], w=[SMDt[i]])
                    for g in range(4):
                        LT = LTs[g % 2]
                        hs = slice(g * 8, (g + 1) * 8)

                        def dmin(e, g=g, csB=csB, cs=cs):
                            ins = None
                            for r8 in range(8):
                                h = g * 8 + r8
                                ins = e.tensor_scalar(out=csB[:, h, :], in0=csB[:, h, :], scalar1=cs[:, h:h + 1], scalar2=0.0,
                                                      op0=ALU.subtract, op1=ALU.min)
                            return ins
                        P.op("dve", dmin, r=[csB, sm, wv], w=[csB])
                        P.op("act", ("activation", dict(out=LT[:], in_=csB[:, hs, :], func=AF.Exp)), r=[csB], w=[LT])
                        P.op("dve", ("tensor_tensor", dict(out=Ma[:, hs, :], in0=LT[:],
                                                           in1=cbm[:, g, :].unsqueeze(1).broadcast_to([128, 8, 128]), op=ALU.mult)),
                             r=[LT, cbm], w=[Ma])
                    P.dma("act", MD[i], Ma[:].rearrange("p h t -> p (h t)"), r=[Ma], w=[MDt[i]])

        def phase3():
            P.barrier()
            with PhaseStack() as es3:
                def sb(name, shape, dt):
                    return T(es3.enter_context(nc.sbuf_tensor("p3_" + name, list(shape), dt)), name)
                w_out = sb("wout", [128, 16, D], BF16)
                for k in range(4):
                    P.dma("pool", w_out[:, k * 4:(k + 1) * 4, :],
                          IN("ssd_w_out")[k * 512:(k + 1) * 512, :].rearrange("(k p) n -> p k n", p=128), w=[w_out])
                nwb = sb("nwb", [128, DIN], BF16)
                P.dma("pool", nwb[:], IN("ssd_norm_w").partition_broadcast(128), w=[nwb])
                dsk = sb("dsk", [128, SSD_H], F32)
                P.dma("sp", dsk[:], IN("ssd_d").partition_broadcast(128), w=[dsk])
                Did = sb("Did", [128, SSD_H, 128], BF16)

                def mkdid(e):
                    ins = None
                    for h in range(SSD_H):
                        ins = e.tensor_scalar(out=Did[:, h, :], in0=ident_f[:], scalar1=dsk[:, h:h + 1], scalar2=None, op0=ALU.mult)
                    return ins
                P.op("dve", mkdid, r=[dsk, ident_f], w=[Did])
                NBUF = 3
                xt_ = [sb("xt%d" % i, [128, D], F32) for i in range(4)]
                szs = [sb("sz%d" % i, [128, DIN], BF16) for i in range(NBUF)]
                xtoks = [sb("xtok%d" % i, [128, DIN], BF16) for i in range(NBUF)]
                bcts = [sb("bct%d" % i, [128, 8, 128], BF16) for i in range(NBUF)]
                btks = [sb("btk%d" % i, [128, 4, 128], BF16) for i in range(NBUF)]
                sms = [sb("sm%d" % i, [128, 128], F32) for i in range(NBUF)]
                Mts = [sb("Mt%d" % i, [128, SSD_H, 128], BF16) for i in range(NBUF)]
                xdts = [sb("xdt%d" % i, [128, DIN], BF16) for i in range(NBUF)]
                xws = [sb("xw%d" % i, [128, DIN], BF16) for i in range(NBUF)]
                t1 = [sb("t1_%d" % i, [128, 512], F32) for i in range(2)]
                t2 = [sb("t2_%d" % i, [128, 512], F32) for i in range(2)]
                gss = sb("gss", [128, 4], F32)
                grs = sb("grs", [128, 4], F32)
                ybs = [sb("yb%d" % i, [128, DIN], BF16) for i in range(2)]
                ybT = sb("ybT", [128, 16, 128], BF16)
                ST = sb("ST", [128, DIN], F32)
                STb = sb("STb", [128, DIN], BF16)
                P.op("pool", ("memset", dict(ap=ST[:], constant=0.0)), w=[ST])
                P.op("pool", ("memset", dict(ap=STb[:], constant=0.0)), w=[STb])

                def loads(i):
                    q = "act" if i % 2 == 0 else "sp"
                    P.dma("sp", xt_[i % 4][:], X2[i * 128:(i + 1) * 128, :], r=[X2t[i]], w=[xt_[i % 4]])
                    P.dma("sp", szs[i % NBUF][:], ZS[i * 128:(i + 1) * 128, :], r=[ZSt[i]], w=[szs[i % NBUF]])
                    P.dma("sp", xtoks[i % NBUF][:], XT[i * 128:(i + 1) * 128, :], r=[XTt[i]], w=[xtoks[i % NBUF]])
                    P.dma("sp", bcts[i % NBUF][:].rearrange("p g t -> p (g t)"), BCD[i], r=[BCDt[i]], w=[bcts[i % NBUF]])
                    P.dma("sp", btks[i % NBUF][:].rearrange("p g n -> p (g n)"), BTK[i * 128:(i + 1) * 128, :], r=[BTKt[i]], w=[btks[i % NBUF]])
                    P.dma("sp", sms[i % NBUF][:], SMD[i * 128:(i + 1) * 128, :], r=[SMDt[i]], w=[sms[i % NBUF]])
                    P.dma("sp", Mts[i % NBUF][:].rearrange("p h t -> p (h t)"), MD[i], r=[MDt[i]], w=[Mts[i % NBUF]])
                    P.dma("sp", xdts[i % NBUF][:], XDT[i * 128:(i + 1) * 128, :], r=[XDTt[i]], w=[xdts[i % NBUF]])
                    P.dma("sp", xws[i % NBUF][:], XWD[i * 128:(i + 1) * 128, :], r=[XWDt[i]], w=[xws[i % NBUF]])


                def bufs(i):
                    return dict(xt=xt_[i % 4], sz=szs[i % NBUF], xtok=xtoks[i % NBUF], BCT=bcts[i % NBUF], Btok=btks[i % NBUF],
                                sm=sms[i % NBUF], xdt=xdts[i % NBUF], xw=xws[i % NBUF], yb=ybs[i % 2], Mt=Mts[i % NBUF])

                def stageB(i):
                    B = bufs(i)
                    sm, xtok, xdt, sz, yb, BCT = B["sm"], B["xtok"], B["xdt"], B["sz"], B["yb"], B["BCT"]
                    ecs = sm[:, 64:96]
                    for g in range(4):
                        M = B["Mt"]
                        hs = slice(g * 8, (g + 1) * 8)
                        gsl = slice(g * 512, (g + 1) * 512)
                        pyi = psb[4 + (g % 2)]

                        def yimm(e, pyi=pyi, M=M, g=g, xdt=xdt, xtok=xtok):
                            ins = None
                            for r8 in range(8):
                                h = g * 8 + r8
                                e.matmul(pyi[:, r8 * 64:(r8 + 1) * 64], lhsT=M[:, h, :], rhs=xdt[:, h * 64:(h + 1) * 64],
                                         start=True, stop=False)
                                ins = e.matmul(pyi[:, r8 * 64:(r8 + 1) * 64], lhsT=Did[:, h, :], rhs=xtok[:, h * 64:(h + 1) * 64],
                                               start=False, stop=True)
                            return ins
                        P.op("pe", yimm, r=[M, xdt, Did, xtok], w=[pyi])
                        pyo = psb[6 + (g % 2)]
                        P.op("pe", ("matmul", dict(out=pyo[:], lhsT=BCT[:, 4 + g, :], rhs=STb[:, gsl], start=True, stop=True)),
                             r=[BCT, STb], w=[pyo])
                        a1 = t1[g % 2]
                        a2 = t2[g % 2]
                        P.op("dve", ("tensor_tensor", dict(out=a1[:].rearrange("p (h q) -> p h q", h=8),
                                                           in0=pyo[:].rearrange("p (h q) -> p h q", h=8),
                                                           in1=ecs[:, hs].unsqueeze(2).broadcast_to([128, 8, 64]), op=ALU.mult)),
                             r=[pyo, sm], w=[a1])
                        P.op("dve", ("tensor_tensor", dict(out=a1[:], in0=pyi[:], in1=a1[:], op=ALU.add)), r=[pyi, a1], w=[a1])
                        P.op("dve", ("tensor_tensor", dict(out=a1[:], in0=a1[:], in1=sz[:, gsl], op=ALU.mult)), r=[a1, sz], w=[a1])
                        P.op("act", ("activation", dict(out=a2[:], in_=a1[:], func=AF.Square, accum_out=gss[:, g:g + 1])),
                             r=[a1], w=[a2, gss])
                        P.op("dve", ("tensor_scalar", dict(out=grs[:, g:g + 1], in0=gss[:, g:g + 1], scalar1=1.0 / 512, scalar2=EPS,
                                                           op0=ALU.mult, op1=ALU.add)), r=[gss], w=[grs])
                        P.op("act", ("activation", dict(out=grs[:, g:g + 1], in_=grs[:, g:g + 1], func=AF.Sqrt)), r=[grs], w=[grs])
                        P.op("dve", ("reciprocal", dict(out=grs[:, g:g + 1], in_=grs[:, g:g + 1])), r=[grs], w=[grs])
                        P.op("dve", ("scalar_tensor_tensor", dict(out=yb[:, gsl], in0=a1[:], scalar=grs[:, g:g + 1], in1=nwb[:, gsl],
                                                                  op0=ALU.mult, op1=ALU.mult)), r=[a1, grs, nwb], w=[yb])

                def stageC(i):
                    B = bufs(i)
                    Btok, xw, sm = B["Btok"], B["xw"], B["sm"]
                    ecl = sm[:, 96:128]
                    for g in range(4):
                        gsl = slice(g * 512, (g + 1) * 512)
                        hs = slice(g * 8, (g + 1) * 8)
                        pst = ps1()
                        P.op("pe", ("matmul", dict(out=pst[:], lhsT=Btok[:, g, :], rhs=xw[:, gsl], start=True, stop=True)),
                             r=[Btok, xw], w=[pst])
                        P.op("dve", ("tensor_tensor", dict(out=ST[:, gsl].rearrange("p (h q) -> p h q", h=8),
                                                           in0=ST[:, gsl].rearrange("p (h q) -> p h q", h=8),
                                                           in1=ecl[:, hs].unsqueeze(2).broadcast_to([128, 8, 64]), op=ALU.mult)),
                             r=[ST, sm], w=[ST])
                        P.op("dve", ("tensor_tensor", dict(out=ST[:, gsl], in0=pst[:], in1=ST[:, gsl], op=ALU.add)), r=[pst, ST], w=[ST])
                        P.op("act", ("copy", dict(out=STb[:, gsl], in_=ST[:, gsl])), r=[ST], w=[STb])

                def stageD(i):
                    B = bufs(i)
                    xt, yb = B["xt"], B["yb"]
                    for q4 in range(2):
                        pt = ps1()
                        ptv = pt[:].bitcast(BF16).rearrange("p (k t) -> p k t", k=8)

                        def try_(e, ptv=ptv, q4=q4, yb=yb):
                            ins = None
                            for k in range(8):
                                c = q4 * 8 + k
                                ins = e.transpose(out=ptv[:, k, :], in_=yb[:, c * 128:(c + 1) * 128], identity=ident[:])
                            return ins
                        P.op("pe", try_, r=[yb, ident], w=[pt])
                        P.op("act", ("copy", dict(out=ybT[:, q4 * 8:(q4 + 1) * 8, :], in_=ptv)), r=[pt], w=[ybT])
                    for n in range(2):
                        py = ps1()
                        mm_group(P, py, py[:], [(ybT[:, c, :], w_out[:, c, n * 512:(n + 1) * 512]) for c in range(16)], r=[ybT, w_out])
                        P.op("dve", ("tensor_tensor", dict(out=xt[:, n * 512:(n + 1) * 512], in0=py[:], in1=xt[:, n * 512:(n + 1) * 512],
                                                           op=ALU.add)), r=[py, xt], w=[xt])
                    P.dma("sp", X3[i * 128:(i + 1) * 128, :], xt[:], r=[xt], w=[X3t[i]])

                loads(0)
                if NT > 1:
                    loads(1)
                for i in range(NT):
                    if i + 2 < NT:
                        loads(i + 2)
                    stageB(i)
                    stageC(i)
                    if i > 0:
                        stageD(i - 1)
                stageD(NT - 1)

        outT = [T(None, "out%d" % i) for i in range(NT // 2)]
        try:
            phase1()
            if PhaseStack.stopped or debug == "p1":
                raise StopBuild()
            ffn_phase("f0", [[(X1[i * 128:(i + 1) * 128, :], X1t[i])] for i in range(NT)], IN("ffn_norm_w")[0],
                      [(IN("ffn_w_gate"), IN("ffn_w_up"), IN("ffn_w_down"))], False,
                      lambda i: (X2[i * 128:(i + 1) * 128, :], X2t[i]), False)
            if PhaseStack.stopped or debug == "p2":
                raise StopBuild()
            phase3b()
            phase3m()
            phase3()
            if PhaseStack.stopped or debug == "p3":
                raise StopBuild()
            NH = NT // 2
            experts = [(IN("moe_w_gate")[e_], IN("moe_w_up")[e_], IN("moe_w_down")[e_]) for e_ in range(NE)]
            ffn_phase("m1", [[(X3[i * 128:(i + 1) * 128, :], X3t[i]), (X3[(NH + i) * 128:(NH + i + 1) * 128, :], X3t[NH + i])]
                             for i in range(NH)], IN("ffn_norm_w")[1], experts, True,
                      lambda i: (out[i * 128:(i + 1) * 128, :], outT[i]), True)
            P.final_wait("sp", outT)
        except StopBuild:
            pass
        if debug is not None:
            srcX, srcT = {"p1": (X1, X1t), "p2": (X2, X2t), "p3": (X3, X3t)}.get(debug, (X1, X1t))
            dts = [T(None, "dbg%d" % i) for i in range(NT * 4)]
            for i in range(NT):
                for q in range(4):
                    P.dma("sp", dbgt[:], srcX[i * 128:(i + 1) * 128, q * 256:(q + 1) * 256], r=[srcT[i]], w=[dbgt])
                    P.dma("sp", dbg[i * 128:(i + 1) * 128, q * 256:(q + 1) * 256], dbgt[:], r=[dbgt], w=[dts[i * 4 + q]])
            P.final_wait("sp", dts)
        P.emit()
    nc._in_names = list(_ins.keys())
    return nc


_CACHE = {}

SEQ = 8192
NCORES = 8


def kernel(**inputs):
    x = np.asarray(inputs["x"], dtype=np.float32)
    B = x.shape[0]
    L = x.shape[1]
    if L not in _CACHE:
        _CACHE[L] = build_program(L)
    nc = _CACHE[L]
    sq = {}
    for k, v in inputs.items():
        if k == "x":
            continue
        v = np.asarray(v, dtype=np.float32)
        if k in ("mix_norm_w", "ffn_norm_w", "final_norm_w", "hg_lb_logits"):
            sq[k] = np.ascontiguousarray(v)
        else:
            sq[k] = np.ascontiguousarray(v[0])
    in_maps = []
    for c in range(NCORES):
        b, half = c // 2, c % 2
        m = {"x": np.ascontiguousarray(x[b]), "sel": np.array([1.0 - half, float(half)], np.float32)}
        for k in nc._in_names:
            if k not in m:
                m[k] = sq[k]
        in_maps.append({k: m[k] for k in nc._in_names})
    res = run_bass_kernel_spmd(nc, in_maps, core_ids=list(range(NCORES)))
    outp = np.empty((B, L, D), np.float32)
    LH = L // 2
    for c in range(NCORES):
        b, half = c // 2, c % 2
        outp[b, half * LH:(half + 1) * LH] = res.results[c]["out"]
    return outp
```
